# Optimizing a Trainium2 kernel written in Bass

```python
import jax
import jax.numpy as jnp
from jax import lax
import numpy as np


D_MODEL = 2048
BATCH = 1
SEQ = 8192
DEPTH = 4

ROPE_THETA = 10000.0
ROPE_DIM = 64
NORM_EPS = 1e-5

SSM_HEADS = 32
SSM_HEAD_DIM = 64
SSM_INNER = SSM_HEADS * SSM_HEAD_DIM
SSM_GROUPS = 4
SSM_HEADS_PER_GROUP = SSM_HEADS // SSM_GROUPS
SSM_STATE = 128
SSM_CONV = 4
SSM_CHUNK = 128
SSM_CONV_DIM = SSM_INNER + 2 * SSM_GROUPS * SSM_STATE

SWA_Q_HEADS = 16
SWA_KV_HEADS = 2
SWA_HEAD_DIM = ROPE_DIM
SWA_WINDOW = 128
ATTN_BLOCK = 128

EVEN_IN = SSM_INNER + SSM_CONV_DIM + SSM_HEADS + (SWA_Q_HEADS + 2 * SWA_KV_HEADS) * SWA_HEAD_DIM
EVEN_MIX = SSM_INNER + SWA_Q_HEADS * SWA_HEAD_DIM

MLA_HEADS = 16
MLA_NOPE = 128
MLA_ROPE = ROPE_DIM
MLA_V = 128
MLA_RANK = 512
MLA_SCALE = (MLA_NOPE + MLA_ROPE) ** -0.5
IDX_HEADS = 16
IDX_DIM = ROPE_DIM
IDX_TOPK_MAX = 256
ODD_IN = MLA_HEADS * (MLA_NOPE + MLA_ROPE) + MLA_RANK + MLA_ROPE + IDX_HEADS * IDX_DIM + IDX_DIM + IDX_HEADS
ODD_MIX = MLA_HEADS * MLA_V

MOE_GROUPS = 4
MOE_EXPERTS_PER_GROUP = 8
MOE_EXPERTS = MOE_GROUPS * MOE_EXPERTS_PER_GROUP
MOE_TOP_K = 2
MOE_FF = 512
MOE_BLOCK = 128

PLE_DIM = 256

DEEPNORM_ALPHA = (2 * DEPTH) ** 0.25
DEEPNORM_BETA = (8 * DEPTH) ** -0.25
N_EVEN = (DEPTH + 1) // 2
N_ODD = DEPTH // 2

kernel_name = 'hybrid_ssd_swa_dsa_hmoe_deepnorm'

F32 = jnp.float32


def _split(a, sizes):
    return jnp.split(a, [int(v) for v in np.cumsum(sizes)[:-1]], axis=-1)


def layer_norm(x, g, b):
    xf = x.astype(F32)
    xc = xf - xf.mean(-1, keepdims=True)
    var = jnp.mean(xc * xc, -1, keepdims=True)
    return (xc * lax.rsqrt(var + NORM_EPS) * g.astype(F32) + b.astype(F32)).astype(x.dtype)


def rms_norm(x, g):
    xf = x.astype(F32)
    return (xf * lax.rsqrt(jnp.mean(xf * xf, -1, keepdims=True) + NORM_EPS) * g.astype(F32)).astype(x.dtype)


def rope_tables(positions):
    inv = ROPE_THETA ** (-jnp.arange(0, ROPE_DIM, 2, dtype=F32) / ROPE_DIM)
    ang = positions.astype(F32)[..., None] * inv
    return jnp.cos(ang), jnp.sin(ang)


def apply_rope(t, cos, sin):
    tf = t.astype(F32)
    t1, t2 = jnp.split(tf, 2, axis=-1)
    c = cos[:, :, None, :]
    s_ = sin[:, :, None, :]
    return jnp.concatenate([t1 * c - t2 * s_, t2 * c + t1 * s_], axis=-1).astype(t.dtype)


def causal_dwconv(u, w, bias):
    c = u.shape[-1]
    out = lax.conv_general_dilated(u, w[:, None, :].astype(u.dtype), window_strides=(1,),
                                   padding=[(SSM_CONV - 1, 0)],
                                   dimension_numbers=('NWC', 'WIO', 'NWC'),
                                   feature_group_count=c)
    return out + bias.astype(u.dtype)


def ssd_chunked(xdt, a, bm, cm):
    b, s, g, r, p = xdt.shape
    n = bm.shape[-1]
    nc = s // SSM_CHUNK
    X = xdt.reshape(b, nc, SSM_CHUNK, g, r, p)
    A = a.reshape(b, nc, SSM_CHUNK, g, r)
    B = bm.reshape(b, nc, SSM_CHUNK, g, n)
    C = cm.reshape(b, nc, SSM_CHUNK, g, n)
    a_cs = jnp.cumsum(A, axis=2)
    seg = a_cs[:, :, :, None] - a_cs[:, :, None, :]
    causal = jnp.tril(jnp.ones((SSM_CHUNK, SSM_CHUNK), dtype=bool))[:, :, None, None]
    decay = jnp.exp(jnp.where(causal, seg, -jnp.inf))
    cb = jnp.einsum('bclgn,bcsgn->bclsg', C, B)
    y_diag = jnp.einsum('bclsgr,bcsgrp->bclgrp', decay * cb[..., None], X)
    decay_to_end = jnp.exp(a_cs[:, :, -1:] - a_cs)
    states = jnp.einsum('bclgn,bclgrp->bcgrpn', B, X * decay_to_end[..., None])
    chunk_decay = jnp.exp(a_cs[:, :, -1])

    def step(h, inp):
        st, dec = inp
        return h * dec[..., None, None] + st, h

    h0 = jnp.zeros((b, g, r, p, n), F32)
    _, prev = lax.scan(step, h0, (jnp.moveaxis(states, 1, 0), jnp.moveaxis(chunk_decay, 1, 0)))
    prev = jnp.moveaxis(prev, 0, 1)
    y_off = jnp.einsum('bclgn,bcgrpn->bclgrp', C, prev) * jnp.exp(a_cs)[..., None]
    return (y_diag + y_off).reshape(b, s, g, r, p)


def mamba2_group(z, xbc, dt_raw, conv_w, conv_b, dt_bias, a_log, d_skip, ssm_norm):
    b, s, _ = z.shape
    G, R, P, N = SSM_GROUPS, SSM_HEADS_PER_GROUP, SSM_HEAD_DIM, SSM_STATE
    xbc = jax.nn.silu(causal_dwconv(xbc, conv_w, conv_b))
    xs, bm, cm = _split(xbc, [SSM_INNER, G * N, G * N])
    xf = xs.reshape(b, s, G, R, P).astype(F32)
    bm = bm.reshape(b, s, G, N).astype(F32)
    cm = cm.reshape(b, s, G, N).astype(F32)
    dt = jax.nn.softplus(dt_raw.astype(F32) + dt_bias.astype(F32)).reshape(b, s, G, R)
    a = -jnp.exp(a_log.astype(F32)).reshape(G, R)
    y = ssd_chunked(xf * dt[..., None], dt * a, bm, cm)
    y = y + xf * d_skip.astype(F32).reshape(G, R)[:, :, None]
    y = y * jax.nn.silu(z.astype(F32)).reshape(b, s, G, R, P)
    y = y.reshape(b, s, G, R * P)
    y = y * lax.rsqrt(jnp.mean(y * y, -1, keepdims=True) + NORM_EPS) * ssm_norm.astype(F32).reshape(G, R * P)
    return y.reshape(b, s, SSM_INNER).astype(z.dtype)


def swa_sink_attention(q, k, v, sinks):
    b, s, _, d = q.shape
    nb = s // ATTN_BLOCK
    grp = SWA_Q_HEADS // SWA_KV_HEADS
    qb = q.reshape(b, nb, ATTN_BLOCK, SWA_KV_HEADS, grp, d)

    def banded(t):
        t = t.reshape(b, nb, ATTN_BLOCK, SWA_KV_HEADS, d)
        prev = jnp.concatenate([jnp.zeros_like(t[:, :1]), t[:, :-1]], axis=1)
        return jnp.concatenate([prev, t], axis=2)

    kw, vw = banded(k), banded(v)
    logits = jnp.einsum('bnqkgd,bnskd->bnkgqs', qb, kw).astype(F32) * (d ** -0.5)
    qpos = jnp.arange(ATTN_BLOCK)[:, None] + ATTN_BLOCK
    kpos = jnp.arange(2 * ATTN_BLOCK)[None, :]
    rel = qpos - kpos
    band = (rel >= 0) & (rel < SWA_WINDOW)
    key_abs = jnp.arange(nb)[:, None] * ATTN_BLOCK + kpos - ATTN_BLOCK
    mask = band[None] & (key_abs >= 0)[:, None, :]
    logits = jnp.where(mask[None, :, None, None], logits, -jnp.inf)
    sink = sinks.astype(F32).reshape(SWA_KV_HEADS, grp)[None, None, :, :, None, None]
    m = jnp.maximum(logits.max(-1, keepdims=True), sink)
    e = jnp.exp(logits - m)
    prob = e / (e.sum(-1, keepdims=True) + jnp.exp(sink - m))
    out = jnp.einsum('bnkgqs,bnskd->bnqkgd', prob.astype(v.dtype), vw)
    return out.reshape(b, s, SWA_Q_HEADS * d)


def ssd_swa_mixer(x, cos, sin, w_in, conv_w, conv_b, dt_bias, a_log, d_skip, ssm_norm, sinks, w_out):
    b, s, _ = x.shape
    kvw = SWA_KV_HEADS * SWA_HEAD_DIM
    z, xbc, dt_raw, q, k, v = _split(x @ w_in, [SSM_INNER, SSM_CONV_DIM, SSM_HEADS,
                                                 SWA_Q_HEADS * SWA_HEAD_DIM, kvw, kvw])
    y_ssm = mamba2_group(z, xbc, dt_raw, conv_w, conv_b, dt_bias, a_log, d_skip, ssm_norm)
    q = apply_rope(q.reshape(b, s, SWA_Q_HEADS, SWA_HEAD_DIM), cos, sin)
    k = apply_rope(k.reshape(b, s, SWA_KV_HEADS, SWA_HEAD_DIM), cos, sin)
    v = v.reshape(b, s, SWA_KV_HEADS, SWA_HEAD_DIM)
    y_att = swa_sink_attention(q, k, v, sinks)
    return jnp.concatenate([y_ssm, y_att], axis=-1) @ w_out


def dsa_mla_mixer(x, cos, sin, w_in, kv_norm, w_uk, w_uv, w_out):
    b, s, _ = x.shape
    q, ckv, krope, qi, ki, wi = _split(x @ w_in, [MLA_HEADS * (MLA_NOPE + MLA_ROPE), MLA_RANK, MLA_ROPE,
                                                  IDX_HEADS * IDX_DIM, IDX_DIM, IDX_HEADS])
    q = q.reshape(b, s, MLA_HEADS, MLA_NOPE + MLA_ROPE)
    q_nope, q_rope = q[..., :MLA_NOPE], q[..., MLA_NOPE:]
    q_rope = apply_rope(q_rope, cos, sin)
    krope = apply_rope(krope[:, :, None, :], cos, sin)[:, :, 0]
    ckv = rms_norm(ckv, kv_norm)
    q_lat = jnp.einsum('bshd,hdr->bshr', q_nope, w_uk)
    qi = apply_rope(qi.reshape(b, s, IDX_HEADS, IDX_DIM), cos, sin)
    ki = apply_rope(ki[:, :, None, :], cos, sin)[:, :, 0]
    wi = wi * (IDX_HEADS ** -0.5 * IDX_DIM ** -0.5)
    topk = min(IDX_TOPK_MAX, s // 4)
    nb = s // ATTN_BLOCK
    key_pos = jnp.arange(s)
    bi = jnp.arange(b)[:, None, None]

    def blocks(t):
        return jnp.moveaxis(t.reshape(b, nb, ATTN_BLOCK, *t.shape[2:]), 1, 0)

    def attend(args):
        qi_b, wi_b, ql_b, qr_b, t_pos = args
        sc = jax.nn.relu(jnp.einsum('bqhd,bsd->bqhs', qi_b, ki))
        idx_score = jnp.einsum('bqhs,bqh->bqs', sc, wi_b).astype(F32)
        visible = key_pos[None, :] <= t_pos[:, None]
        idx_score = jnp.where(visible[None], idx_score, -jnp.inf)
        _, sel = lax.top_k(idx_score, topk)
        valid = sel <= t_pos[None, :, None]
        c_sel = ckv[bi, sel]
        kr_sel = krope[bi, sel]
        logits = (jnp.einsum('bqhr,bqkr->bqhk', ql_b, c_sel)
                  + jnp.einsum('bqhd,bqkd->bqhk', qr_b, kr_sel)).astype(F32) * MLA_SCALE
        logits = jnp.where(valid[:, :, None, :], logits, -jnp.inf)
        prob = jax.nn.softmax(logits, axis=-1).astype(c_sel.dtype)
        return jnp.einsum('bqhk,bqkr->bqhr', prob, c_sel)

    o_lat = lax.map(attend, (blocks(qi), blocks(wi), blocks(q_lat), blocks(q_rope),
                             jnp.arange(s).reshape(nb, ATTN_BLOCK)))
    o_lat = jnp.moveaxis(o_lat, 0, 1).reshape(b, s, MLA_HEADS, MLA_RANK)
    o = jnp.einsum('bshr,hrv->bshv', o_lat, w_uv).reshape(b, s, ODD_MIX)
    return o @ w_out


def hier_moe(x, r_group, r_group_b, r_expert, r_expert_b, w_gate, w_up, w_down):
    b, s, d = x.shape
    h = x.reshape(-1, d)
    t = h.shape[0]
    g_logits = (h @ r_group).astype(F32) + r_group_b.astype(F32)
    g_sel = jnp.argmax(g_logits, axis=-1)
    g_gate = jnp.take_along_axis(jax.nn.softmax(g_logits, axis=-1), g_sel[:, None], axis=-1)
    e_logits = ((h @ r_expert).astype(F32) + r_expert_b.astype(F32)).reshape(t, MOE_GROUPS, MOE_EXPERTS_PER_GROUP)
    e_logits = jnp.take_along_axis(e_logits, g_sel[:, None, None], axis=1)[:, 0]
    top_v, top_i = lax.top_k(e_logits, MOE_TOP_K)
    gate = jax.nn.softmax(top_v, axis=-1) * g_gate
    expert_id = (g_sel[:, None] * MOE_EXPERTS_PER_GROUP + top_i).reshape(-1)
    n = expert_id.shape[0]
    order = jnp.argsort(expert_id)
    sorted_e = expert_id[order]
    counts = jnp.bincount(expert_id, length=MOE_EXPERTS)
    padded = (counts + MOE_BLOCK - 1) // MOE_BLOCK * MOE_BLOCK
    pad_end = jnp.cumsum(padded)
    pad_start = pad_end - padded
    start = jnp.cumsum(counts) - counts
    dest = pad_start[sorted_e] + jnp.arange(n) - start[sorted_e]
    n_rows = n + MOE_EXPERTS * MOE_BLOCK
    row_token = jnp.full((n_rows,), t, jnp.int32).at[dest].set((order // MOE_TOP_K).astype(jnp.int32))
    h_pad = jnp.concatenate([h, jnp.zeros((1, d), h.dtype)], axis=0)
    xin = h_pad[row_token].reshape(n_rows // MOE_BLOCK, MOE_BLOCK, d)
    blk_start = jnp.arange(n_rows // MOE_BLOCK) * MOE_BLOCK
    blk_e = jnp.minimum(jnp.searchsorted(pad_end, blk_start, side='right'), MOE_EXPERTS - 1)

    def expert_block(args):
        xb, e = args
        hid = jax.nn.silu(xb @ w_gate[e]) * (xb @ w_up[e])
        return hid @ w_down[e]

    y_rows = lax.map(expert_block, (xin, blk_e)).reshape(n_rows, d)
    y_assign = jnp.zeros((n, d), y_rows.dtype).at[order].set(y_rows[dest])
    y = (y_assign.reshape(t, MOE_TOP_K, d) * gate[..., None].astype(y_rows.dtype)).sum(axis=1)
    return y.reshape(b, s, d)


def setup_inputs(seed: int = 0) -> dict:
    key = jax.random.key(seed)
    ks = iter(jax.random.split(key, 40))

    def nrm(shape, scale):
        return jax.random.normal(next(ks), shape, F32) * scale

    dt = jnp.exp(jax.random.uniform(next(ks), (N_EVEN, SSM_HEADS), F32)
                 * (np.log(0.1) - np.log(0.001)) + np.log(0.001))
    return {
        'x': nrm((BATCH, SEQ, D_MODEL), 1.0),
        'p': nrm((DEPTH, BATCH, SEQ, PLE_DIM), 1.0),
        'positions': (jax.random.randint(next(ks), (BATCH, 1), 0, 4096, jnp.int32)
                      + jnp.arange(SEQ, dtype=jnp.int32)[None, :]),
        'ev_w_in': nrm((N_EVEN, D_MODEL, EVEN_IN), D_MODEL ** -0.5),
        'ev_conv_w': nrm((N_EVEN, SSM_CONV, SSM_CONV_DIM), SSM_CONV ** -0.5),
        'ev_conv_b': nrm((N_EVEN, SSM_CONV_DIM), 0.01),
        'ev_dt_bias': dt + jnp.log(-jnp.expm1(-dt)),
        'ev_a_log': jnp.log(jax.random.uniform(next(ks), (N_EVEN, SSM_HEADS), F32, 1.0, 16.0)),
        'ev_d_skip': 1.0 + nrm((N_EVEN, SSM_HEADS), 0.01),
        'ev_ssm_norm': 1.0 + nrm((N_EVEN, SSM_INNER), 0.01),
        'ev_sinks': nrm((N_EVEN, SWA_Q_HEADS), 0.5),
        'ev_w_out': nrm((N_EVEN, EVEN_MIX, D_MODEL), EVEN_MIX ** -0.5 * DEEPNORM_BETA),
        'od_w_in': nrm((N_ODD, D_MODEL, ODD_IN), D_MODEL ** -0.5),
        'od_kv_norm': 1.0 + nrm((N_ODD, MLA_RANK), 0.01),
        'od_w_uk': nrm((N_ODD, MLA_HEADS, MLA_NOPE, MLA_RANK), MLA_RANK ** -0.5),
        'od_w_uv': nrm((N_ODD, MLA_HEADS, MLA_RANK, MLA_V), MLA_RANK ** -0.5),
        'od_w_out': nrm((N_ODD, ODD_MIX, D_MODEL), ODD_MIX ** -0.5 * DEEPNORM_BETA),
        'ln1_g': 1.0 + nrm((DEPTH, D_MODEL), 0.01),
        'ln1_b': nrm((DEPTH, D_MODEL), 0.01),
        'ln2_g': 1.0 + nrm((DEPTH, D_MODEL), 0.01),
        'ln2_b': nrm((DEPTH, D_MODEL), 0.01),
        'moe_router_group': nrm((DEPTH, D_MODEL, MOE_GROUPS), D_MODEL ** -0.5),
        'moe_router_group_b': nrm((DEPTH, MOE_GROUPS), 0.01),
        'moe_router_expert': nrm((DEPTH, D_MODEL, MOE_EXPERTS), D_MODEL ** -0.5),
        'moe_router_expert_b': nrm((DEPTH, MOE_EXPERTS), 0.01),
        'moe_w_gate': nrm((DEPTH, MOE_EXPERTS, D_MODEL, MOE_FF), D_MODEL ** -0.5),
        'moe_w_up': nrm((DEPTH, MOE_EXPERTS, D_MODEL, MOE_FF), D_MODEL ** -0.5),
        'moe_w_down': nrm((DEPTH, MOE_EXPERTS, MOE_FF, D_MODEL), MOE_FF ** -0.5 * DEEPNORM_BETA),
        'ple_w_proj': nrm((DEPTH, PLE_DIM, D_MODEL), PLE_DIM ** -0.5),
        'ple_w_gate': nrm((DEPTH, D_MODEL, D_MODEL), D_MODEL ** -0.5),
        'ple_b_gate': nrm((DEPTH, D_MODEL), 0.01),
    }


def reference(x, p, positions, ev_w_in, ev_conv_w, ev_conv_b, ev_dt_bias, ev_a_log, ev_d_skip,
              ev_ssm_norm, ev_sinks, ev_w_out, od_w_in, od_kv_norm, od_w_uk, od_w_uv, od_w_out,
              ln1_g, ln1_b, ln2_g, ln2_b, moe_router_group, moe_router_group_b, moe_router_expert,
              moe_router_expert_b, moe_w_gate, moe_w_up, moe_w_down, ple_w_proj, ple_w_gate, ple_b_gate):
    cos, sin = rope_tables(positions)
    for i in range(DEPTH):
        j = i // 2
        if i % 2 == 0:
            mix = ssd_swa_mixer(x, cos, sin, ev_w_in[j], ev_conv_w[j], ev_conv_b[j], ev_dt_bias[j],
                                ev_a_log[j], ev_d_skip[j], ev_ssm_norm[j], ev_sinks[j], ev_w_out[j])
        else:
            mix = dsa_mla_mixer(x, cos, sin, od_w_in[j], od_kv_norm[j], od_w_uk[j], od_w_uv[j], od_w_out[j])
        x = layer_norm(DEEPNORM_ALPHA * x + mix, ln1_g[i], ln1_b[i])
        ffn = hier_moe(x, moe_router_group[i], moe_router_group_b[i], moe_router_expert[i],
                       moe_router_expert_b[i], moe_w_gate[i], moe_w_up[i], moe_w_down[i])
        x = layer_norm(DEEPNORM_ALPHA * x + ffn, ln2_g[i], ln2_b[i])
        x = x + jax.nn.sigmoid(x @ ple_w_gate[i] + ple_b_gate[i]) * (p[i] @ ple_w_proj[i])
    return x
```

```python
import numpy as np
from contextlib import ExitStack
import concourse.bass as bass
import concourse.mybir as mybir
from concourse.bass_utils import run_bass_kernel_spmd

F32 = mybir.dt.float32
BF16 = mybir.dt.bfloat16
I32 = mybir.dt.int32
AF = mybir.ActivationFunctionType
ALU = mybir.AluOpType
AX = mybir.AxisListType

COMPUTE = ("tensor", "vector", "scalar", "gpsimd")
QUEUES = ("sync", "scalar", "gpsimd")
NSLOT = 8


class Prog:
    def __init__(self, nc):
        self.nc = nc
        self.ops = []
        self.es = None

    def sb(self, name, shape, dt=F32):
        self.uid = getattr(self, "uid", 0) + 1
        return self.es.enter_context(self.nc.sbuf_tensor("s%d_%s" % (self.uid, name), list(shape), dt))

    def ps(self, name, shape, dt=F32):
        self.uid = getattr(self, "uid", 0) + 1
        return self.es.enter_context(self.nc.psum_tensor("p%d_%s" % (self.uid, name), list(shape), dt))

    def dram(self, name, shape, dt=F32, kind=None):
        if kind is None:
            return self.nc.dram_tensor(name, list(shape), dt)
        return self.nc.dram_tensor(name, list(shape), dt, kind=kind)

    def allgather(self, out_t, in_t, n=8):
        self.add("gpsimd", lambda e: e.collective_compute("AllGather", ALU.bypass, replica_groups=[list(range(n))],
                                                          ins=[in_t.ap().opt()], outs=[out_t.ap().opt()]),
                 [in_t], [out_t], dma="cc")

    @staticmethod
    def _keys(aps):
        ks = []
        for a in aps:
            if a is None or isinstance(a, (int, float)):
                continue
            ks.append(a.tensor.name if hasattr(a, "tensor") else a.name)
        return ks

    def add(self, eng, fn, r, w, dma=False):
        self.ops.append(dict(eng=eng, fn=fn, r=self._keys(r), w=self._keys(w), dma=dma))

    def mm(self, out, lhsT, rhs, start=True, stop=True):
        self.add("tensor", lambda e: e.matmul(out, lhsT, rhs, start=start, stop=stop), [lhsT, rhs], [out])

    def tr(self, out, in_, ident):
        self.add("tensor", lambda e: e.transpose(out, in_, ident), [in_, ident], [out])

    def act(self, out, in_, func, bias=0.0, scale=1.0, accum_out=None, eng="scalar"):
        r = [in_] + [b for b in (bias, scale) if not isinstance(b, (int, float))]
        w = [out] + ([accum_out] if accum_out is not None else [])
        if accum_out is None:
            self.add(eng, lambda e: e.activation(out, in_, func, bias=bias, scale=scale), r, w)
        else:
            self.add(eng, lambda e: e.activation(out, in_, func, bias=bias, scale=scale, accum_out=accum_out), r, w)

    def tt(self, out, in0, in1, op, eng="vector"):
        self.add(eng, lambda e: e.tensor_tensor(out, in0, in1, op), [in0, in1], [out])

    def ts(self, out, in0, s1, s2=None, op0=ALU.mult, op1=None, accum_out=None, eng="vector"):
        r = [in0] + [s for s in (s1, s2) if s is not None and not isinstance(s, (int, float))]
        w = [out] + ([accum_out] if accum_out is not None else [])
        kw = {}
        if op1 is not None:
            kw["op1"] = op1
        if accum_out is not None:
            kw["accum_out"] = accum_out
        self.add(eng, lambda e: e.tensor_scalar(out, in0, s1, s2, op0, **kw), r, w)

    def stt(self, out, in0, scalar, in1, op0, op1, eng="vector"):
        r = [in0, in1] + ([scalar] if not isinstance(scalar, (int, float)) else [])
        self.add(eng, lambda e: e.scalar_tensor_tensor(out, in0, scalar, in1, op0, op1), r, [out])

    def copy(self, out, in_, eng="vector"):
        if eng == "scalar":
            self.add(eng, lambda e: e.copy(out, in_), [in_], [out])
        else:
            self.add(eng, lambda e: e.tensor_copy(out, in_), [in_], [out])

    def reduce(self, out, in_, op, axis=AX.X, eng="vector"):
        self.add(eng, lambda e: e.tensor_reduce(out, in_, axis, op), [in_], [out])

    def memset(self, ap, val, eng="vector"):
        self.add(eng, lambda e: e.memset(ap, val), [], [ap])

    def recip(self, out, in_):
        self.add("vector", lambda e: e.reciprocal(out, in_), [in_], [out])

    def dma(self, out, in_, q="sync", **kw):
        self.add(q, lambda e: e.dma_start(out, in_, **kw), [in_], [out], dma=True)

    def raw(self, eng, fn, r, w):
        self.add(eng, fn, r, w)

    def _init_sems(self):
        nc = self.nc
        self.ges = ExitStack()
        self.eng_sem = {e: self.ges.enter_context(nc.semaphore("sem_" + e)) for e in COMPUTE}
        self.slot_sem = {q: [self.ges.enter_context(nc.semaphore("dq_%s_%d" % (q, s))) for s in range(NSLOT)]
                         for q in QUEUES}
        self.cc_sem = self.ges.enter_context(nc.semaphore("cc_sem"))
        self.cc_cnt = 0
        self.eng_cnt = {e: 0 for e in COMPUTE}
        self.q_cnt = {q: 0 for q in QUEUES}
        self.slot_val = {q: [0] * NSLOT for q in QUEUES}
        self.total_ops = 0

    def begin(self):
        if not hasattr(self, "eng_sem"):
            self._init_sems()
        self.es = ExitStack()
        self.ops = []

    def end(self):
        nc = self.nc
        ops = self.ops
        last_w = {}
        readers = {}
        for i, op in enumerate(ops):
            deps = set()
            for k in op["r"]:
                if k in last_w:
                    deps.add(last_w[k])
            for k in op["w"]:
                if k in last_w:
                    deps.add(last_w[k])
                deps.update(readers.get(k, ()))
            deps.discard(i)
            op["deps"] = deps
            for k in op["r"]:
                readers.setdefault(k, []).append(i)
            for k in op["w"]:
                last_w[k] = i
                readers[k] = []
        eng_sem, slot_sem = self.eng_sem, self.slot_sem
        eng_cnt, q_cnt, slot_val = self.eng_cnt, self.q_cnt, self.slot_val
        for op in ops:
            if op["dma"] == "cc":
                self.cc_cnt += 1
                op["tok"] = (self.cc_sem, self.cc_cnt)
                op["slot_prev"] = (self.cc_sem, self.cc_cnt - 1)
            elif op["dma"]:
                q = op["eng"]
                s = q_cnt[q] % NSLOT
                q_cnt[q] += 1
                op["slot_prev"] = (slot_sem[q][s], slot_val[q][s])
                slot_val[q][s] += 16
                op["tok"] = (slot_sem[q][s], slot_val[q][s])
            else:
                e = op["eng"]
                eng_cnt[e] += 1
                op["tok"] = (eng_sem[e], eng_cnt[e])
        per_eng = {e: [] for e in set(COMPUTE) | set(QUEUES)}
        for i, op in enumerate(ops):
            per_eng[op["eng"]].append(i)
        self.total_ops += len(ops)
        final = []
        for q in QUEUES:
            for s in range(NSLOT):
                if slot_val[q][s] > 0:
                    final.append((slot_sem[q][s], slot_val[q][s]))
        for en in COMPUTE:
            if eng_cnt[en] > 0:
                final.append((eng_sem[en], eng_cnt[en]))
        if self.cc_cnt > 0:
            final.append((self.cc_sem, self.cc_cnt))

        def run_engine(ename, e):
            seen = {}
            for i in per_eng[ename]:
                op = ops[i]
                waits = {}
                for j in op["deps"]:
                    dj = ops[j]
                    if ename == "tensor" and dj["eng"] == "tensor" and not dj["dma"]:
                        continue
                    sem, val = dj["tok"]
                    waits[sem] = max(waits.get(sem, 0), val)
                if op["dma"]:
                    sem, val = op["slot_prev"]
                    if val > 0:
                        waits[sem] = max(waits.get(sem, 0), val)
                for sem, val in waits.items():
                    if seen.get(sem, 0) >= val:
                        continue
                    e.wait_ge(sem, val)
                    seen[sem] = val
                ins = op["fn"](e)
                sem, val = op["tok"]
                if op["dma"] == "cc":
                    ins.then_inc(sem)
                else:
                    ins.then_inc(sem, 16 if op["dma"] else 1)
            for sem, val in final:
                e.wait_ge(sem, val)

        with nc.Block() as block:
            @block.tensor
            def _(e):
                run_engine("tensor", e)

            @block.vector
            def _(e):
                run_engine("vector", e)

            @block.scalar
            def _(e):
                run_engine("scalar", e)

            @block.gpsimd
            def _(e):
                run_engine("gpsimd", e)

            @block.sync
            def _(e):
                run_engine("sync", e)
        self.es.close()
        self.ops = []

    def finish(self):
        self.ges.close()


T = 1024
NT = T // 128
D = 2048
KC = D // 128
ALPHA = 8 ** 0.25
EPS = 1e-5


def bcast_rows(ap1d, n, parts=128):
    return ap1d.rearrange("(o n) -> o n", o=1).broadcast_to([parts, n])


def load_consts(p, c):
    ident = p.sb("ident", [128, 128], F32)
    p.dma(ident[:], c["ident"][:, :])
    identb = p.sb("identb", [128, 128], BF16)
    p.copy(identb[:], ident[:])
    return ident, identb


def ln_scratch(p, n=2):
    return [(p.sb("lnst_%d" % i, [128, 4, 6], F32), p.sb("lnmv_%d" % i, [128, 2], F32),
             p.sb("lnrs_%d" % i, [128, 1], F32)) for i in range(n)]


def layer_norm_tile(p, out, v, g_rows, b_rows, scr):
    stats, mv, rstd = scr
    for c in range(4):
        p.raw("vector", lambda e, c=c: e.bn_stats(stats[:, c, :], v[:, c * 512:(c + 1) * 512]), [v], [stats])
    p.raw("vector", lambda e: e.bn_aggr(mv[:], stats[:].rearrange("p a b -> p (a b)")), [stats], [mv])
    p.ts(rstd[:], mv[:, 1:2], EPS, None, op0=ALU.add)
    p.act(rstd[:], rstd[:], AF.Sqrt)
    p.recip(rstd[:], rstd[:])
    p.ts(out, v, mv[:, 0:1], rstd[:, 0:1], op0=ALU.subtract, op1=ALU.mult)
    p.tt(out, out, g_rows, ALU.mult, eng="gpsimd")
    p.tt(out, out, b_rows, ALU.add, eng="gpsimd")


def transpose_to(p, dstT, src, ident, nchunks, tcol, pst, copy_engs=("vector", "scalar")):
    for g in range(0, nchunks, 4):
        ps = pst[(g // 4) % len(pst)]
        n = min(4, nchunks - g)
        for j in range(n):
            p.tr(ps[:, j * 128:(j + 1) * 128], src[:, (g + j) * 128:(g + j + 1) * 128], ident[:])
        eng = copy_engs[(g // 4) % len(copy_engs)]
        p.copy(dstT[:, g:g + n, tcol:tcol + 128], ps[:, 0:n * 128].rearrange("p (a b) -> p a b", a=n), eng=eng)


def load_w_bf16(p, dst, src, stg, n, engs=("scalar", "gpsimd"), qs=("sync", "sync"), g=4):
    kc = dst.shape[1]
    st = getattr(p, "_wctr", 0)
    for i, k0 in enumerate(range(0, kc, g)):
        k1 = min(kc, k0 + g)
        s_ = stg[(st + i) % len(stg)]
        p.dma(s_[:, 0:k1 - k0, 0:n], src[:, k0:k1, :], q=qs[(st + i) % len(qs)])
        p.copy(dst[:, k0:k1, :], s_[:, 0:k1 - k0, 0:n], eng=engs[(st + i) % len(engs)])
    p._wctr = st + (kc + g - 1) // g


def stage_tail(p, c, x1_d, ffn_d, pl_d, ln_g, ln_b, wg_d, bg_d, wp_d, out_d):
    p.begin()
    ident, identb = load_consts(p, c)
    g_rows = p.sb("g_rows", [128, D], F32)
    b_rows = p.sb("b_rows", [128, D], F32)
    bg_rows = p.sb("bg_rows", [128, D], F32)
    p.dma(g_rows[:], bcast_rows(ln_g, D))
    p.dma(b_rows[:], bcast_rows(ln_b, D))
    p.dma(bg_rows[:], bcast_rows(bg_d, D))
    x2 = p.sb("x2", [128, NT, D], F32)
    x2T = p.sb("x2T", [128, KC, T], BF16)
    plT = p.sb("plT", [128, 2, T], BF16)
    pst = [p.ps("pst0", [128, 512]), p.ps("pst1", [128, 512])]
    fa = [p.sb("fa0", [128, D], F32)] * 2
    pa = [p.sb("pa0", [128, 256], F32), p.sb("pa1", [128, 256], F32)]
    lns = ln_scratch(p)
    for tt in range(NT):
        a, f, pp = x2[:, tt, :], fa[tt % 2], pa[tt % 2]
        p.dma(a, x1_d[tt * 128:(tt + 1) * 128, :])
        p.dma(f[:], ffn_d[tt * 128:(tt + 1) * 128, :], q="gpsimd")
        p.dma(pp[:], pl_d[tt * 128:(tt + 1) * 128, :])
        p.stt(a, a, ALPHA, f[:], ALU.mult, ALU.add)
        layer_norm_tile(p, a, a, g_rows[:], b_rows[:], lns[tt % 2])
        transpose_to(p, x2T, x2[:, tt, :], ident, KC, tt * 128, pst)
        transpose_to(p, plT, pp, ident, 2, tt * 128, pst)
    wst = [p.sb("wst0", [128, 4, 512], F32), p.sb("wst1", [128, 4, 512], F32)]
    wbf = [p.sb("wbf0", [128, KC, 512], BF16), p.sb("wbf1", [128, KC, 512], BF16)]
    wpbf = [p.sb("wpbf0", [128, 2, 512], BF16), p.sb("wpbf1", [128, 2, 512], BF16)]
    psg = [p.ps("psg0", [128, 512]), p.ps("psg1", [128, 512])]
    psp = [p.ps("psp0", [128, 512]), p.ps("psp1", [128, 512])]
    gate = [p.sb("gate0", [128, 512], F32), p.sb("gate1", [128, 512], F32)]
    ob = [p.sb("ob0", [128, 512], F32), p.sb("ob1", [128, 512], F32)]
    for n in range(4):
        cs = slice(n * 512, (n + 1) * 512)
        wb, wpb = wbf[n % 2], wpbf[n % 2]
        load_w_bf16(p, wb, wg_d[:, cs].rearrange("(kc k) n -> k kc n", k=128), wst, 512)
        load_w_bf16(p, wpb, wp_d[:, cs].rearrange("(kc k) n -> k kc n", k=128), wst, 512)
        for tt in range(NT):
            i = n * NT + tt
            pg, pp_, gt, o = psg[i % 2], psp[i % 2], gate[i % 2], ob[i % 2]
            for kc in range(KC):
                p.mm(pg[:], x2T[:, kc, tt * 128:(tt + 1) * 128], wb[:, kc, :], start=(kc == 0), stop=(kc == KC - 1))
            for kc in range(2):
                p.mm(pp_[:], plT[:, kc, tt * 128:(tt + 1) * 128], wpb[:, kc, :], start=(kc == 0), stop=(kc == 1))
            p.tt(gt[:], pg[:], bg_rows[:, cs], ALU.add)
            p.act(gt[:], gt[:], AF.Sigmoid)
            p.tt(o[:], pp_[:], gt[:], ALU.mult)
            p.tt(o[:], o[:], x2[:, tt, cs], ALU.add, eng="gpsimd")
            p.dma(out_d[tt * 128:(tt + 1) * 128, cs], o[:], q="scalar")
    p.end()


CST_NAMES = ["ident", "triu", "trius", "ones", "iota", "tril_s"]


def make_cst():
    r = np.arange(128)
    ident = np.eye(128, dtype=np.float32)
    triu = (r[:, None] <= r[None, :]).astype(np.float32)
    trius = (r[:, None] < r[None, :]).astype(np.float32)
    ones = np.ones((128, 128), np.float32)
    iota = np.broadcast_to(r[None, :].astype(np.float32), (128, 128)).copy()
    tril_s = (r[:, None] > r[None, :]).astype(np.float32)
    return np.concatenate([ident, triu, trius, ones, iota, tril_s], axis=1)


def load_cst(p, cst_d, names, bf=()):
    out = {}
    for nm in names:
        i = CST_NAMES.index(nm)
        t = p.sb("c_" + nm, [128, 128], F32)
        p.dma(t[:], cst_d[:, i * 128:(i + 1) * 128])
        out[nm] = t
    for nm in bf:
        tb = p.sb("cb_" + nm, [128, 128], BF16)
        p.copy(tb[:], out[nm][:])
        out[nm + "_bf"] = tb
    return out


CAP = 128
NE = 32
FF = 512


def stage_moe(p, cst_d, x1_d, rg_d, rgb_d, re_d, reb_d, wg_d, wu_d, wd_d, ffn_d, tag):
    xbf_d = p.dram("moe_xbf_" + tag, [T, D], BF16).ap()
    rt_d = p.dram("moe_rt_" + tag, [3, 128, NT * NE], F32).ap()
    p.begin()
    C = load_cst(p, cst_d, ["ident", "trius", "ones", "iota"], bf=["ident", "trius", "ones"])
    ident, iota = C["ident"], C["iota"]
    PA = p.ps("PA", [128, 2048])
    PG = p.ps("PG", [128, 512])
    PU = p.ps("PU", [128, 512])
    Xbf = p.sb("Xbf", [128, NT, D], BF16)
    asg = p.sb("asg", [128, NT, NE], F32)
    gts = p.sb("gts", [128, NT, NE], F32)
    pos = p.sb("pos", [128, NT, NE], F32)
    asgb = p.sb("asgb", [128, NT, NE], BF16)
    wr = p.sb("wr", [128, KC, 36], F32)
    p.dma(wr[:, :, 0:4], rg_d.rearrange("(kc k) n -> k kc n", k=128))
    p.dma(wr[:, :, 4:36], re_d.rearrange("(kc k) n -> k kc n", k=128))
    rb = p.sb("rb", [128, 36], F32)
    p.dma(rb[:, 0:4], bcast_rows(rgb_d, 4))
    p.dma(rb[:, 4:36], bcast_rows(reb_d, 32))
    xt = [p.sb("xt0", [128, D], F32), p.sb("xt1", [128, D], F32)]
    xTf = [p.sb("xTf0", [128, KC, 128], F32), p.sb("xTf1", [128, KC, 128], F32)]
    sm = {k: p.sb("sm_" + k, [128, n], F32) for k, n in
          [("lg", 36), ("gmax", 1), ("oh", 4), ("ge", 4), ("gsum", 1), ("tmp", 32), ("es", 8), ("v1", 1), ("m1", 8),
           ("e2", 8), ("v2", 1), ("m2", 8), ("d", 1), ("p1", 1), ("p2", 1), ("mm", 8), ("gm", 8)]}
    for tt in range(NT):
        x, xf = xt[tt % 2], xTf[tt % 2]
        p.dma(x[:], x1_d[tt * 128:(tt + 1) * 128, :])
        p.copy(Xbf[:, tt, :], x[:], eng="gpsimd")
        for g in range(0, KC, 4):
            for j in range(4):
                p.tr(PA[:, j * 128:(j + 1) * 128], x[:, (g + j) * 128:(g + j + 1) * 128], ident[:])
            p.copy(xf[:, g:g + 4, :], PA[:, 0:512].rearrange("p (a b) -> p a b", a=4), eng="scalar" if (g // 4) % 2 else "vector")
        for kc in range(KC):
            p.mm(PG[:, 0:36], xf[:, kc, :], wr[:, kc, :], start=(kc == 0), stop=(kc == KC - 1))
        lg = sm["lg"]
        p.tt(lg[:], PG[:, 0:36], rb[:], ALU.add)
        p.reduce(sm["gmax"][:], lg[:, 0:4], ALU.max)
        p.ts(sm["oh"][:], lg[:, 0:4], sm["gmax"][:, 0:1], None, op0=ALU.is_equal)
        p.ts(sm["ge"][:], lg[:, 0:4], sm["gmax"][:, 0:1], None, op0=ALU.subtract)
        p.act(sm["ge"][:], sm["ge"][:], AF.Exp, accum_out=sm["gsum"][:])
        p.recip(sm["gsum"][:], sm["gsum"][:])
        p.tt(sm["tmp"][:].rearrange("p (g i) -> p g i", g=4), lg[:, 4:36].rearrange("p (g i) -> p g i", g=4),
             sm["oh"][:, :, None].to_broadcast([128, 4, 8]), ALU.mult)
        p.reduce(sm["es"][:], sm["tmp"][:].rearrange("p (g i) -> p i g", g=4), ALU.add)
        p.reduce(sm["v1"][:], sm["es"][:], ALU.max)
        p.ts(sm["m1"][:], sm["es"][:], sm["v1"][:, 0:1], None, op0=ALU.is_equal)
        p.stt(sm["e2"][:], sm["m1"][:], -1e30, sm["es"][:], ALU.mult, ALU.add)
        p.reduce(sm["v2"][:], sm["e2"][:], ALU.max)
        p.ts(sm["m2"][:], sm["e2"][:], sm["v2"][:, 0:1], None, op0=ALU.is_equal)
        p.tt(sm["d"][:], sm["v2"][:], sm["v1"][:], ALU.subtract)
        p.act(sm["d"][:], sm["d"][:], AF.Exp)
        p.ts(sm["p1"][:], sm["d"][:], 1.0, None, op0=ALU.add)
        p.recip(sm["p1"][:], sm["p1"][:])
        p.tt(sm["p2"][:], sm["d"][:], sm["p1"][:], ALU.mult)
        p.tt(sm["p1"][:], sm["p1"][:], sm["gsum"][:], ALU.mult)
        p.tt(sm["p2"][:], sm["p2"][:], sm["gsum"][:], ALU.mult)
        p.tt(sm["mm"][:], sm["m1"][:], sm["m2"][:], ALU.add)
        p.ts(sm["gm"][:], sm["m1"][:], sm["p1"][:, 0:1], None, op0=ALU.mult)
        p.stt(sm["gm"][:], sm["m2"][:], sm["p2"][:, 0:1], sm["gm"][:], ALU.mult, ALU.add)
        p.tt(asg[:, tt, :].rearrange("p (g i) -> p g i", g=4), sm["oh"][:, :, None].to_broadcast([128, 4, 8]),
             sm["mm"][:, None, :].to_broadcast([128, 4, 8]), ALU.mult)
        p.tt(gts[:, tt, :].rearrange("p (g i) -> p g i", g=4), sm["oh"][:, :, None].to_broadcast([128, 4, 8]),
             sm["gm"][:, None, :].to_broadcast([128, 4, 8]), ALU.mult)
        p.copy(asgb[:, tt, :], asg[:, tt, :])
        for t2 in range(tt + 1):
            lhs = C["trius_bf"] if t2 == tt else C["ones_bf"]
            p.mm(PU[:, 0:32], lhs[:], asgb[:, t2, :], start=(t2 == 0), stop=(t2 == tt))
        p.copy(pos[:, tt, :], PU[:, 0:32])
        p.dma(xbf_d[tt * 128:(tt + 1) * 128, :], Xbf[:, tt, :], q="gpsimd")
    p.dma(rt_d[0], asg[:].rearrange("p a b -> p (a b)"))
    p.dma(rt_d[1], gts[:].rearrange("p a b -> p (a b)"))
    p.dma(rt_d[2], pos[:].rearrange("p a b -> p (a b)"))
    p.end()
    p.begin()
    C = load_cst(p, cst_d, ["ident", "iota"], bf=["ident"])
    iota = C["iota"]
    PA = p.ps("PA", [128, 2048])
    PG = p.ps("PG", [128, 512])
    PU = p.ps("PU", [128, 512])
    PT = p.ps("PT", [128, 1024], BF16)
    PC = p.ps("PC", [128, 512])
    Xbf = p.sb("Xbf", [128, NT, D], BF16)
    yacc = p.sb("yacc", [128, NT, D], F32)
    asg = p.sb("asg", [128, NT, NE], F32)
    gts = p.sb("gts", [128, NT, NE], F32)
    pos = p.sb("pos", [128, NT, NE], F32)
    p.dma(Xbf[:], xbf_d.rearrange("(a p) d -> p a d", p=128))
    p.dma(asg[:].rearrange("p a b -> p (a b)"), rt_d[0])
    p.dma(gts[:].rearrange("p a b -> p (a b)"), rt_d[1])
    p.dma(pos[:].rearrange("p a b -> p (a b)"), rt_d[2])
    wst = [p.sb("wst%d" % i, [128, 4, 512], F32) for i in range(2)]
    Wg = p.sb("Wg", [128, KC, FF], BF16)
    Wu = p.sb("Wu", [128, KC, FF], BF16)
    Wd = p.sb("Wd", [128, 4, D], BF16)
    Se = [p.sb("Se%d" % i, [128, NT, CAP], BF16) for i in range(2)]
    GeT = p.sb("GeT", [128, 4, NT, 128], BF16)
    Ge = [p.sb("Ge%d" % i, [128, NT, CAP], BF16) for i in range(2)]
    XeT = p.sb("XeT", [128, KC, CAP], BF16)
    sg = p.sb("sg", [128, FF], F32)
    hidT = p.sb("hidT", [128, 4, CAP], BF16)
    Y = p.sb("Y", [128, 4, D], BF16)
    for e in range(NE):
        slot = e % 4
        S, G = Se[e % 2], Ge[e % 2]
        posb = pos[:, :, e:e + 1].to_broadcast([128, NT, CAP])
        p.tt(S[:], iota[:, None, :].to_broadcast([128, NT, CAP]), posb, ALU.is_equal)
        p.tt(G[:], S[:], gts[:, :, e:e + 1].to_broadcast([128, NT, CAP]), ALU.mult)
        p.tt(S[:], S[:], asg[:, :, e:e + 1].to_broadcast([128, NT, CAP]), ALU.mult)
        load_w_bf16(p, Wg, wg_d[e].rearrange("(kc k) n -> k kc n", k=128), wst, 512, engs=("scalar", "gpsimd", "vector"), qs=("sync",))
        load_w_bf16(p, Wu, wu_d[e].rearrange("(kc k) n -> k kc n", k=128), wst, 512, engs=("scalar", "gpsimd", "vector"), qs=("sync",))
        wdv = wd_d[e].rearrange("(kc k) n -> k kc n", k=128)
        for dc in range(4):
            load_w_bf16(p, Wd[:, :, dc * 512:(dc + 1) * 512], wdv[:, :, dc * 512:(dc + 1) * 512], wst, 512,
                        engs=("scalar", "gpsimd", "vector"), qs=("sync",))
        for fc in range(KC):
            for tt in range(NT):
                p.mm(PA[:, fc * 128:(fc + 1) * 128], Xbf[:, tt, fc * 128:(fc + 1) * 128], S[:, tt, :],
                     start=(tt == 0), stop=(tt == NT - 1))
        p.copy(XeT[:], PA[:].rearrange("p (a b) -> p a b", a=KC), eng="scalar")
        for fch in range(4):
            for kc in range(KC):
                p.mm(PG[:, fch * 128:(fch + 1) * 128], Wg[:, kc, fch * 128:(fch + 1) * 128], XeT[:, kc, :],
                     start=(kc == 0), stop=(kc == KC - 1))
        for fch in range(4):
            for kc in range(KC):
                p.mm(PU[:, fch * 128:(fch + 1) * 128], Wu[:, kc, fch * 128:(fch + 1) * 128], XeT[:, kc, :],
                     start=(kc == 0), stop=(kc == KC - 1))
        p.act(sg[:], PG[:], AF.Silu)
        p.tt(hidT[:].rearrange("p a b -> p (a b)"), sg[:], PU[:], ALU.mult)
        for dc in range(4):
            for fch in range(4):
                p.mm(PA[:, dc * 512:(dc + 1) * 512], hidT[:, fch, :], Wd[:, fch, dc * 512:(dc + 1) * 512],
                     start=(fch == 0), stop=(fch == 3))
        p.copy(Y[:, slot, :], PA[:], eng="scalar")
        for tt in range(NT):
            p.tr(PT[:, tt * 128:(tt + 1) * 128], G[:, tt, :], C["ident_bf"][:])
        p.copy(GeT[:, slot, :, :], PT[:].rearrange("p (a b) -> p a b", a=NT))
        if slot == 3:
            grp = e // 4
            for tt in range(NT):
                for dc in range(4):
                    for s4 in range(4):
                        p.mm(PC[:], GeT[:, s4, tt, :], Y[:, s4, dc * 512:(dc + 1) * 512], start=(s4 == 0), stop=(s4 == 3))
                    dst = yacc[:, tt, dc * 512:(dc + 1) * 512]
                    if grp == 0:
                        p.copy(dst, PC[:])
                    else:
                        p.tt(dst, PC[:], dst, ALU.add)
    for tt in range(NT):
        p.dma(ffn_d[tt * 128:(tt + 1) * 128, :], yacc[:, tt, :])
    p.end()


TWO_PI = 2.0 * np.pi
CW1 = 6.28125
CW2 = TWO_PI - CW1


def make_inv():
    inv = (10000.0 ** (-np.arange(0, 64, 2, dtype=np.float32) / 64)).astype(np.float32)
    return np.broadcast_to(inv[None, :], (128, 32)).copy()


def rope_tables(p, pos_d, inv_d, ntiles):
    n = ntiles * 32
    posi = p.sb("posi", [128, ntiles], I32)
    posf = p.sb("posf", [128, ntiles], F32)
    inv = p.sb("inv", [128, 32], F32)
    ang = p.sb("ang", [128, ntiles, 32], F32)
    ki = p.sb("ki", [128, ntiles, 32], I32)
    kf = p.sb("kf", [128, ntiles, 32], F32)
    r = p.sb("r", [128, ntiles, 32], F32)
    m = p.sb("rm", [128, ntiles, 32], F32)
    cos = p.sb("cos", [128, ntiles, 32], F32)
    sin = p.sb("sin", [128, ntiles, 32], F32)
    p.dma(posi[:], pos_d)
    p.dma(inv[:], inv_d)
    p.copy(posf[:], posi[:], eng="scalar")
    p.tt(ang[:], inv[:, None, :].to_broadcast([128, ntiles, 32]), posf[:, :, None].to_broadcast([128, ntiles, 32]), ALU.mult)
    p.ts(kf[:], ang[:], 1.0 / TWO_PI, None, op0=ALU.mult)
    p.copy(ki[:], kf[:])
    p.copy(kf[:], ki[:])
    p.stt(r[:], kf[:], -CW1, ang[:], ALU.mult, ALU.add)
    p.stt(r[:], kf[:], -CW2, r[:], ALU.mult, ALU.add)

    def wrap(t):
        p.ts(m[:], t[:], np.pi, None, op0=ALU.is_gt)
        p.stt(t[:], m[:], -TWO_PI, t[:], ALU.mult, ALU.add)
        p.ts(m[:], t[:], -np.pi, None, op0=ALU.is_lt)
        p.stt(t[:], m[:], TWO_PI, t[:], ALU.mult, ALU.add)
    wrap(r)
    p.act(sin[:], r[:], AF.Sin)
    p.ts(r[:], r[:], np.pi / 2, None, op0=ALU.add)
    wrap(r)
    p.act(cos[:], r[:], AF.Sin)
    return cos, sin


def rope_tm(p, dst, src, cos, sin, H, ta, tb):
    cb = cos[:, None, :].to_broadcast([128, H, 32])
    sb_ = sin[:, None, :].to_broadcast([128, H, 32])
    t1, t2 = src[:, :, 0:32], src[:, :, 32:64]
    p.tt(ta, t1, cb, ALU.mult)
    p.tt(tb, t2, sb_, ALU.mult)
    p.tt(dst[:, :, 0:32], ta, tb, ALU.subtract)
    p.tt(ta, t2, cb, ALU.mult)
    p.tt(tb, t1, sb_, ALU.mult)
    p.tt(dst[:, :, 32:64], ta, tb, ALU.add)


TT_ = T + 128
NTT = NT + 1
E_IN = 6432


def ea_scratch(p, tag):
    S = {}
    S["dt"] = p.dram("ea_dt_" + tag, [128, NT * 32], F32).ap()
    S["qT"] = p.dram("ea_qT_" + tag, [8, 128, T], BF16).ap()
    S["kT"] = p.dram("ea_kT_" + tag, [2, 128, TT_], BF16).ap()
    S["v"] = p.dram("ea_v_" + tag, [TT_, 128], BF16).ap()
    S["xs"] = p.dram("ea_xs_" + tag, [T, 2048], BF16).ap()
    S["BT"] = p.dram("ea_BT_" + tag, [4, 128, T], BF16).ap()
    S["Btm"] = p.dram("ea_Btm_" + tag, [T, 512], BF16).ap()
    return S


def stage_ea1(p, cst_d, inv_d, pos_d, xh_d, w_in_d, conv_w_d, conv_b_d, z_d, ct_d, S):
    import os
    UPTO = int(os.environ.get("EA1_UPTO", "9"))
    p.begin()
    C = load_cst(p, cst_d, ["ident"])
    ident = C["ident"]
    cos, sin = rope_tables(p, pos_d, inv_d, NTT)
    xT = p.sb("xT", [128, KC, TT_], BF16)
    PS = [p.ps("P%d" % i, [128, 512]) for i in range(4)]
    PB = [p.ps("PB%d" % i, [128, 1024]) for i in range(2)]
    xt = [p.sb("xt0", [128, D], F32), p.sb("xt1", [128, D], F32)]
    for tt in range(NTT):
        x = xt[tt % 2]
        p.dma(x[:], xh_d[tt * 128:(tt + 1) * 128, :], q="sync" if tt % 2 else "gpsimd")
        transpose_to(p, xT, x, ident, KC, tt * 128, PS[0:2])
    if UPTO <= 1:
        p.end()
        return
    wst = [p.sb("wst%d" % i, [128, 4, 512], F32) for i in range(2)]
    wbf = [p.sb("wbf%d" % i, [128, KC, 512], BF16) for i in range(2)]
    ob = [p.sb("ob%d" % i, [128, 512], F32) for i in range(2)]

    def wview(c0, n):
        return w_in_d[:, c0:c0 + n].rearrange("(kc k) n -> k kc n", k=128)
    cnt = 0
    for n in range(4):
        wb = wbf[n % 2]
        load_w_bf16(p, wb, wview(n * 512, 512), wst, 512)
        for tt in range(1, NTT):
            ps, o = PS[cnt % 4], ob[cnt % 2]
            for kc in range(KC):
                p.mm(ps[:], xT[:, kc, tt * 128:(tt + 1) * 128], wb[:, kc, :], start=(kc == 0), stop=(kc == KC - 1))
            p.copy(o[:], ps[:], eng="scalar" if cnt % 2 else "vector")
            p.dma(z_d[(tt - 1) * 128:tt * 128, n * 512:(n + 1) * 512], o[:], q="scalar")
            cnt += 1
    if UPTO <= 2:
        p.end()
        return
    wb = wbf[0]
    load_w_bf16(p, wb[:, :, 0:32], wview(5120, 32), wst, 32)
    dtraw = p.sb("dtraw", [128, NT, 32], F32)
    for tt in range(1, NTT):
        ps = PS[cnt % 4]
        for kc in range(KC):
            p.mm(ps[:, 0:32], xT[:, kc, tt * 128:(tt + 1) * 128], wb[:, kc, 0:32], start=(kc == 0), stop=(kc == KC - 1))
        p.copy(dtraw[:, tt - 1, :], ps[:, 0:32])
        cnt += 1
    p.dma(S["dt"], dtraw[:].rearrange("p a b -> p (a b)"))
    if UPTO <= 3:
        p.end()
        return
    qr = [p.sb("qr%d" % i, [128, 8, 64], F32) for i in range(2)]
    ta = p.sb("ta", [128, 8, 32], F32)
    tb = p.sb("tb", [128, 8, 32], F32)
    qTb = [p.sb("qTb%d" % i, [128, 4, 128], BF16) for i in range(2)]
    for hc in range(2):
        wb = wbf[(hc + 1) % 2]
        load_w_bf16(p, wb, wview(5152 + hc * 512, 512), wst, 512)
        for tt in range(1, NTT):
            ps, q_, qt = PS[cnt % 4], qr[cnt % 2], qTb[cnt % 2]
            for kc in range(KC):
                p.mm(ps[:], xT[:, kc, tt * 128:(tt + 1) * 128], wb[:, kc, :], start=(kc == 0), stop=(kc == KC - 1))
            rope_tm(p, q_[:], ps[:].rearrange("p (h d) -> p h d", h=8), cos[:, tt, :], sin[:, tt, :], 8, ta[:], tb[:])
            pt = PS[(cnt + 2) % 4]
            qf = q_[:].rearrange("p h d -> p (h d)")
            for j in range(4):
                p.tr(pt[:, j * 128:(j + 1) * 128], qf[:, j * 128:(j + 1) * 128], ident[:])
            p.copy(qt[:], pt[:].rearrange("p (a b) -> p a b", a=4), eng="scalar")
            p.dma(S["qT"][hc * 4:(hc + 1) * 4, :, (tt - 1) * 128:tt * 128].rearrange("a p t -> p a t"), qt[:], q="scalar")
            cnt += 1
    if UPTO <= 4:
        p.end()
        return
    wb = wbf[1]
    load_w_bf16(p, wb[:, :, 0:256], wview(6176, 256), wst, 256)
    kr = p.sb("kr", [128, 2, 64], F32)
    ksb = p.sb("ksb", [128, 128], F32)
    kd = [p.sb("kd%d" % i, [128, 2, 2, 64], F32) for i in range(2)]
    kTb = [p.sb("kTb%d" % i, [128, 2, 128], BF16) for i in range(2)]
    vb = [p.sb("vb%d" % i, [128, 128], BF16) for i in range(2)]
    KVL = int(os.environ.get("EA1_KV", "9"))
    for tt in range(NTT):
        ps, kd_, kt_, v_ = PS[cnt % 4], kd[cnt % 2], kTb[cnt % 2], vb[cnt % 2]
        for kc in range(KC):
            p.mm(ps[:, 0:256], xT[:, kc, tt * 128:(tt + 1) * 128], wb[:, kc, 0:256], start=(kc == 0), stop=(kc == KC - 1))
        if KVL >= 2:
            p.copy(ksb[:], ps[:, 0:128], eng="scalar")
            rope_tm(p, kr[:], ksb[:].rearrange("p (h d) -> p h d", h=2), cos[:, tt, :], sin[:, tt, :], 2, ta[:, 0:2, :], tb[:, 0:2, :])
        if KVL >= 3:
            p.copy(kd_[:, :, 0, :], kr[:])
            p.copy(kd_[:, :, 1, :], kr[:])
        p.copy(v_[:], ps[:, 128:256], eng="scalar")
        if KVL >= 4:
            pt = PS[(cnt + 2) % 4]
            kf = kd_[:].rearrange("p a b d -> p (a b d)")
            for j in range(2):
                p.tr(pt[:, j * 128:(j + 1) * 128], kf[:, j * 128:(j + 1) * 128], ident[:])
            p.copy(kt_[:], pt[:, 0:256].rearrange("p (a b) -> p a b", a=2), eng="scalar")
        if KVL >= 5:
            p.dma(S["kT"][:, :, tt * 128:(tt + 1) * 128].rearrange("a p t -> p a t"), kt_[:], q="scalar")
        if KVL >= 1:
            p.dma(S["v"][tt * 128:(tt + 1) * 128, :], v_[:], q="scalar")
        cnt += 1
    if UPTO <= 5:
        p.end()
        return
    cw5 = p.sb("cw5", [5, 3072], F32)
    p.dma(cw5[0:4, :], conv_w_d)
    p.dma(cw5[4:5, :], conv_b_d.rearrange("(o c) -> o c", o=1))
    cwt = p.sb("cwt", [128, 24, 5], F32)
    for ch in range(24):
        p.tr(PS[0][:, ch * 5:(ch + 1) * 5], cw5[:, ch * 128:(ch + 1) * 128], ident[0:5, 0:5])
    p.copy(cwt[:], PS[0][:, 0:120].rearrange("p (a b) -> p a b", a=24))
    cw = cwt
    cb = cwt[:, :, 4]
    if UPTO <= 6:
        p.end()
        return
    wc = [p.sb("wc%d" % i, [128, KC, 128], BF16) for i in range(2)]
    u = [p.sb("u%d" % i, [128, TT_], F32) for i in range(2)]
    acc = [p.sb("acc%d" % i, [128, T], F32) for i in range(2)]
    xcb = [p.sb("xcb%d" % i, [128, T], BF16) for i in range(2)]
    xsb = [p.sb("xsb%d" % i, [128, NT, 128], BF16) for i in range(2)]
    for ch in range(24):
        w_, u_, a_, xb_, xs_ = wc[ch % 2], u[ch % 2], acc[ch % 2], xcb[ch % 2], xsb[ch % 2]
        load_w_bf16(p, w_, wview(2048 + ch * 128, 128), wst, 128)
        for gi, (t0, tn) in enumerate([(0, 512), (512, 512), (1024, 128)]):
            ps = PS[cnt % 4]
            for kc in range(KC):
                p.mm(ps[:, 0:tn], w_[:, kc, :], xT[:, kc, t0:t0 + tn], start=(kc == 0), stop=(kc == KC - 1))
            p.copy(u_[:, t0:t0 + tn], ps[:, 0:tn], eng="scalar" if cnt % 2 else "vector")
            cnt += 1
        p.ts(a_[:], u_[:, 128:TT_], cw[:, ch, 3:4], cw[:, ch, 4:5], op0=ALU.mult, op1=ALU.add)
        for k in range(3):
            sh = 3 - k
            p.stt(a_[:], u_[:, 128 - sh:TT_ - sh], cw[:, ch, k:k + 1], a_[:], ALU.mult, ALU.add)
        if ch < 16 or 16 <= ch < 20:
            p.act(a_[:], a_[:], AF.Silu)
            pb = PB[ch % 2]
            for j in range(NT):
                p.tr(pb[:, j * 128:(j + 1) * 128], a_[:, j * 128:(j + 1) * 128], ident[:])
            p.copy(xs_[:], pb[:].rearrange("p (a b) -> p a b", a=NT), eng="vector")
            if ch < 16:
                p.dma(S["xs"][:, ch * 128:(ch + 1) * 128].rearrange("(a p) c -> p a c", p=128), xs_[:], q="scalar")
            else:
                g = ch - 16
                p.dma(S["Btm"][:, g * 128:(g + 1) * 128].rearrange("(a p) c -> p a c", p=128), xs_[:], q="scalar")
                p.copy(xb_[:], a_[:], eng="gpsimd")
                p.dma(S["BT"][g], xb_[:], q="scalar")
        else:
            g = ch - 20
            p.act(xb_[:], a_[:], AF.Silu)
            p.dma(ct_d[g], xb_[:], q="scalar")
    p.end()


def stage_ea2(p, cst_d, dt_bias_d, a_log_d, d_skip_d, ct_d, S, y_d, e_d, hloc_d, dtot_d):
    p.begin()
    C = load_cst(p, cst_d, ["triu", "tril_s", "ones"])
    U, Lm, ones = C["triu"], C["tril_s"], C["ones"]
    Psm = p.ps("Psm", [128, 512])
    PX = p.ps("PX", [128, 512])
    PA = p.ps("PA", [128, 2048])
    xs = p.sb("xs", [128, NT, 2048], BF16)
    BT = p.sb("BT", [128, 4, T], BF16)
    CT = p.sb("CT", [128, 4, T], BF16)
    Btm = p.sb("Btm", [128, NT, 512], BF16)
    dtraw = p.sb("dtraw", [128, NT, 32], F32)
    p.dma(xs[:], S["xs"].rearrange("(a p) c -> p a c", p=128))
    p.dma(BT[:], S["BT"].rearrange("g n t -> n g t"), q="gpsimd")
    p.dma(CT[:], ct_d.rearrange("g n t -> n g t"), q="gpsimd")
    p.dma(Btm[:], S["Btm"].rearrange("(a p) c -> p a c", p=128))
    p.dma(dtraw[:].rearrange("p a b -> p (a b)"), S["dt"])
    rows = {}
    for nm, d_ in (("dtb", dt_bias_d), ("alog", a_log_d), ("dsk", d_skip_d)):
        rows[nm] = p.sb("row_" + nm, [128, 32], F32)
        p.dma(rows[nm][:], bcast_rows(d_, 32))
    A = p.sb("A", [128, 32], F32)
    p.act(A[:], rows["alog"][:], AF.Exp)
    p.ts(A[:], A[:], -1.0, None, op0=ALU.mult)
    sm = {k: p.sb("s_" + k, [128, 32], F32) for k in ["x", "ab", "e", "l", "dt", "a", "acs", "tot", "eloc", "acc", "dte", "cdec", "carry"]}
    Eall = p.sb("Eall", [128, NT, 32], F32)
    p.memset(sm["carry"][:], 0.0)
    H = p.sb("H", [128, 32, 64], F32)
    Hbf = p.sb("Hbf", [128, 2048], BF16)
    p.memset(H[:], 0.0)
    p.memset(Hbf[:], 0.0)
    xdt = p.sb("xdt", [128, 32, 64], BF16)
    xd2 = p.sb("xd2", [128, 32, 64], BF16)
    cbm = p.sb("cbm", [128, 4, 128], F32)
    rhsA = p.sb("rhsA", [128, 16, 128], F32)
    dec = p.sb("dec", [128, 16, 128], F32)
    G = p.sb("G", [128, 16, 128], BF16)
    ybuf = p.sb("ybuf", [128, 32, 64], F32)
    ytmp = p.sb("ytmp", [128, 16, 64], F32)
    Ht = p.sb("Ht", [128, 32, 64], F32)
    for c in range(NT):
        cs = slice(c * 128, (c + 1) * 128)
        x = sm["x"]
        p.tt(x[:], dtraw[:, c, :], rows["dtb"][:], ALU.add)
        p.stt(sm["ab"][:], x[:], -1.0, x[:], ALU.mult, ALU.max)
        p.act(sm["e"][:], sm["ab"][:], AF.Exp, scale=-1.0)
        p.act(sm["l"][:], sm["e"][:], AF.Ln, bias=1.0)
        p.stt(sm["dt"][:], x[:], 0.0, sm["l"][:], ALU.max, ALU.add)
        p.tt(sm["a"][:], sm["dt"][:], A[:], ALU.mult)
        p.mm(Psm[:, 0:32], U[:], sm["a"][:])
        p.mm(Psm[:, 32:64], ones[:], sm["a"][:])
        p.copy(sm["acs"][:], Psm[:, 0:32])
        p.copy(sm["tot"][:], Psm[:, 32:64])
        p.act(sm["eloc"][:], sm["acs"][:], AF.Exp)
        p.tt(sm["acc"][:], sm["acs"][:], sm["carry"][:], ALU.add)
        p.act(Eall[:, c, :], sm["acc"][:], AF.Exp)
        p.tt(sm["carry"][:], sm["carry"][:], sm["tot"][:], ALU.add)
        p.tt(sm["dte"][:], sm["tot"][:], sm["acs"][:], ALU.subtract)
        p.act(sm["dte"][:], sm["dte"][:], AF.Exp)
        p.act(sm["cdec"][:], sm["tot"][:], AF.Exp)
        p.tt(xdt[:], xs[:, c, :].rearrange("p (h d) -> p h d", h=32), sm["dt"][:, :, None].to_broadcast([128, 32, 64]), ALU.mult)
        p.tt(xd2[:], xdt[:], sm["dte"][:, :, None].to_broadcast([128, 32, 64]), ALU.mult)
        for g in range(4):
            p.mm(PX[:, g * 128:(g + 1) * 128], BT[:, g, cs], CT[:, g, cs])
        p.tt(cbm[:], PX[:].rearrange("p (g l) -> p g l", g=4), U[:, None, :].to_broadcast([128, 4, 128]), ALU.mult)
        for half in range(2):
            hs = slice(half * 16, (half + 1) * 16)
            p.tt(rhsA[:], U[:, None, :].to_broadcast([128, 16, 128]), sm["a"][:, hs, None].to_broadcast([128, 16, 128]), ALU.mult)
            rf = rhsA[:].rearrange("p h l -> p (h l)")
            for j in range(4):
                p.mm(PA[:, j * 512:(j + 1) * 512], Lm[:], rf[:, j * 512:(j + 1) * 512])
            p.act(dec[:].rearrange("p h l -> p (h l)"), PA[:], AF.Exp)
            p.tt(G[:].rearrange("p (g r) l -> p g r l", g=2), dec[:].rearrange("p (g r) l -> p g r l", g=2),
                 cbm[:, half * 2:half * 2 + 2, None, :].to_broadcast([128, 2, 8, 128]), ALU.mult)
            for hh in range(16):
                p.mm(PA[:, hh * 64:(hh + 1) * 64], G[:, hh, :], xdt[:, half * 16 + hh, :])
            for g in range(2):
                gg = half * 2 + g
                p.mm(PA[:, 1024 + g * 512:1024 + (g + 1) * 512], CT[:, gg, cs], Hbf[:, gg * 512:(gg + 1) * 512])
            yh = ybuf[:, hs, :]
            p.tt(yh, PA[:, 1024:2048].rearrange("p (h d) -> p h d", h=16), sm["eloc"][:, hs, None].to_broadcast([128, 16, 64]), ALU.mult)
            p.tt(yh, yh, PA[:, 0:1024].rearrange("p (h d) -> p h d", h=16), ALU.add)
            p.tt(ytmp[:], xs[:, c, half * 1024:(half + 1) * 1024].rearrange("p (h d) -> p h d", h=16),
                 rows["dsk"][:, hs, None].to_broadcast([128, 16, 64]), ALU.mult)
            p.tt(yh, yh, ytmp[:], ALU.add)
        p.dma(y_d[cs, :], ybuf[:].rearrange("p h d -> p (h d)"))
        x2f = xd2[:].rearrange("p h d -> p (h d)")
        for g in range(4):
            p.mm(PA[:, g * 512:(g + 1) * 512], Btm[:, c, g * 128:(g + 1) * 128], x2f[:, g * 512:(g + 1) * 512])
        p.tt(Ht[:], H[:], sm["cdec"][:, :, None].to_broadcast([128, 32, 64]), ALU.mult)
        p.tt(H[:], Ht[:], PA[:].rearrange("p (h d) -> p h d", h=32), ALU.add)
        p.copy(Hbf[:], H[:].rearrange("p h d -> p (h d)"), eng="scalar")
    p.dma(hloc_d, H[:].rearrange("p h d -> p (h d)"))
    p.act(sm["cdec"][:], sm["carry"][:], AF.Exp)
    p.dma(dtot_d, sm["cdec"][:])
    p.dma(e_d.rearrange("(a p) h -> p a h", p=128), Eall[:])
    p.end()


def stage_ea3(p, cst_d, sinks_d, mask_d, S, yatt_d):
    p.begin()
    C = load_cst(p, cst_d, ["ident"], bf=["ident"])
    identb = C["ident_bf"]
    qT = p.sb("qT", [128, 8, T], BF16)
    kT = p.sb("kT", [128, 2, TT_], BF16)
    v = p.sb("v", [128, NTT, 128], BF16)
    p.dma(qT[:], S["qT"].rearrange("a p t -> p a t"))
    p.dma(kT[:], S["kT"].rearrange("a p t -> p a t"), q="gpsimd")
    p.dma(v[:], S["v"].rearrange("(a p) d -> p a d", p=128), q="gpsimd")
    msk = p.sb("msk", [128, 2, 256], F32)
    p.dma(msk[:], mask_d.rearrange("a p s -> p a s"))
    snk = p.sb("snk", [128, 16], F32)
    p.dma(snk[:], bcast_rows(sinks_d, 16))
    PL = [p.ps("PL%d" % i, [128, 2, 512]) for i in range(2)]
    PT = [p.ps("PTa%d" % i, [128, 1024], BF16) for i in range(2)]
    PO = p.ps("PO", [128, 1024])
    lg = [p.sb("lg%d" % i, [128, 2, 256], F32) for i in range(2)]
    P_ = [p.sb("Pp%d" % i, [128, 2, 256], BF16) for i in range(2)]
    PTs = [p.sb("PTs%d" % i, [128, 4, 128], BF16) for i in range(2)]
    sm = [{k: p.sb("a_%s%d" % (k, i), [128, 2], F32) for k in ["mx", "m", "nm", "rs", "sk"]} for i in range(2)]
    rden = p.sb("rden", [128, 16], F32)
    yo = [p.sb("yo%d" % i, [128, 16, 64], F32) for i in range(2)]
    scale = 64 ** -0.5
    it = 0
    for blk in range(NT):
        mk = msk[:, 0 if blk == 0 else 1, :]
        for pr in range(8):
            kh = pr // 4
            pl, l_, pp, pt, pts, s_ = PL[it % 2], lg[it % 2], P_[it % 2], PT[it % 2], PTs[it % 2], sm[it % 2]
            it += 1
            for hh in range(2):
                rs_ = slice(hh * 64, (hh + 1) * 64)
                p.mm(pl[:, hh, 0:256], qT[rs_, pr, blk * 128:(blk + 1) * 128], kT[rs_, kh, blk * 128:blk * 128 + 256])
            p.stt(l_[:], pl[:, :, 0:256], scale, mk[:, None, :].to_broadcast([128, 2, 256]), ALU.mult, ALU.add)
            p.reduce(s_["mx"][:], l_[:], ALU.max)
            p.tt(s_["m"][:], s_["mx"][:], snk[:, 2 * pr:2 * pr + 2], ALU.max)
            p.ts(s_["nm"][:], s_["m"][:], -1.0, None, op0=ALU.mult)
            for hh in range(2):
                p.act(pp[:, hh, :], l_[:, hh, :], AF.Exp, bias=s_["nm"][:, hh:hh + 1], accum_out=s_["rs"][:, hh:hh + 1])
            p.tt(s_["sk"][:], snk[:, 2 * pr:2 * pr + 2], s_["m"][:], ALU.subtract)
            p.act(s_["sk"][:], s_["sk"][:], AF.Exp)
            p.tt(s_["sk"][:], s_["sk"][:], s_["rs"][:], ALU.add)
            p.recip(rden[:, 2 * pr:2 * pr + 2], s_["sk"][:])
            for hh in range(2):
                for kt in range(2):
                    p.tr(pt[:, (hh * 2 + kt) * 128:(hh * 2 + kt + 1) * 128], pp[:, hh, kt * 128:(kt + 1) * 128], identb[:])
            p.copy(pts[:], pt[:, 0:512].rearrange("p (a b) -> p a b", a=4), eng="scalar")
            for hh in range(2):
                for kt in range(2):
                    p.mm(PO[:, (2 * pr + hh) * 64:(2 * pr + hh + 1) * 64], pts[:, hh * 2 + kt, :],
                         v[:, blk + kt, kh * 64:(kh + 1) * 64], start=(kt == 0), stop=(kt == 1))
        y_ = yo[blk % 2]
        p.tt(y_[:], PO[:].rearrange("p (h d) -> p h d", h=16), rden[:, :, None].to_broadcast([128, 16, 64]), ALU.mult)
        p.dma(yatt_d[blk * 128:(blk + 1) * 128, :], y_[:].rearrange("p h d -> p (h d)"))
    p.end()


def stage_eb1(p, cst_d, y_d, z_d, yatt_d, ct_d, e_d, hprev_d, dprev_d, ssm_norm_d, mixT_d):
    p.begin()
    C = load_cst(p, cst_d, ["ident"])
    ident = C["ident"]
    PA = p.ps("PA", [128, 2048])
    PB = [p.ps("PB%d" % i, [128, 1024]) for i in range(2)]
    H = p.sb("H", [128, 32, 64], F32)
    Ht = p.sb("Ht", [128, 32, 64], F32)
    Hbf = p.sb("Hbf", [128, 2048], BF16)
    Sj = [p.sb("Sj%d" % i, [128, 32, 64], F32) for i in range(2)]
    Dj = [p.sb("Dj%d" % i, [128, 32], F32) for i in range(2)]
    p.memset(H[:], 0.0)
    for j in range(7):
        s_, d_ = Sj[j % 2], Dj[j % 2]
        p.dma(s_[:].rearrange("p h d -> p (h d)"), hprev_d[j])
        p.dma(d_[:], dprev_d[j], q="gpsimd")
        p.tt(Ht[:], H[:], d_[:, :, None].to_broadcast([128, 32, 64]), ALU.mult)
        p.tt(H[:], Ht[:], s_[:], ALU.add)
    p.copy(Hbf[:], H[:].rearrange("p h d -> p (h d)"))
    CT = p.sb("CT", [128, 4, T], BF16)
    p.dma(CT[:], ct_d.rearrange("g n t -> n g t"), q="gpsimd")
    E = p.sb("E", [128, NT, 32], F32)
    p.dma(E[:], e_d.rearrange("(a p) h -> p a h", p=128))
    nrm = p.sb("nrm", [128, 2048], F32)
    p.dma(nrm[:], bcast_rows(ssm_norm_d, 2048))
    yl = [p.sb("yl%d" % i, [128, 2048], F32) for i in range(2)]
    zt = [p.sb("zt%d" % i, [128, 2048], F32) for i in range(2)]
    mix = [p.sb("mix%d" % i, [128, 3072], F32) for i in range(2)]
    junk = p.sb("junk", [128, 512], F32)
    ss = [p.sb("ss%d" % i, [128, 4], F32) for i in range(2)]
    mT = [p.sb("mT%d" % i, [128, 24, 128], BF16) for i in range(2)]
    for c in range(NT):
        cs = slice(c * 128, (c + 1) * 128)
        y_, z_, m_, s_, t_ = yl[c % 2], zt[c % 2], mix[c % 2], ss[c % 2], mT[c % 2]
        p.dma(y_[:], y_d[cs, :])
        p.dma(z_[:], z_d[cs, :], q="gpsimd")
        p.dma(m_[:, 2048:3072], yatt_d[cs, :])
        for g in range(4):
            p.mm(PA[:, g * 512:(g + 1) * 512], CT[:, g, cs], Hbf[:, g * 512:(g + 1) * 512])
        yv = m_[:, 0:2048]
        p.tt(yv.rearrange("p (h d) -> p h d", h=32), PA[:].rearrange("p (h d) -> p h d", h=32),
             E[:, c, :, None].to_broadcast([128, 32, 64]), ALU.mult)
        p.tt(yv, yv, y_[:], ALU.add)
        p.act(z_[:], z_[:], AF.Silu)
        p.tt(yv, yv, z_[:], ALU.mult)
        for g in range(4):
            p.act(junk[:], m_[:, g * 512:(g + 1) * 512], AF.Square, accum_out=s_[:, g:g + 1])
        p.ts(s_[:], s_[:], 1.0 / 512, EPS, op0=ALU.mult, op1=ALU.add)
        p.act(s_[:], s_[:], AF.Sqrt)
        p.recip(s_[:], s_[:])
        p.tt(yv.rearrange("p (g d) -> p g d", g=4), yv.rearrange("p (g d) -> p g d", g=4),
             s_[:, :, None].to_broadcast([128, 4, 512]), ALU.mult)
        p.tt(yv, yv, nrm[:], ALU.mult, eng="gpsimd")
        for g3 in range(3):
            pb = PB[g3 % 2]
            for j in range(8):
                k = g3 * 8 + j
                p.tr(pb[:, j * 128:(j + 1) * 128], m_[:, k * 128:(k + 1) * 128], ident[:])
            p.copy(t_[:, g3 * 8:(g3 + 1) * 8, :], pb[:].rearrange("p (a b) -> p a b", a=8), eng="scalar" if g3 % 2 else "vector")
        p.dma(mixT_d[:, :, cs].rearrange("a p t -> p a t"), t_[:], q="scalar")
    p.end()


def stage_proj_ln(p, mixT_d, nk, w_d, x_d, ln_g, ln_b, out_d):
    p.begin()
    mT = p.sb("mT", [128, nk, T], BF16)
    p.dma(mT[:], mixT_d.rearrange("a p t -> p a t"))
    g_rows = p.sb("g_rows", [128, D], F32)
    b_rows = p.sb("b_rows", [128, D], F32)
    p.dma(g_rows[:], bcast_rows(ln_g, D), q="gpsimd")
    p.dma(b_rows[:], bcast_rows(ln_b, D), q="gpsimd")
    vb = p.sb("vb", [128, NT, D], F32)
    p.dma(vb[:], x_d.rearrange("(a p) d -> p a d", p=128))
    wst = [p.sb("wst%d" % i, [128, 4, 512], F32) for i in range(2)]
    wbf = [p.sb("wbf%d" % i, [128, nk, 512], BF16) for i in range(2)]
    PS = [p.ps("P%d" % i, [128, 512]) for i in range(4)]
    cnt = 0
    for n in range(4):
        wb = wbf[n % 2]
        load_w_bf16(p, wb, w_d[:, n * 512:(n + 1) * 512].rearrange("(kc k) n -> k kc n", k=128), wst, 512)
        for tt in range(NT):
            ps = PS[cnt % 4]
            cnt += 1
            for kc in range(nk):
                p.mm(ps[:], mT[:, kc, tt * 128:(tt + 1) * 128], wb[:, kc, :], start=(kc == 0), stop=(kc == nk - 1))
            dst = vb[:, tt, n * 512:(n + 1) * 512]
            p.stt(dst, dst, ALPHA, ps[:], ALU.mult, ALU.add)
    lns = ln_scratch(p)
    for tt in range(NT):
        layer_norm_tile(p, vb[:, tt, :], vb[:, tt, :], g_rows[:], b_rows[:], lns[tt % 2])
        p.dma(out_d[tt * 128:(tt + 1) * 128, :], vb[:, tt, :], q="scalar")
    p.end()


O_IN = 4752
NKEY = 8192
NST = NKEY // 128
NIT = 20
TOPK = 256


def stage_op(p, cst_d, inv_d, pos_d, x_d, w_in_d, kv_norm_d, w_uk_d, kiT_d, kcatT_d, ckv_d, qcatT_d, qiT_d, wi_d, mode):
    p.begin()
    C = load_cst(p, cst_d, ["ident"])
    ident = C["ident"]
    cos, sin = rope_tables(p, pos_d, inv_d, NT)
    BFM = mode == "bf"
    xT = p.sb("xT", [128, KC, T], BF16) if BFM else None
    xTf = None if BFM else p.sb("xTf", [128, KC, T], F32)
    PS = [p.ps("P%d" % i, [128, 512]) for i in range(6)]
    xt = [p.sb("xt0", [128, D], F32), p.sb("xt1", [128, D], F32)]
    for tt in range(NT):
        x = xt[tt % 2]
        p.dma(x[:], x_d[tt * 128:(tt + 1) * 128, :], q="sync" if tt % 2 else "gpsimd")
        for g in range(0, KC, 4):
            ps = PS[(g // 4) % 2]
            for j in range(4):
                p.tr(ps[:, j * 128:(j + 1) * 128], x[:, (g + j) * 128:(g + j + 1) * 128], ident[:])
            pv = ps[:].rearrange("p (a b) -> p a b", a=4)
            if BFM:
                p.copy(xT[:, g:g + 4, tt * 128:(tt + 1) * 128], pv, eng="scalar" if (g // 4) % 2 else "vector")
            else:
                p.copy(xTf[:, g:g + 4, tt * 128:(tt + 1) * 128], pv, eng="scalar" if (g // 4) % 2 else "vector")
    wf = None if BFM else p.sb("wf", [128, KC, 512], F32)
    wst = [p.sb("wst%d" % i, [128, 4, 512] if BFM else [128, 1], F32) for i in range(2)]
    wbf = [p.sb("wbf%d" % i, [128, KC, 512] if BFM else [128, 1], BF16) for i in range(2)]
    cnt = 0
    wc = [p.sb("wc%d" % i, [128, KC, 128] if BFM else [128, 1], BF16) for i in range(2)]
    wuk = [p.sb("wuk%d" % i, [128, 1, 512] if BFM else [128, 1], BF16) for i in range(2)]
    qn = [p.sb("qn%d" % i, [128, 512] if BFM else [128, 1], BF16) for i in range(2)]
    ql = [p.sb("ql%d" % i, [128, 4, 512] if BFM else [128, 1], BF16) for i in range(2)]
    for h in range(16 if BFM else 0):
        w_, uk_ = wc[h % 2], wuk[h % 2]
        load_w_bf16(p, w_, w_in_d[:, h * 192:h * 192 + 128].rearrange("(kc k) n -> k kc n", k=128), wst, 128)
        load_w_bf16(p, uk_, w_uk_d[h].rearrange("(a k) n -> k a n", a=1), wst, 512)
        for tg in range(2):
            ps, q_, l_ = PS[cnt % 6], qn[cnt % 2], ql[cnt % 2]
            cnt += 1
            for kc in range(KC):
                p.mm(ps[:], w_[:, kc, :], xT[:, kc, tg * 512:(tg + 1) * 512], start=(kc == 0), stop=(kc == KC - 1))
            p.copy(q_[:], ps[:], eng="scalar")
            for rc in range(4):
                ps2 = PS[cnt % 6]
                cnt += 1
                p.mm(ps2[:], uk_[:, 0, rc * 128:(rc + 1) * 128], q_[:])
                p.copy(l_[:, rc, :], ps2[:], eng="scalar" if rc % 2 else "vector")
            p.dma(qcatT_d[0:512, h, tg * 512:(tg + 1) * 512].rearrange("(rc r) t -> r rc t", r=128), l_[:], q="scalar")
    qr = [p.sb("qr%d" % i, [128, 8, 64], F32) for i in range(2)]
    ta = p.sb("ta", [128, 8, 32], F32)
    tb = p.sb("tb", [128, 8, 32], F32)
    qTb = [p.sb("qTb%d" % i, [128, 4, 128] if BFM else [128, 1], BF16) for i in range(2)]
    qTf = [p.sb("qTf%d" % i, [128, 4, 128] if not BFM else [128, 1], F32) for i in range(2)]
    for which in ([0] if BFM else [1]):
        for hc in range(2):
            wb = wbf[(which * 2 + hc) % 2]
            if which == 0:
                for hh in range(8):
                    hd = hc * 8 + hh
                    load_w_bf16(p, wb[:, :, hh * 64:(hh + 1) * 64],
                                w_in_d[:, hd * 192 + 128:hd * 192 + 192].rearrange("(kc k) n -> k kc n", k=128), wst, 64, g=4)
            else:
                p.dma(wf[:], w_in_d[:, 3648 + hc * 512:3648 + (hc + 1) * 512].rearrange("(kc k) n -> k kc n", k=128))
            for tt in range(NT):
                ps, q_, qt = PS[cnt % 6], qr[cnt % 2], (qTb if which == 0 else qTf)[cnt % 2]
                cnt += 1
                for kc in range(KC):
                    if which == 0:
                        p.mm(ps[:], xT[:, kc, tt * 128:(tt + 1) * 128], wb[:, kc, :], start=(kc == 0), stop=(kc == KC - 1))
                    else:
                        p.mm(ps[:], xTf[:, kc, tt * 128:(tt + 1) * 128], wf[:, kc, :], start=(kc == 0), stop=(kc == KC - 1))
                rope_tm(p, q_[:], ps[:].rearrange("p (h d) -> p h d", h=8), cos[:, tt, :], sin[:, tt, :], 8, ta[:], tb[:])
                pt = PS[cnt % 6]
                cnt += 1
                qf = q_[:].rearrange("p h d -> p (h d)")
                for j in range(4):
                    p.tr(pt[:, j * 128:(j + 1) * 128], qf[:, j * 128:(j + 1) * 128], ident[:])
                p.copy(qt[:], pt[:].rearrange("p (a b) -> p a b", a=4), eng="scalar")
                ts_ = slice(tt * 128, (tt + 1) * 128)
                for hh2 in range(2):
                    src = qt[hh2 * 64:(hh2 + 1) * 64, :, :]
                    if which == 0:
                        dst = qcatT_d[512:576, hc * 8 + hh2:hc * 8 + 8:2, ts_]
                    else:
                        dst = qiT_d[:, hc * 8 + hh2:hc * 8 + 8:2, ts_]
                    p.dma(dst, src, q="scalar")
    wb = wbf[0]
    if BFM:
        load_w_bf16(p, wb, w_in_d[:, 3072:3584].rearrange("(kc k) n -> k kc n", k=128), wst, 512)
    nrm = p.sb("nrm", [128, 512], F32)
    if BFM:
        p.dma(nrm[:], bcast_rows(kv_norm_d, 512))
    cf = [p.sb("cf%d" % i, [128, 512] if BFM else [128, 1], F32) for i in range(2)]
    cbf = [p.sb("cbf%d" % i, [128, 512] if BFM else [128, 1], BF16) for i in range(2)]
    cT = [p.sb("cT%d" % i, [128, 4, 128] if BFM else [128, 1], BF16) for i in range(2)]
    ss = [p.sb("ss%d" % i, [128, 1], F32) for i in range(2)]
    junk = p.sb("junk", [128, 512], F32)
    for tt in range(NT if BFM else 0):
        ps, c_, cb_, ct_, s_ = PS[cnt % 6], cf[tt % 2], cbf[tt % 2], cT[tt % 2], ss[tt % 2]
        cnt += 1
        ts_ = slice(tt * 128, (tt + 1) * 128)
        for kc in range(KC):
            p.mm(ps[:], xT[:, kc, ts_], wb[:, kc, :], start=(kc == 0), stop=(kc == KC - 1))
        p.act(junk[:], ps[:], AF.Square, accum_out=s_[:])
        p.ts(s_[:], s_[:], 1.0 / 512, EPS, op0=ALU.mult, op1=ALU.add)
        p.act(s_[:], s_[:], AF.Sqrt)
        p.recip(s_[:], s_[:])
        p.stt(c_[:], ps[:], s_[:, 0:1], nrm[:], ALU.mult, ALU.mult)
        p.copy(cb_[:], c_[:], eng="scalar")
        p.dma(ckv_d[ts_, :], cb_[:], q="scalar")
        pt = PS[cnt % 6]
        cnt += 1
        for j in range(4):
            p.tr(pt[:, j * 128:(j + 1) * 128], c_[:, j * 128:(j + 1) * 128], ident[:])
        p.copy(ct_[:], pt[:].rearrange("p (a b) -> p a b", a=4))
        p.dma(kcatT_d[0:512, ts_].rearrange("(rc r) t -> r rc t", r=128), ct_[:], q="scalar")
    wb = wbf[1]
    if BFM:
        load_w_bf16(p, wb[:, :, 0:64], w_in_d[:, 3584:3648].rearrange("(kc k) n -> k kc n", k=128), wst, 64)
    else:
        p.dma(wf[:, :, 0:80], w_in_d[:, 4672:4752].rearrange("(kc k) n -> k kc n", k=128))
    k2 = [p.sb("k2_%d" % i, [128, 2, 64], F32) for i in range(2)]
    k2T = [p.sb("k2T_%d" % i, [128, 128] if BFM else [128, 1], BF16) for i in range(2)]
    k2Tf = [p.sb("k2Tf_%d" % i, [128, 128] if not BFM else [128, 1], F32) for i in range(2)]
    wis = [p.sb("wis%d" % i, [128, 16], F32) for i in range(2)]
    for tt in range(NT):
        ps, k_, kt_, w_, ktf_ = PS[cnt % 6], k2[tt % 2], k2T[tt % 2], wis[tt % 2], k2Tf[tt % 2]
        cnt += 1
        ts_ = slice(tt * 128, (tt + 1) * 128)
        if BFM:
            for kc in range(KC):
                p.mm(ps[:, 0:64], xT[:, kc, ts_], wb[:, kc, 0:64], start=(kc == 0), stop=(kc == KC - 1))
            p.copy(k_[:, 1, :], ps[:, 0:64], eng="scalar")
            rope_tm(p, k_[:, 0:1, :], k_[:, 1:2, :], cos[:, tt, :], sin[:, tt, :], 1, ta[:, 0:1, :], tb[:, 0:1, :])
            pt = PS[cnt % 6]
            cnt += 1
            p.tr(pt[0:64, 0:128], k_[:, 0, :], ident[:])
            p.copy(kt_[0:64, :], pt[0:64, 0:128], eng="scalar")
            p.dma(kcatT_d[512:576, ts_], kt_[0:64, :], q="scalar")
        else:
            for kc in range(KC):
                p.mm(ps[:, 0:80], xTf[:, kc, ts_], wf[:, kc, 0:80], start=(kc == 0), stop=(kc == KC - 1))
            p.copy(k_[:, 1, :], ps[:, 0:64], eng="scalar")
            rope_tm(p, k_[:, 0:1, :], k_[:, 1:2, :], cos[:, tt, :], sin[:, tt, :], 1, ta[:, 0:1, :], tb[:, 0:1, :])
            p.ts(w_[:], ps[:, 64:80], 1.0 / 32, None, op0=ALU.mult)
            p.dma(wi_d[ts_, :], w_[:], q="scalar")
            pt = PS[cnt % 6]
            cnt += 1
            p.tr(pt[0:64, 0:128], k_[:, 0, :], ident[:])
            p.copy(ktf_[0:64, :], pt[0:64, 0:128])
            p.dma(kiT_d[:, ts_], ktf_[0:64, :], q="scalar")
    p.end()


def stage_oq(p, cst_d, tqm_d, kiT_d, kcatT_d, ckv_d, qcatT_d, qiT_d, wi_d, w_uv_d, oT_d, nqt=NT):
    p.begin()
    C = load_cst(p, cst_d, ["ident", "iota", "ones"], bf=["ident", "ones"])
    identb, onesb, iota = C["ident_bf"], C["ones_bf"], C["iota"]
    I4 = p.sb("I4", [128, 4, 128], BF16)
    for j in range(4):
        p.copy(I4[:, j, :], identb[:])
    iota512 = p.sb("iota512", [128, 4, 128], F32)
    for j in range(4):
        p.ts(iota512[:, j, :], iota[:], float(j * 128), None, op0=ALU.add)
    io5 = iota512[:].rearrange("p a b -> p (a b)")
    tqm = p.sb("tqm", [128, NT, 16], F32)
    p.dma(tqm[:], tqm_d)
    PLT = [p.ps("PLT%d" % i, [128, 512]) for i in range(2)]
    POT = p.ps("POT", [128, 4, 512])
    PD = p.ps("PD", [128, 512])
    PM = p.ps("PM", [128, 512])
    kich = [p.sb("kich%d" % i, [64, 512], F32) for i in range(2)]
    wuv = p.sb("wuv", [128, 16, 4, 128], BF16)
    wst = [p.sb("wst%d" % i, [128, 4, 128], F32) for i in range(2)]
    for h in range(16):
        s_ = wst[h % 2]
        p.dma(s_[:], w_uv_d[h].rearrange("(rc r) v -> r rc v", r=128))
        p.copy(wuv[:, h, :, :], s_[:], eng="scalar" if h % 2 else "gpsimd")
    SG = 8
    kc_ = [p.sb("kcs%d" % i, [128, 5, SG * 128], BF16) for i in range(2)]
    cv_ = [p.sb("cvs%d" % i, [128, SG, 512], BF16) for i in range(2)]
    qc = [p.sb("qc%d" % i, [128, 5, 16, 128], BF16) for i in range(2)]
    qi = [p.sb("qi%d" % i, [64, 16, 128], F32) for i in range(2)]
    wi = [p.sb("wi%d" % i, [128, 16], F32) for i in range(2)]
    sc = p.sb("sc", [128, NKEY], F32)
    mb = p.sb("mb", [128, NKEY], BF16)
    junk = mb
    rl = [p.sb("rl%d" % i, [128, 512], F32) for i in range(2)]
    acc = p.sb("acc", [128, 512], F32)
    cm = p.sb("cm", [128, 512], F32)
    mn16 = p.sb("mn16", [128, 16], F32)
    b = {k: p.sb("b_" + k, [128, 1], F32) for k in ["lo", "hi", "mid", "hs", "cnt", "ge", "dl", "dh"]}
    PTt = [p.sb("PTt%d" % i, [128, 512], BF16) for i in range(2)]
    OTn = p.sb("OTn", [128, 4, 512], BF16)
    rrow = p.sb("rrow", [1, 512], F32)
    rrowb = p.sb("rrowb", [1, 512], BF16)
    rdb = p.sb("rdb", [128, 512], F32)
    oTt = [p.sb("oTt%d" % i, [128, 128], BF16) for i in range(2)]
    scale = 192 ** -0.5
    it = 0
    ld = 0
    for qt in range(nqt):
        q_, qi_, wi_ = qc[qt % 2], qi[qt % 2], wi[qt % 2]
        ts_ = slice(qt * 128, (qt + 1) * 128)
        for ch in range(4):
            p.dma(q_[:, ch, :, :], qcatT_d[ch * 128:(ch + 1) * 128, :, ts_], q="gpsimd")
        p.dma(q_[0:64, 4, :, :], qcatT_d[512:576, :, ts_], q="gpsimd")
        p.dma(qi_[:], qiT_d[:, :, ts_], q="gpsimd")
        p.dma(wi_[:], wi_d[ts_, :], q="gpsimd")
        for ck in range(16):
            kk = kich[ck % 2]
            p.dma(kk[:], kiT_d[:, ck * 512:(ck + 1) * 512], q="gpsimd")
            for h in range(16):
                ps, r_ = PLT[it % 2], rl[it % 2]
                it += 1
                p.mm(ps[:], qi_[:, h, :], kk[:])
                p.act(r_[:], ps[:], AF.Relu)
                if h == 0:
                    p.ts(acc[:], r_[:], wi_[:, 0:1], None, op0=ALU.mult)
                else:
                    p.stt(acc[:], r_[:], wi_[:, h:h + 1], acc[:], ALU.mult, ALU.add)
            p.reduce(mn16[:, ck:ck + 1], acc[:], ALU.min)
            p.ts(cm[:], io5, tqm[:, qt, ck:ck + 1], -1e30, op0=ALU.is_gt, op1=ALU.mult)
            p.tt(sc[:, ck * 512:(ck + 1) * 512], acc[:], cm[:], ALU.add)
        p.reduce(b["lo"][:], mn16[:], ALU.min)
        p.reduce(b["hi"][:], sc[:], ALU.max)
        for _ in range(NIT):
            p.ts(b["hs"][:], b["hi"][:], 0.5, None, op0=ALU.mult)
            p.stt(b["mid"][:], b["lo"][:], 0.5, b["hs"][:], ALU.mult, ALU.add)
            p.ts(junk[:], sc[:], b["mid"][:, 0:1], 0.0, op0=ALU.is_ge, op1=ALU.add, accum_out=b["cnt"][:])
            p.ts(b["ge"][:], b["cnt"][:], float(TOPK), None, op0=ALU.is_ge)
            p.tt(b["dl"][:], b["mid"][:], b["lo"][:], ALU.subtract)
            p.tt(b["dh"][:], b["hi"][:], b["mid"][:], ALU.subtract)
            p.stt(b["lo"][:], b["dl"][:], b["ge"][:, 0:1], b["lo"][:], ALU.mult, ALU.add)
            p.stt(b["hi"][:], b["dh"][:], b["ge"][:, 0:1], b["mid"][:], ALU.mult, ALU.add)
        p.ts(mb[:], sc[:], b["lo"][:, 0:1], -30000.0, op0=ALU.is_lt, op1=ALU.mult)
        for hg in range(4):
            hs = slice(hg * 4, (hg + 1) * 4)
            for st in range(NST):
                if st % SG == 0:
                    k_, c_ = kc_[ld % 2], cv_[ld % 2]
                    ld += 1
                    ks = slice(st * 128, (st + SG) * 128)
                    p.dma(k_[:, 0:4, :], kcatT_d[0:512, ks].rearrange("(c r) s -> r c s", r=128), q="sync")
                    p.dma(k_[0:64, 4, :], kcatT_d[512:576, ks], q="sync")
                    p.dma(c_[:], ckv_d[ks, :].rearrange("(a s) r -> s a r", s=128), q="sync")
                so = (st % SG) * 128
                ps, pt_ = PLT[it % 2], PTt[it % 2]
                it += 1
                for ch in range(4):
                    p.mm(ps[:], k_[:, ch, so:so + 128], q_[:, ch, hs, :], start=(ch == 0), stop=False)
                p.mm(ps[:], k_[0:64, 4, so:so + 128], q_[0:64, 4, hs, :], start=False, stop=False)
                p.mm(ps[:], mb[:, st * 128:(st + 1) * 128], I4[:], start=False, stop=True)
                p.act(pt_[:], ps[:], AF.Exp, scale=scale)
                for rc in range(4):
                    p.mm(POT[:, rc, :], c_[:, st % SG, rc * 128:(rc + 1) * 128], pt_[:], start=(st == 0), stop=(st == NST - 1))
                p.mm(PD[0:1, :], onesb[:, 0:1], pt_[:], start=(st == 0), stop=(st == NST - 1))
            p.copy(rrow[:], PD[0:1, :])
            p.recip(rrow[:], rrow[:])
            p.mm(PM[:], C["ones"][0:1, :], rrow[:])
            p.copy(rdb[:], PM[:], eng="scalar")
            for rc in range(4):
                p.tt(OTn[:, rc, :], POT[:, rc, :], rdb[:], ALU.mult)
            for hh in range(4):
                h = hg * 4 + hh
                o_ = oTt[h % 2]
                for rc in range(4):
                    p.mm(PM[:, 0:128], wuv[:, h, rc, :], OTn[:, rc, hh * 128:(hh + 1) * 128], start=(rc == 0), stop=(rc == 3))
                p.copy(o_[:], PM[:, 0:128], eng="scalar")
                p.dma(oT_d[h, :, ts_], o_[:], q="scalar")
    p.end()


NPDT = {F32: np.float32, I32: np.int32}


def _mk(nc):
    def din(name, shape, dt=F32):
        return nc.dram_tensor(name, list(shape), dt, kind="ExternalInput").ap()

    def dout(name, shape, dt=F32):
        return nc.dram_tensor(name, list(shape), dt, kind="ExternalOutput").ap()
    return din, dout


NCST = 128 * len(CST_NAMES)


def build_ea(stages=(1, 2, 3)):
    nc = bass.Bass("TRN2", target_bir_lowering=False)
    din, dout = _mk(nc)
    cst = din("cst", [128, NCST]); inv = din("inv", [128, 32]); pos = din("pos", [128, NTT], I32)
    xh = din("xh", [TT_, D]); w_in = din("w_in", [D, E_IN]); conv_w = din("conv_w", [4, 3072]); conv_b = din("conv_b", [3072])
    dt_bias = din("dt_bias", [32]); a_log = din("a_log", [32]); d_skip = din("d_skip", [32]); sinks = din("sinks", [16])
    mask = din("mask", [2, 128, 256])
    y = dout("y", [T, 2048]); z = dout("z", [T, 2048]); yatt = dout("yatt", [T, 1024]); ct = dout("ct", [4, 128, T], BF16)
    e = dout("e", [T, 32]); hloc = dout("hloc", [128, 2048]); dtot = dout("dtot", [128, 32])
    p = Prog(nc)
    SC = ea_scratch(p, "a")
    if 1 in stages:
        stage_ea1(p, cst, inv, pos, xh, w_in, conv_w, conv_b, z, ct, SC)
    if 2 in stages:
        stage_ea2(p, cst, dt_bias, a_log, d_skip, ct, SC, y, e, hloc, dtot)
    if 3 in stages:
        stage_ea3(p, cst, sinks, mask, SC, yatt)
    p.finish()
    return nc


def _moe_tail(p, nc, din, cst, x1, out):
    rg = din("rg", [D, 4]); rgb = din("rgb", [4]); re_ = din("re", [D, 32]); reb = din("reb", [32])
    wg = din("wg", [32, D, 512]); wu = din("wu", [32, D, 512]); wd = din("wd", [32, 512, D])
    pl = din("pl", [T, 256]); g2 = din("g2", [D]); b2 = din("b2", [D]); pwg = din("pwg", [D, D]); pbg = din("pbg", [D]); pwp = din("pwp", [256, D])
    ffn = p.dram("ffn_s", [T, D]).ap()
    stage_moe(p, cst, x1, rg, rgb, re_, reb, wg, wu, wd, ffn, "m")
    stage_tail(p, {"ident": cst[:, 0:128]}, x1, ffn, pl, g2, b2, pwg, pbg, pwp, out)


def _op(p, din, cst, inv, x, ext_k):
    pos8 = din("pos8", [128, NT], I32)
    ow_in = din("ow_in", [D, O_IN]); kvn = din("kvn", [512]); wuk = din("wuk", [16, 128, 512])
    if ext_k:
        _, dout = _mk(p.nc)
        kiT = dout("kiT", [64, T]); kcatT = dout("kcatT", [576, T], BF16); ckv = dout("ckv", [T, 512], BF16)
    else:
        kiT = p.dram("kiT_s", [64, T]).ap(); kcatT = p.dram("kcatT_s", [576, T], BF16).ap(); ckv = p.dram("ckv_s", [T, 512], BF16).ap()
    qcatT = p.dram("qcatT_s", [576, 16, T], BF16).ap(); qiT = p.dram("qiT_s", [64, 16, T]).ap(); wi = p.dram("wi_s", [T, 16]).ap()
    for mode in ("bf", "f32"):
        stage_op(p, cst, inv, pos8, x, ow_in, kvn, wuk, kiT, kcatT, ckv, qcatT, qiT, wi, mode)
    return qcatT, qiT, wi


def build_eb():
    nc = bass.Bass("TRN2", target_bir_lowering=False)
    din, dout = _mk(nc)
    cst = din("cst", [128, NCST]); inv = din("inv", [128, 32])
    y = din("y", [T, 2048]); z = din("z", [T, 2048]); yatt = din("yatt", [T, 1024]); ct = din("ct", [4, 128, T], BF16)
    e = din("e", [T, 32]); hprev = din("hprev", [7, 128, 2048]); dprev = din("dprev", [7, 128, 32])
    x = din("x", [T, D]); ssm_norm = din("ssm_norm", [2048]); w_out = din("w_out", [3072, D]); g1 = din("g1", [D]); b1 = din("b1", [D])
    xo = dout("xo", [T, D])
    p = Prog(nc)
    mixT = p.dram("mixT_s", [24, 128, T], BF16).ap()
    x1 = p.dram("x1_s", [T, D]).ap()
    stage_eb1(p, cst, y, z, yatt, ct, e, hprev, dprev, ssm_norm, mixT)
    stage_proj_ln(p, mixT, 24, w_out, x, g1, b1, x1)
    _moe_tail(p, nc, din, cst, x1, xo)
    _op(p, din, cst, inv, xo, True)
    p.finish()
    return nc


def build_odd():
    nc = bass.Bass("TRN2", target_bir_lowering=False)
    din, dout = _mk(nc)
    cst = din("cst", [128, NCST]); inv = din("inv", [128, 32])
    x = din("x", [T, D]); tqm = din("tqm", [128, NT, 16])
    kiT_f = din("kiT_f", [64, NKEY]); kcatT_f = din("kcatT_f", [576, NKEY], BF16); ckv_f = din("ckv_f", [NKEY, 512], BF16)
    wuv = din("wuv", [16, 512, 128]); ow_out = din("ow_out", [D, D]); g1 = din("g1", [D]); b1 = din("b1", [D])
    xo = dout("xo", [T, D])
    p = Prog(nc)
    qcatT, qiT, wi = _op(p, din, cst, inv, x, False)
    oT = p.dram("oT_s", [16, 128, T], BF16).ap()
    x1 = p.dram("x1_s", [T, D]).ap()
    stage_oq(p, cst, tqm, kiT_f, kcatT_f, ckv_f, qcatT, qiT, wi, wuv, oT)
    stage_proj_ln(p, oT, 16, ow_out, x, g1, b1, x1)
    _moe_tail(p, nc, din, cst, x1, xo)
    p.finish()
    return nc


def _run(nc, in_maps):
    res = run_bass_kernel_spmd(nc, in_maps, core_ids=list(range(8)))
    return res.results


def kernel(_dbg=None, **I):
    NCORE = 8
    C = lambda a: np.ascontiguousarray(a)
    cst = make_cst(); inv = make_inv()
    xcur = C(I["x"][0])
    P = np.asarray(I["positions"][0], np.int32)
    qi_ = np.arange(128)[:, None]; kj = np.arange(256)[None, :]
    band = (kj > qi_) & (kj <= qi_ + 128)
    m_rest = np.where(band, 0.0, -30000.0).astype(np.float32)
    m_first0 = np.where(band & (kj >= 128), 0.0, -30000.0).astype(np.float32)

    def moe_tail_inputs(L, c):
        sl = slice(c * T, (c + 1) * T)
        return {"rg": C(I["moe_router_group"][L]), "rgb": C(I["moe_router_group_b"][L]), "re": C(I["moe_router_expert"][L]),
                "reb": C(I["moe_router_expert_b"][L]), "wg": WG[L], "wu": WU[L], "wd": WD[L],
                "pl": C(I["p"][L, 0, sl]), "g2": C(I["ln2_g"][L]), "b2": C(I["ln2_b"][L]), "pwg": PWG[L],
                "pbg": C(I["ple_b_gate"][L]), "pwp": C(I["ple_w_proj"][L])}
    WG = [C(I["moe_w_gate"][L]) for L in range(4)]; WU = [C(I["moe_w_up"][L]) for L in range(4)]; WD = [C(I["moe_w_down"][L]) for L in range(4)]
    PWG = [C(I["ple_w_gate"][L]) for L in range(4)]
    nc_ea = nc_eb = nc_odd = None
    for L in range(4):
        j = L // 2
        if L % 2 == 0:
            nc_ea = nc_ea or build_ea()
            w_in = C(I["ev_w_in"][j])
            ims = []
            for c in range(NCORE):
                xh = np.zeros((TT_, D), np.float32); pe = np.zeros(TT_, np.int32)
                lo = c * T - 128
                if c > 0:
                    xh[:] = xcur[lo:lo + TT_]; pe[:] = P[lo:lo + TT_]
                else:
                    xh[128:] = xcur[0:T]; pe[128:] = P[0:T]
                ims.append({"cst": cst, "inv": inv, "pos": C(pe.reshape(NTT, 128).T), "xh": xh, "w_in": w_in,
                            "conv_w": C(I["ev_conv_w"][j]), "conv_b": C(I["ev_conv_b"][j]), "dt_bias": C(I["ev_dt_bias"][j]),
                            "a_log": C(I["ev_a_log"][j]), "d_skip": C(I["ev_d_skip"][j]), "sinks": C(I["ev_sinks"][j]),
                            "mask": np.stack([m_first0 if c == 0 else m_rest, m_rest])})
            ra = _run(nc_ea, ims)
            nc_eb = nc_eb or build_eb()
            ims = []
            for c in range(NCORE):
                sl = slice(c * T, (c + 1) * T)
                hp = np.zeros((7, 128, 2048), np.float32); dp = np.ones((7, 128, 32), np.float32)
                for cc in range(c):
                    hp[7 - c + cc] = ra[cc]["hloc"]; dp[7 - c + cc] = ra[cc]["dtot"]
                m = {"cst": cst, "inv": inv, "y": ra[c]["y"], "z": ra[c]["z"], "yatt": ra[c]["yatt"], "ct": ra[c]["ct"], "e": ra[c]["e"],
                     "hprev": hp, "dprev": dp, "x": C(xcur[sl]), "ssm_norm": C(I["ev_ssm_norm"][j]), "w_out": C(I["ev_w_out"][j]),
                     "g1": C(I["ln1_g"][L]), "b1": C(I["ln1_b"][L]), "pos8": C(P[sl].reshape(NT, 128).T),
                     "ow_in": C(I["od_w_in"][j]), "kvn": C(I["od_kv_norm"][j]), "wuk": C(I["od_w_uk"][j])}
                m.update(moe_tail_inputs(L, c))
                ims.append(m)
            rb = _run(nc_eb, ims)
            xcur = np.concatenate([rb[c]["xo"] for c in range(NCORE)], 0)
            kiT_f = np.concatenate([rb[c]["kiT"] for c in range(NCORE)], 1)
            kcatT_f = np.concatenate([rb[c]["kcatT"] for c in range(NCORE)], 1)
            ckv_f = np.concatenate([rb[c]["ckv"] for c in range(NCORE)], 0)
        else:
            nc_odd = nc_odd or build_odd()
            ims = []
            for c in range(NCORE):
                sl = slice(c * T, (c + 1) * T)
                tq = (c * T + np.arange(NT)[None, :] * 128 + np.arange(128)[:, None]).astype(np.float32)
                tqm = C(tq[:, :, None] - (np.arange(16) * 512)[None, None, :].astype(np.float32))
                m = {"cst": cst, "inv": inv, "x": C(xcur[sl]), "tqm": tqm, "kiT_f": C(kiT_f), "kcatT_f": C(kcatT_f), "ckv_f": C(ckv_f),
                     "wuv": C(I["od_w_uv"][j]), "ow_out": C(I["od_w_out"][j]), "g1": C(I["ln1_g"][L]), "b1": C(I["ln1_b"][L]),
                     "pos8": C(P[sl].reshape(NT, 128).T), "ow_in": C(I["od_w_in"][j]), "kvn": C(I["od_kv_norm"][j]), "wuk": C(I["od_w_uk"][j])}
                m.update(moe_tail_inputs(L, c))
                ims.append(m)
            ro = _run(nc_odd, ims)
            xcur = np.concatenate([ro[c]["xo"] for c in range(NCORE)], 0)
        if _dbg is not None:
            _dbg(L, xcur)
    return xcur[None].astype(np.float32)
```

```python
import numpy as np
from contextlib import ExitStack
import concourse.bass as bass
import concourse.mybir as mybir
from concourse.bass_utils import run_bass_kernel_spmd

F32 = mybir.dt.float32
BF16 = mybir.dt.bfloat16
I32 = mybir.dt.int32
AF = mybir.ActivationFunctionType
ALU = mybir.AluOpType
AX = mybir.AxisListType

COMPUTE = ("tensor", "vector", "scalar", "gpsimd")
QUEUES = ("sync", "scalar", "gpsimd")
NSLOT = 8


class Prog:
    def __init__(self, nc):
        self.nc = nc
        self.ops = []
        self.es = None

    def sb(self, name, shape, dt=F32):
        self.uid = getattr(self, "uid", 0) + 1
        return self.es.enter_context(self.nc.sbuf_tensor("s%d_%s" % (self.uid, name), list(shape), dt))

    def ps(self, name, shape, dt=F32):
        self.uid = getattr(self, "uid", 0) + 1
        return self.es.enter_context(self.nc.psum_tensor("p%d_%s" % (self.uid, name), list(shape), dt))

    def dram(self, name, shape, dt=F32, kind=None):
        if kind is None:
            return self.nc.dram_tensor(name, list(shape), dt)
        return self.nc.dram_tensor(name, list(shape), dt, kind=kind)

    def allgather(self, out_t, in_t, n=8):
        self.add("gpsimd", lambda e: e.collective_compute("AllGather", ALU.bypass, replica_groups=[list(range(n))],
                                                          ins=[in_t.ap().opt()], outs=[out_t.ap().opt()]),
                 [in_t], [out_t], dma="cc")

    @staticmethod
    def _keys(aps):
        ks = []
        for a in aps:
            if a is None or isinstance(a, (int, float)):
                continue
            ks.append(a.tensor.name if hasattr(a, "tensor") else a.name)
        return ks

    def add(self, eng, fn, r, w, dma=False):
        self.ops.append(dict(eng=eng, fn=fn, r=self._keys(r), w=self._keys(w), dma=dma))

    def mm(self, out, lhsT, rhs, start=True, stop=True):
        self.add("tensor", lambda e: e.matmul(out, lhsT, rhs, start=start, stop=stop), [lhsT, rhs], [out])

    def tr(self, out, in_, ident):
        self.add("tensor", lambda e: e.transpose(out, in_, ident), [in_, ident], [out])

    def act(self, out, in_, func, bias=0.0, scale=1.0, accum_out=None, eng="scalar"):
        r = [in_] + [b for b in (bias, scale) if not isinstance(b, (int, float))]
        w = [out] + ([accum_out] if accum_out is not None else [])
        if accum_out is None:
            self.add(eng, lambda e: e.activation(out, in_, func, bias=bias, scale=scale), r, w)
        else:
            self.add(eng, lambda e: e.activation(out, in_, func, bias=bias, scale=scale, accum_out=accum_out), r, w)

    def tt(self, out, in0, in1, op, eng="vector"):
        self.add(eng, lambda e: e.tensor_tensor(out, in0, in1, op), [in0, in1], [out])

    def ts(self, out, in0, s1, s2=None, op0=ALU.mult, op1=None, accum_out=None, eng="vector"):
        r = [in0] + [s for s in (s1, s2) if s is not None and not isinstance(s, (int, float))]
        w = [out] + ([accum_out] if accum_out is not None else [])
        kw = {}
        if op1 is not None:
            kw["op1"] = op1
        if accum_out is not None:
            kw["accum_out"] = accum_out
        self.add(eng, lambda e: e.tensor_scalar(out, in0, s1, s2, op0, **kw), r, w)

    def stt(self, out, in0, scalar, in1, op0, op1, eng="vector"):
        r = [in0, in1] + ([scalar] if not isinstance(scalar, (int, float)) else [])
        self.add(eng, lambda e: e.scalar_tensor_tensor(out, in0, scalar, in1, op0, op1), r, [out])

    def copy(self, out, in_, eng="vector"):
        if eng == "scalar":
            self.add(eng, lambda e: e.copy(out, in_), [in_], [out])
        else:
            self.add(eng, lambda e: e.tensor_copy(out, in_), [in_], [out])

    def reduce(self, out, in_, op, axis=AX.X, eng="vector"):
        self.add(eng, lambda e: e.tensor_reduce(out, in_, axis, op), [in_], [out])

    def memset(self, ap, val, eng="vector"):
        self.add(eng, lambda e: e.memset(ap, val), [], [ap])

    def recip(self, out, in_):
        self.add("vector", lambda e: e.reciprocal(out, in_), [in_], [out])

    def dma(self, out, in_, q="sync", **kw):
        self.add(q, lambda e: e.dma_start(out, in_, **kw), [in_], [out], dma=True)

    def raw(self, eng, fn, r, w):
        self.add(eng, fn, r, w)

    def _init_sems(self):
        nc = self.nc
        self.ges = ExitStack()
        self.eng_sem = {e: self.ges.enter_context(nc.semaphore("sem_" + e)) for e in COMPUTE}
        self.slot_sem = {q: [self.ges.enter_context(nc.semaphore("dq_%s_%d" % (q, s))) for s in range(NSLOT)]
                         for q in QUEUES}
        self.cc_sem = self.ges.enter_context(nc.semaphore("cc_sem"))
        self.cc_cnt = 0
        self.eng_cnt = {e: 0 for e in COMPUTE}
        self.q_cnt = {q: 0 for q in QUEUES}
        self.slot_val = {q: [0] * NSLOT for q in QUEUES}
        self.total_ops = 0

    def begin(self):
        if not hasattr(self, "eng_sem"):
            self._init_sems()
        self.es = ExitStack()
        self.ops = []

    def end(self):
        nc = self.nc
        ops = self.ops
        last_w = {}
        readers = {}
        for i, op in enumerate(ops):
            deps = set()
            for k in op["r"]:
                if k in last_w:
                    deps.add(last_w[k])
            for k in op["w"]:
                if k in last_w:
                    deps.add(last_w[k])
                deps.update(readers.get(k, ()))
            deps.discard(i)
            op["deps"] = deps
            for k in op["r"]:
                readers.setdefault(k, []).append(i)
            for k in op["w"]:
                last_w[k] = i
                readers[k] = []
        eng_sem, slot_sem = self.eng_sem, self.slot_sem
        eng_cnt, q_cnt, slot_val = self.eng_cnt, self.q_cnt, self.slot_val
        for op in ops:
            if op["dma"] == "cc":
                self.cc_cnt += 1
                op["tok"] = (self.cc_sem, self.cc_cnt)
                op["slot_prev"] = (self.cc_sem, self.cc_cnt - 1)
            elif op["dma"]:
                q = op["eng"]
                s = q_cnt[q] % NSLOT
                q_cnt[q] += 1
                op["slot_prev"] = (slot_sem[q][s], slot_val[q][s])
                slot_val[q][s] += 16
                op["tok"] = (slot_sem[q][s], slot_val[q][s])
            else:
                e = op["eng"]
                eng_cnt[e] += 1
                op["tok"] = (eng_sem[e], eng_cnt[e])
        per_eng = {e: [] for e in set(COMPUTE) | set(QUEUES)}
        for i, op in enumerate(ops):
            per_eng[op["eng"]].append(i)
        self.total_ops += len(ops)
        final = []
        for q in QUEUES:
            for s in range(NSLOT):
                if slot_val[q][s] > 0:
                    final.append((slot_sem[q][s], slot_val[q][s]))
        for en in COMPUTE:
            if eng_cnt[en] > 0:
                final.append((eng_sem[en], eng_cnt[en]))
        if self.cc_cnt > 0:
            final.append((self.cc_sem, self.cc_cnt))

        def run_engine(ename, e):
            seen = {}
            for i in per_eng[ename]:
                op = ops[i]
                waits = {}
                for j in op["deps"]:
                    dj = ops[j]
                    if ename == "tensor" and dj["eng"] == "tensor" and not dj["dma"]:
                        continue
                    sem, val = dj["tok"]
                    waits[sem] = max(waits.get(sem, 0), val)
                if op["dma"]:
                    sem, val = op["slot_prev"]
                    if val > 0:
                        waits[sem] = max(waits.get(sem, 0), val)
                for sem, val in waits.items():
                    if seen.get(sem, 0) >= val:
                        continue
                    e.wait_ge(sem, val)
                    seen[sem] = val
                ins = op["fn"](e)
                sem, val = op["tok"]
                if op["dma"] == "cc":
                    ins.then_inc(sem)
                else:
                    ins.then_inc(sem, 16 if op["dma"] else 1)
            for sem, val in final:
                e.wait_ge(sem, val)

        with nc.Block() as block:
            @block.tensor
            def _(e):
                run_engine("tensor", e)

            @block.vector
            def _(e):
                run_engine("vector", e)

            @block.scalar
            def _(e):
                run_engine("scalar", e)

            @block.gpsimd
            def _(e):
                run_engine("gpsimd", e)

            @block.sync
            def _(e):
                run_engine("sync", e)
        self.es.close()
        self.ops = []

    def finish(self):
        self.ges.close()


T = 1024
NT = T // 128
D = 2048
KC = D // 128
ALPHA = 8 ** 0.25
EPS = 1e-5


def bcast_rows(ap1d, n, parts=128):
    return ap1d.rearrange("(o n) -> o n", o=1).broadcast_to([parts, n])


def load_consts(p, c):
    ident = p.sb("ident", [128, 128], F32)
    p.dma(ident[:], c["ident"][:, :])
    identb = p.sb("identb", [128, 128], BF16)
    p.copy(identb[:], ident[:])
    return ident, identb


def ln_scratch(p, n=2):
    return [(p.sb("lnst_%d" % i, [128, 4, 6], F32), p.sb("lnmv_%d" % i, [128, 2], F32),
             p.sb("lnrs_%d" % i, [128, 1], F32)) for i in range(n)]


def layer_norm_tile(p, out, v, g_rows, b_rows, scr):
    stats, mv, rstd = scr
    for c in range(4):
        p.raw("vector", lambda e, c=c: e.bn_stats(stats[:, c, :], v[:, c * 512:(c + 1) * 512]), [v], [stats])
    p.raw("vector", lambda e: e.bn_aggr(mv[:], stats[:].rearrange("p a b -> p (a b)")), [stats], [mv])
    p.ts(rstd[:], mv[:, 1:2], EPS, None, op0=ALU.add)
    p.act(rstd[:], rstd[:], AF.Sqrt)
    p.recip(rstd[:], rstd[:])
    p.ts(out, v, mv[:, 0:1], rstd[:, 0:1], op0=ALU.subtract, op1=ALU.mult)
    p.tt(out, out, g_rows, ALU.mult, eng="gpsimd")
    p.tt(out, out, b_rows, ALU.add, eng="gpsimd")


def transpose_to(p, dstT, src, ident, nchunks, tcol, pst, copy_engs=("vector", "scalar")):
    for g in range(0, nchunks, 4):
        ps = pst[(g // 4) % len(pst)]
        n = min(4, nchunks - g)
        for j in range(n):
            p.tr(ps[:, j * 128:(j + 1) * 128], src[:, (g + j) * 128:(g + j + 1) * 128], ident[:])
        eng = copy_engs[(g // 4) % len(copy_engs)]
        p.copy(dstT[:, g:g + n, tcol:tcol + 128], ps[:, 0:n * 128].rearrange("p (a b) -> p a b", a=n), eng=eng)


def load_w_bf16(p, dst, src, stg, n, engs=("scalar", "gpsimd"), qs=("sync", "sync"), g=4):
    kc = dst.shape[1]
    st = getattr(p, "_wctr", 0)
    for i, k0 in enumerate(range(0, kc, g)):
        k1 = min(kc, k0 + g)
        s_ = stg[(st + i) % len(stg)]
        p.dma(s_[:, 0:k1 - k0, 0:n], src[:, k0:k1, :], q=qs[(st + i) % len(qs)])
        p.copy(dst[:, k0:k1, :], s_[:, 0:k1 - k0, 0:n], eng=engs[(st + i) % len(engs)])
    p._wctr = st + (kc + g - 1) // g


def stage_tail(p, c, x1_d, ffn_d, pl_d, ln_g, ln_b, wg_d, bg_d, wp_d, out_d):
    p.begin()
    ident, identb = load_consts(p, c)
    g_rows = p.sb("g_rows", [128, D], F32)
    b_rows = p.sb("b_rows", [128, D], F32)
    bg_rows = p.sb("bg_rows", [128, D], F32)
    p.dma(g_rows[:], bcast_rows(ln_g, D))
    p.dma(b_rows[:], bcast_rows(ln_b, D))
    p.dma(bg_rows[:], bcast_rows(bg_d, D))
    x2 = p.sb("x2", [128, NT, D], F32)
    x2T = p.sb("x2T", [128, KC, T], BF16)
    plT = p.sb("plT", [128, 2, T], BF16)
    pst = [p.ps("pst0", [128, 512]), p.ps("pst1", [128, 512])]
    fa = [p.sb("fa0", [128, D], F32)] * 2
    pa = [p.sb("pa0", [128, 256], F32), p.sb("pa1", [128, 256], F32)]
    lns = ln_scratch(p)
    for tt in range(NT):
        a, f, pp = x2[:, tt, :], fa[tt % 2], pa[tt % 2]
        p.dma(a, x1_d[tt * 128:(tt + 1) * 128, :])
        p.dma(f[:], ffn_d[tt * 128:(tt + 1) * 128, :], q="gpsimd")
        p.dma(pp[:], pl_d[tt * 128:(tt + 1) * 128, :])
        p.stt(a, a, ALPHA, f[:], ALU.mult, ALU.add)
        layer_norm_tile(p, a, a, g_rows[:], b_rows[:], lns[tt % 2])
        transpose_to(p, x2T, x2[:, tt, :], ident, KC, tt * 128, pst)
        transpose_to(p, plT, pp, ident, 2, tt * 128, pst)
    wst = [p.sb("wst0", [128, 4, 512], F32), p.sb("wst1", [128, 4, 512], F32)]
    wbf = [p.sb("wbf0", [128, KC, 512], BF16), p.sb("wbf1", [128, KC, 512], BF16)]
    wpbf = [p.sb("wpbf0", [128, 2, 512], BF16), p.sb("wpbf1", [128, 2, 512], BF16)]
    psg = [p.ps("psg0", [128, 512]), p.ps("psg1", [128, 512])]
    psp = [p.ps("psp0", [128, 512]), p.ps("psp1", [128, 512])]
    gate = [p.sb("gate0", [128, 512], F32), p.sb("gate1", [128, 512], F32)]
    ob = [p.sb("ob0", [128, 512], F32), p.sb("ob1", [128, 512], F32)]
    for n in range(4):
        cs = slice(n * 512, (n + 1) * 512)
        wb, wpb = wbf[n % 2], wpbf[n % 2]
        load_w_bf16(p, wb, wg_d[:, cs].rearrange("(kc k) n -> k kc n", k=128), wst, 512)
        load_w_bf16(p, wpb, wp_d[:, cs].rearrange("(kc k) n -> k kc n", k=128), wst, 512)
        for tt in range(NT):
            i = n * NT + tt
            pg, pp_, gt, o = psg[i % 2], psp[i % 2], gate[i % 2], ob[i % 2]
            for kc in range(KC):
                p.mm(pg[:], x2T[:, kc, tt * 128:(tt + 1) * 128], wb[:, kc, :], start=(kc == 0), stop=(kc == KC - 1))
            for kc in range(2):
                p.mm(pp_[:], plT[:, kc, tt * 128:(tt + 1) * 128], wpb[:, kc, :], start=(kc == 0), stop=(kc == 1))
            p.tt(gt[:], pg[:], bg_rows[:, cs], ALU.add)
            p.act(gt[:], gt[:], AF.Sigmoid)
            p.tt(o[:], pp_[:], gt[:], ALU.mult)
            p.tt(o[:], o[:], x2[:, tt, cs], ALU.add, eng="gpsimd")
            p.dma(out_d[tt * 128:(tt + 1) * 128, cs], o[:], q="scalar")
    p.end()


CST_NAMES = ["ident", "triu", "trius", "ones", "iota", "tril_s"]


def make_cst():
    r = np.arange(128)
    ident = np.eye(128, dtype=np.float32)
    triu = (r[:, None] <= r[None, :]).astype(np.float32)
    trius = (r[:, None] < r[None, :]).astype(np.float32)
    ones = np.ones((128, 128), np.float32)
    iota = np.broadcast_to(r[None, :].astype(np.float32), (128, 128)).copy()
    tril_s = (r[:, None] > r[None, :]).astype(np.float32)
    return np.concatenate([ident, triu, trius, ones, iota, tril_s], axis=1)


def load_cst(p, cst_d, names, bf=()):
    out = {}
    for nm in names:
        i = CST_NAMES.index(nm)
        t = p.sb("c_" + nm, [128, 128], F32)
        p.dma(t[:], cst_d[:, i * 128:(i + 1) * 128])
        out[nm] = t
    for nm in bf:
        tb = p.sb("cb_" + nm, [128, 128], BF16)
        p.copy(tb[:], out[nm][:])
        out[nm + "_bf"] = tb
    return out


CAP = 128
NE = 32
FF = 512


def stage_moe(p, cst_d, x1_d, rg_d, rgb_d, re_d, reb_d, wg_d, wu_d, wd_d, ffn_d, tag):
    xbf_d = p.dram("moe_xbf_" + tag, [T, D], BF16).ap()
    rt_d = p.dram("moe_rt_" + tag, [3, 128, NT * NE], F32).ap()
    p.begin()
    C = load_cst(p, cst_d, ["ident", "trius", "ones", "iota"], bf=["ident", "trius", "ones"])
    ident, iota = C["ident"], C["iota"]
    PA = p.ps("PA", [128, 2048])
    PG = p.ps("PG", [128, 512])
    PU = p.ps("PU", [128, 512])
    Xbf = p.sb("Xbf", [128, NT, D], BF16)
    asg = p.sb("asg", [128, NT, NE], F32)
    gts = p.sb("gts", [128, NT, NE], F32)
    pos = p.sb("pos", [128, NT, NE], F32)
    asgb = p.sb("asgb", [128, NT, NE], BF16)
    wr = p.sb("wr", [128, KC, 36], F32)
    p.dma(wr[:, :, 0:4], rg_d.rearrange("(kc k) n -> k kc n", k=128))
    p.dma(wr[:, :, 4:36], re_d.rearrange("(kc k) n -> k kc n", k=128))
    rb = p.sb("rb", [128, 36], F32)
    p.dma(rb[:, 0:4], bcast_rows(rgb_d, 4))
    p.dma(rb[:, 4:36], bcast_rows(reb_d, 32))
    xt = [p.sb("xt0", [128, D], F32), p.sb("xt1", [128, D], F32)]
    xTf = [p.sb("xTf0", [128, KC, 128], F32), p.sb("xTf1", [128, KC, 128], F32)]
    sm = {k: p.sb("sm_" + k, [128, n], F32) for k, n in
          [("lg", 36), ("gmax", 1), ("oh", 4), ("ge", 4), ("gsum", 1), ("tmp", 32), ("es", 8), ("v1", 1), ("m1", 8),
           ("e2", 8), ("v2", 1), ("m2", 8), ("d", 1), ("p1", 1), ("p2", 1), ("mm", 8), ("gm", 8)]}
    for tt in range(NT):
        x, xf = xt[tt % 2], xTf[tt % 2]
        p.dma(x[:], x1_d[tt * 128:(tt + 1) * 128, :])
        p.copy(Xbf[:, tt, :], x[:], eng="gpsimd")
        for g in range(0, KC, 4):
            for j in range(4):
                p.tr(PA[:, j * 128:(j + 1) * 128], x[:, (g + j) * 128:(g + j + 1) * 128], ident[:])
            p.copy(xf[:, g:g + 4, :], PA[:, 0:512].rearrange("p (a b) -> p a b", a=4), eng="scalar" if (g // 4) % 2 else "vector")
        for kc in range(KC):
            p.mm(PG[:, 0:36], xf[:, kc, :], wr[:, kc, :], start=(kc == 0), stop=(kc == KC - 1))
        lg = sm["lg"]
        p.tt(lg[:], PG[:, 0:36], rb[:], ALU.add)
        p.reduce(sm["gmax"][:], lg[:, 0:4], ALU.max)
        p.ts(sm["oh"][:], lg[:, 0:4], sm["gmax"][:, 0:1], None, op0=ALU.is_equal)
        p.ts(sm["ge"][:], lg[:, 0:4], sm["gmax"][:, 0:1], None, op0=ALU.subtract)
        p.act(sm["ge"][:], sm["ge"][:], AF.Exp, accum_out=sm["gsum"][:])
        p.recip(sm["gsum"][:], sm["gsum"][:])
        p.tt(sm["tmp"][:].rearrange("p (g i) -> p g i", g=4), lg[:, 4:36].rearrange("p (g i) -> p g i", g=4),
             sm["oh"][:, :, None].to_broadcast([128, 4, 8]), ALU.mult)
        p.reduce(sm["es"][:], sm["tmp"][:].rearrange("p (g i) -> p i g", g=4), ALU.add)
        p.reduce(sm["v1"][:], sm["es"][:], ALU.max)
        p.ts(sm["m1"][:], sm["es"][:], sm["v1"][:, 0:1], None, op0=ALU.is_equal)
        p.stt(sm["e2"][:], sm["m1"][:], -1e30, sm["es"][:], ALU.mult, ALU.add)
        p.reduce(sm["v2"][:], sm["e2"][:], ALU.max)
        p.ts(sm["m2"][:], sm["e2"][:], sm["v2"][:, 0:1], None, op0=ALU.is_equal)
        p.tt(sm["d"][:], sm["v2"][:], sm["v1"][:], ALU.subtract)
        p.act(sm["d"][:], sm["d"][:], AF.Exp)
        p.ts(sm["p1"][:], sm["d"][:], 1.0, None, op0=ALU.add)
        p.recip(sm["p1"][:], sm["p1"][:])
        p.tt(sm["p2"][:], sm["d"][:], sm["p1"][:], ALU.mult)
        p.tt(sm["p1"][:], sm["p1"][:], sm["gsum"][:], ALU.mult)
        p.tt(sm["p2"][:], sm["p2"][:], sm["gsum"][:], ALU.mult)
        p.tt(sm["mm"][:], sm["m1"][:], sm["m2"][:], ALU.add)
        p.ts(sm["gm"][:], sm["m1"][:], sm["p1"][:, 0:1], None, op0=ALU.mult)
        p.stt(sm["gm"][:], sm["m2"][:], sm["p2"][:, 0:1], sm["gm"][:], ALU.mult, ALU.add)
        p.tt(asg[:, tt, :].rearrange("p (g i) -> p g i", g=4), sm["oh"][:, :, None].to_broadcast([128, 4, 8]),
             sm["mm"][:, None, :].to_broadcast([128, 4, 8]), ALU.mult)
        p.tt(gts[:, tt, :].rearrange("p (g i) -> p g i", g=4), sm["oh"][:, :, None].to_broadcast([128, 4, 8]),
             sm["gm"][:, None, :].to_broadcast([128, 4, 8]), ALU.mult)
        p.copy(asgb[:, tt, :], asg[:, tt, :])
        for t2 in range(tt + 1):
            lhs = C["trius_bf"] if t2 == tt else C["ones_bf"]
            p.mm(PU[:, 0:32], lhs[:], asgb[:, t2, :], start=(t2 == 0), stop=(t2 == tt))
        p.copy(pos[:, tt, :], PU[:, 0:32])
        p.dma(xbf_d[tt * 128:(tt + 1) * 128, :], Xbf[:, tt, :], q="gpsimd")
    p.dma(rt_d[0], asg[:].rearrange("p a b -> p (a b)"))
    p.dma(rt_d[1], gts[:].rearrange("p a b -> p (a b)"))
    p.dma(rt_d[2], pos[:].rearrange("p a b -> p (a b)"))
    p.end()
    p.begin()
    C = load_cst(p, cst_d, ["ident", "iota"], bf=["ident"])
    iota = C["iota"]
    PA = p.ps("PA", [128, 2048])
    PG = p.ps("PG", [128, 512])
    PU = p.ps("PU", [128, 512])
    PT = p.ps("PT", [128, 1024], BF16)
    PC = p.ps("PC", [128, 512])
    Xbf = p.sb("Xbf", [128, NT, D], BF16)
    yacc = p.sb("yacc", [128, NT, D], F32)
    asg = p.sb("asg", [128, NT, NE], F32)
    gts = p.sb("gts", [128, NT, NE], F32)
    pos = p.sb("pos", [128, NT, NE], F32)
    p.dma(Xbf[:], xbf_d.rearrange("(a p) d -> p a d", p=128))
    p.dma(asg[:].rearrange("p a b -> p (a b)"), rt_d[0])
    p.dma(gts[:].rearrange("p a b -> p (a b)"), rt_d[1])
    p.dma(pos[:].rearrange("p a b -> p (a b)"), rt_d[2])
    wst = [p.sb("wst%d" % i, [128, 4, 512], F32) for i in range(2)]
    Wg = p.sb("Wg", [128, KC, FF], BF16)
    Wu = p.sb("Wu", [128, KC, FF], BF16)
    Wd = p.sb("Wd", [128, 4, D], BF16)
    Se = [p.sb("Se%d" % i, [128, NT, CAP], BF16) for i in range(2)]
    GeT = p.sb("GeT", [128, 4, NT, 128], BF16)
    Ge = [p.sb("Ge%d" % i, [128, NT, CAP], BF16) for i in range(2)]
    XeT = p.sb("XeT", [128, KC, CAP], BF16)
    sg = p.sb("sg", [128, FF], F32)
    hidT = p.sb("hidT", [128, 4, CAP], BF16)
    Y = p.sb("Y", [128, 4, D], BF16)
    for e in range(NE):
        slot = e % 4
        S, G = Se[e % 2], Ge[e % 2]
        posb = pos[:, :, e:e + 1].to_broadcast([128, NT, CAP])
        p.tt(S[:], iota[:, None, :].to_broadcast([128, NT, CAP]), posb, ALU.is_equal)
        p.tt(G[:], S[:], gts[:, :, e:e + 1].to_broadcast([128, NT, CAP]), ALU.mult)
        p.tt(S[:], S[:], asg[:, :, e:e + 1].to_broadcast([128, NT, CAP]), ALU.mult)
        load_w_bf16(p, Wg, wg_d[e].rearrange("(kc k) n -> k kc n", k=128), wst, 512, engs=("scalar", "gpsimd", "vector"), qs=("sync",))
        load_w_bf16(p, Wu, wu_d[e].rearrange("(kc k) n -> k kc n", k=128), wst, 512, engs=("scalar", "gpsimd", "vector"), qs=("sync",))
        wdv = wd_d[e].rearrange("(kc k) n -> k kc n", k=128)
        for dc in range(4):
            load_w_bf16(p, Wd[:, :, dc * 512:(dc + 1) * 512], wdv[:, :, dc * 512:(dc + 1) * 512], wst, 512,
                        engs=("scalar", "gpsimd", "vector"), qs=("sync",))
        for fc in range(KC):
            for tt in range(NT):
                p.mm(PA[:, fc * 128:(fc + 1) * 128], Xbf[:, tt, fc * 128:(fc + 1) * 128], S[:, tt, :],
                     start=(tt == 0), stop=(tt == NT - 1))
        p.copy(XeT[:], PA[:].rearrange("p (a b) -> p a b", a=KC), eng="scalar")
        for fch in range(4):
            for kc in range(KC):
                p.mm(PG[:, fch * 128:(fch + 1) * 128], Wg[:, kc, fch * 128:(fch + 1) * 128], XeT[:, kc, :],
                     start=(kc == 0), stop=(kc == KC - 1))
        for fch in range(4):
            for kc in range(KC):
                p.mm(PU[:, fch * 128:(fch + 1) * 128], Wu[:, kc, fch * 128:(fch + 1) * 128], XeT[:, kc, :],
                     start=(kc == 0), stop=(kc == KC - 1))
        p.act(sg[:], PG[:], AF.Silu)
        p.tt(hidT[:].rearrange("p a b -> p (a b)"), sg[:], PU[:], ALU.mult)
        for dc in range(4):
            for fch in range(4):
                p.mm(PA[:, dc * 512:(dc + 1) * 512], hidT[:, fch, :], Wd[:, fch, dc * 512:(dc + 1) * 512],
                     start=(fch == 0), stop=(fch == 3))
        p.copy(Y[:, slot, :], PA[:], eng="scalar")
        for tt in range(NT):
            p.tr(PT[:, tt * 128:(tt + 1) * 128], G[:, tt, :], C["ident_bf"][:])
        p.copy(GeT[:, slot, :, :], PT[:].rearrange("p (a b) -> p a b", a=NT))
        if slot == 3:
            grp = e // 4
            for tt in range(NT):
                for dc in range(4):
                    for s4 in range(4):
                        p.mm(PC[:], GeT[:, s4, tt, :], Y[:, s4, dc * 512:(dc + 1) * 512], start=(s4 == 0), stop=(s4 == 3))
                    dst = yacc[:, tt, dc * 512:(dc + 1) * 512]
                    if grp == 0:
                        p.copy(dst, PC[:])
                    else:
                        p.tt(dst, PC[:], dst, ALU.add)
    for tt in range(NT):
        p.dma(ffn_d[tt * 128:(tt + 1) * 128, :], yacc[:, tt, :])
    p.end()


TWO_PI = 2.0 * np.pi
CW1 = 6.28125
CW2 = TWO_PI - CW1


def make_inv():
    inv = (10000.0 ** (-np.arange(0, 64, 2, dtype=np.float32) / 64)).astype(np.float32)
    return np.broadcast_to(inv[None, :], (128, 32)).copy()


def rope_tables(p, pos_d, inv_d, ntiles):
    n = ntiles * 32
    posi = p.sb("posi", [128, ntiles], I32)
    posf = p.sb("posf", [128, ntiles], F32)
    inv = p.sb("inv", [128, 32], F32)
    ang = p.sb("ang", [128, ntiles, 32], F32)
    ki = p.sb("ki", [128, ntiles, 32], I32)
    kf = p.sb("kf", [128, ntiles, 32], F32)
    r = p.sb("r", [128, ntiles, 32], F32)
    m = p.sb("rm", [128, ntiles, 32], F32)
    cos = p.sb("cos", [128, ntiles, 32], F32)
    sin = p.sb("sin", [128, ntiles, 32], F32)
    p.dma(posi[:], pos_d)
    p.dma(inv[:], inv_d)
    p.copy(posf[:], posi[:], eng="scalar")
    p.tt(ang[:], inv[:, None, :].to_broadcast([128, ntiles, 32]), posf[:, :, None].to_broadcast([128, ntiles, 32]), ALU.mult)
    p.ts(kf[:], ang[:], 1.0 / TWO_PI, None, op0=ALU.mult)
    p.copy(ki[:], kf[:])
    p.copy(kf[:], ki[:])
    p.stt(r[:], kf[:], -CW1, ang[:], ALU.mult, ALU.add)
    p.stt(r[:], kf[:], -CW2, r[:], ALU.mult, ALU.add)

    def wrap(t):
        p.ts(m[:], t[:], np.pi, None, op0=ALU.is_gt)
        p.stt(t[:], m[:], -TWO_PI, t[:], ALU.mult, ALU.add)
        p.ts(m[:], t[:], -np.pi, None, op0=ALU.is_lt)
        p.stt(t[:], m[:], TWO_PI, t[:], ALU.mult, ALU.add)
    wrap(r)
    p.act(sin[:], r[:], AF.Sin)
    p.ts(r[:], r[:], np.pi / 2, None, op0=ALU.add)
    wrap(r)
    p.act(cos[:], r[:], AF.Sin)
    return cos, sin


def rope_tm(p, dst, src, cos, sin, H, ta, tb):
    cb = cos[:, None, :].to_broadcast([128, H, 32])
    sb_ = sin[:, None, :].to_broadcast([128, H, 32])
    t1, t2 = src[:, :, 0:32], src[:, :, 32:64]
    p.tt(ta, t1, cb, ALU.mult)
    p.tt(tb, t2, sb_, ALU.mult)
    p.tt(dst[:, :, 0:32], ta, tb, ALU.subtract)
    p.tt(ta, t2, cb, ALU.mult)
    p.tt(tb, t1, sb_, ALU.mult)
    p.tt(dst[:, :, 32:64], ta, tb, ALU.add)


TT_ = T + 128
NTT = NT + 1
E_IN = 6432


def ea_scratch(p, tag):
    S = {}
    S["dt"] = p.dram("ea_dt_" + tag, [128, NT * 32], F32).ap()
    S["qT"] = p.dram("ea_qT_" + tag, [8, 128, T], BF16).ap()
    S["kT"] = p.dram("ea_kT_" + tag, [2, 128, TT_], BF16).ap()
    S["v"] = p.dram("ea_v_" + tag, [TT_, 128], BF16).ap()
    S["xs"] = p.dram("ea_xs_" + tag, [T, 2048], BF16).ap()
    S["BT"] = p.dram("ea_BT_" + tag, [4, 128, T], BF16).ap()
    S["Btm"] = p.dram("ea_Btm_" + tag, [T, 512], BF16).ap()
    return S


def stage_ea1(p, cst_d, inv_d, pos_d, xh_d, w_in_d, conv_w_d, conv_b_d, z_d, ct_d, S):
    import os
    UPTO = int(os.environ.get("EA1_UPTO", "9"))
    p.begin()
    C = load_cst(p, cst_d, ["ident"])
    ident = C["ident"]
    cos, sin = rope_tables(p, pos_d, inv_d, NTT)
    xT = p.sb("xT", [128, KC, TT_], BF16)
    PS = [p.ps("P%d" % i, [128, 512]) for i in range(4)]
    PB = [p.ps("PB%d" % i, [128, 1024]) for i in range(2)]
    xt = [p.sb("xt0", [128, D], F32), p.sb("xt1", [128, D], F32)]
    for tt in range(NTT):
        x = xt[tt % 2]
        p.dma(x[:], xh_d[tt * 128:(tt + 1) * 128, :], q="sync" if tt % 2 else "gpsimd")
        transpose_to(p, xT, x, ident, KC, tt * 128, PS[0:2])
    if UPTO <= 1:
        p.end()
        return
    wst = [p.sb("wst%d" % i, [128, 4, 512], F32) for i in range(2)]
    wbf = [p.sb("wbf%d" % i, [128, KC, 512], BF16) for i in range(2)]
    ob = [p.sb("ob%d" % i, [128, 512], F32) for i in range(2)]

    def wview(c0, n):
        return w_in_d[:, c0:c0 + n].rearrange("(kc k) n -> k kc n", k=128)
    cnt = 0
    for n in range(4):
        wb = wbf[n % 2]
        load_w_bf16(p, wb, wview(n * 512, 512), wst, 512)
        for tt in range(1, NTT):
            ps, o = PS[cnt % 4], ob[cnt % 2]
            for kc in range(KC):
                p.mm(ps[:], xT[:, kc, tt * 128:(tt + 1) * 128], wb[:, kc, :], start=(kc == 0), stop=(kc == KC - 1))
            p.copy(o[:], ps[:], eng="scalar" if cnt % 2 else "vector")
            p.dma(z_d[(tt - 1) * 128:tt * 128, n * 512:(n + 1) * 512], o[:], q="scalar")
            cnt += 1
    if UPTO <= 2:
        p.end()
        return
    wb = wbf[0]
    load_w_bf16(p, wb[:, :, 0:32], wview(5120, 32), wst, 32)
    dtraw = p.sb("dtraw", [128, NT, 32], F32)
    for tt in range(1, NTT):
        ps = PS[cnt % 4]
        for kc in range(KC):
            p.mm(ps[:, 0:32], xT[:, kc, tt * 128:(tt + 1) * 128], wb[:, kc, 0:32], start=(kc == 0), stop=(kc == KC - 1))
        p.copy(dtraw[:, tt - 1, :], ps[:, 0:32])
        cnt += 1
    p.dma(S["dt"], dtraw[:].rearrange("p a b -> p (a b)"))
    if UPTO <= 3:
        p.end()
        return
    qr = [p.sb("qr%d" % i, [128, 8, 64], F32) for i in range(2)]
    ta = p.sb("ta", [128, 8, 32], F32)
    tb = p.sb("tb", [128, 8, 32], F32)
    qTb = [p.sb("qTb%d" % i, [128, 4, 128], BF16) for i in range(2)]
    for hc in range(2):
        wb = wbf[(hc + 1) % 2]
        load_w_bf16(p, wb, wview(5152 + hc * 512, 512), wst, 512)
        for tt in range(1, NTT):
            ps, q_, qt = PS[cnt % 4], qr[cnt % 2], qTb[cnt % 2]
            for kc in range(KC):
                p.mm(ps[:], xT[:, kc, tt * 128:(tt + 1) * 128], wb[:, kc, :], start=(kc == 0), stop=(kc == KC - 1))
            rope_tm(p, q_[:], ps[:].rearrange("p (h d) -> p h d", h=8), cos[:, tt, :], sin[:, tt, :], 8, ta[:], tb[:])
            pt = PS[(cnt + 2) % 4]
            qf = q_[:].rearrange("p h d -> p (h d)")
            for j in range(4):
                p.tr(pt[:, j * 128:(j + 1) * 128], qf[:, j * 128:(j + 1) * 128], ident[:])
            p.copy(qt[:], pt[:].rearrange("p (a b) -> p a b", a=4), eng="scalar")
            p.dma(S["qT"][hc * 4:(hc + 1) * 4, :, (tt - 1) * 128:tt * 128].rearrange("a p t -> p a t"), qt[:], q="scalar")
            cnt += 1
    if UPTO <= 4:
        p.end()
        return
    wb = wbf[1]
    load_w_bf16(p, wb[:, :, 0:256], wview(6176, 256), wst, 256)
    kr = p.sb("kr", [128, 2, 64], F32)
    ksb = p.sb("ksb", [128, 128], F32)
    kd = [p.sb("kd%d" % i, [128, 2, 2, 64], F32) for i in range(2)]
    kTb = [p.sb("kTb%d" % i, [128, 2, 128], BF16) for i in range(2)]
    vb = [p.sb("vb%d" % i, [128, 128], BF16) for i in range(2)]
    KVL = int(os.environ.get("EA1_KV", "9"))
    for tt in range(NTT):
        ps, kd_, kt_, v_ = PS[cnt % 4], kd[cnt % 2], kTb[cnt % 2], vb[cnt % 2]
        for kc in range(KC):
            p.mm(ps[:, 0:256], xT[:, kc, tt * 128:(tt + 1) * 128], wb[:, kc, 0:256], start=(kc == 0), stop=(kc == KC - 1))
        if KVL >= 2:
            p.copy(ksb[:], ps[:, 0:128], eng="scalar")
            rope_tm(p, kr[:], ksb[:].rearrange("p (h d) -> p h d", h=2), cos[:, tt, :], sin[:, tt, :], 2, ta[:, 0:2, :], tb[:, 0:2, :])
        if KVL >= 3:
            p.copy(kd_[:, :, 0, :], kr[:])
            p.copy(kd_[:, :, 1, :], kr[:])
        p.copy(v_[:], ps[:, 128:256], eng="scalar")
        if KVL >= 4:
            pt = PS[(cnt + 2) % 4]
            kf = kd_[:].rearrange("p a b d -> p (a b d)")
            for j in range(2):
                p.tr(pt[:, j * 128:(j + 1) * 128], kf[:, j * 128:(j + 1) * 128], ident[:])
            p.copy(kt_[:], pt[:, 0:256].rearrange("p (a b) -> p a b", a=2), eng="scalar")
        if KVL >= 5:
            p.dma(S["kT"][:, :, tt * 128:(tt + 1) * 128].rearrange("a p t -> p a t"), kt_[:], q="scalar")
        if KVL >= 1:
            p.dma(S["v"][tt * 128:(tt + 1) * 128, :], v_[:], q="scalar")
        cnt += 1
    if UPTO <= 5:
        p.end()
        return
    cw5 = p.sb("cw5", [5, 3072], F32)
    p.dma(cw5[0:4, :], conv_w_d)
    p.dma(cw5[4:5, :], conv_b_d.rearrange("(o c) -> o c", o=1))
    cwt = p.sb("cwt", [128, 24, 5], F32)
    for ch in range(24):
        p.tr(PS[0][:, ch * 5:(ch + 1) * 5], cw5[:, ch * 128:(ch + 1) * 128], ident[0:5, 0:5])
    p.copy(cwt[:], PS[0][:, 0:120].rearrange("p (a b) -> p a b", a=24))
    cw = cwt
    cb = cwt[:, :, 4]
    if UPTO <= 6:
        p.end()
        return
    wc = [p.sb("wc%d" % i, [128, KC, 128], BF16) for i in range(2)]
    u = [p.sb("u%d" % i, [128, TT_], F32) for i in range(2)]
    acc = [p.sb("acc%d" % i, [128, T], F32) for i in range(2)]
    xcb = [p.sb("xcb%d" % i, [128, T], BF16) for i in range(2)]
    xsb = [p.sb("xsb%d" % i, [128, NT, 128], BF16) for i in range(2)]
    for ch in range(24):
        w_, u_, a_, xb_, xs_ = wc[ch % 2], u[ch % 2], acc[ch % 2], xcb[ch % 2], xsb[ch % 2]
        load_w_bf16(p, w_, wview(2048 + ch * 128, 128), wst, 128)
        for gi, (t0, tn) in enumerate([(0, 512), (512, 512), (1024, 128)]):
            ps = PS[cnt % 4]
            for kc in range(KC):
                p.mm(ps[:, 0:tn], w_[:, kc, :], xT[:, kc, t0:t0 + tn], start=(kc == 0), stop=(kc == KC - 1))
            p.copy(u_[:, t0:t0 + tn], ps[:, 0:tn], eng="scalar" if cnt % 2 else "vector")
            cnt += 1
        p.ts(a_[:], u_[:, 128:TT_], cw[:, ch, 3:4], cw[:, ch, 4:5], op0=ALU.mult, op1=ALU.add)
        for k in range(3):
            sh = 3 - k
            p.stt(a_[:], u_[:, 128 - sh:TT_ - sh], cw[:, ch, k:k + 1], a_[:], ALU.mult, ALU.add)
        if ch < 16 or 16 <= ch < 20:
            p.act(a_[:], a_[:], AF.Silu)
            pb = PB[ch % 2]
            for j in range(NT):
                p.tr(pb[:, j * 128:(j + 1) * 128], a_[:, j * 128:(j + 1) * 128], ident[:])
            p.copy(xs_[:], pb[:].rearrange("p (a b) -> p a b", a=NT), eng="vector")
            if ch < 16:
                p.dma(S["xs"][:, ch * 128:(ch + 1) * 128].rearrange("(a p) c -> p a c", p=128), xs_[:], q="scalar")
            else:
                g = ch - 16
                p.dma(S["Btm"][:, g * 128:(g + 1) * 128].rearrange("(a p) c -> p a c", p=128), xs_[:], q="scalar")
                p.copy(xb_[:], a_[:], eng="gpsimd")
                p.dma(S["BT"][g], xb_[:], q="scalar")
        else:
            g = ch - 20
            p.act(xb_[:], a_[:], AF.Silu)
            p.dma(ct_d[g], xb_[:], q="scalar")
    p.end()


def stage_ea2(p, cst_d, dt_bias_d, a_log_d, d_skip_d, ct_d, S, y_d, e_d, hloc_d, dtot_d):
    p.begin()
    C = load_cst(p, cst_d, ["triu", "tril_s", "ones"])
    U, Lm, ones = C["triu"], C["tril_s"], C["ones"]
    Psm = p.ps("Psm", [128, 512])
    PX = p.ps("PX", [128, 512])
    PA = p.ps("PA", [128, 2048])
    xs = p.sb("xs", [128, NT, 2048], BF16)
    BT = p.sb("BT", [128, 4, T], BF16)
    CT = p.sb("CT", [128, 4, T], BF16)
    Btm = p.sb("Btm", [128, NT, 512], BF16)
    dtraw = p.sb("dtraw", [128, NT, 32], F32)
    p.dma(xs[:], S["xs"].rearrange("(a p) c -> p a c", p=128))
    p.dma(BT[:], S["BT"].rearrange("g n t -> n g t"), q="gpsimd")
    p.dma(CT[:], ct_d.rearrange("g n t -> n g t"), q="gpsimd")
    p.dma(Btm[:], S["Btm"].rearrange("(a p) c -> p a c", p=128))
    p.dma(dtraw[:].rearrange("p a b -> p (a b)"), S["dt"])
    rows = {}
    for nm, d_ in (("dtb", dt_bias_d), ("alog", a_log_d), ("dsk", d_skip_d)):
        rows[nm] = p.sb("row_" + nm, [128, 32], F32)
        p.dma(rows[nm][:], bcast_rows(d_, 32))
    A = p.sb("A", [128, 32], F32)
    p.act(A[:], rows["alog"][:], AF.Exp)
    p.ts(A[:], A[:], -1.0, None, op0=ALU.mult)
    sm = {k: p.sb("s_" + k, [128, 32], F32) for k in ["x", "ab", "e", "l", "dt", "a", "acs", "tot", "eloc", "acc", "dte", "cdec", "carry"]}
    Eall = p.sb("Eall", [128, NT, 32], F32)
    p.memset(sm["carry"][:], 0.0)
    H = p.sb("H", [128, 32, 64], F32)
    Hbf = p.sb("Hbf", [128, 2048], BF16)
    p.memset(H[:], 0.0)
    p.memset(Hbf[:], 0.0)
    xdt = p.sb("xdt", [128, 32, 64], BF16)
    xd2 = p.sb("xd2", [128, 32, 64], BF16)
    cbm = p.sb("cbm", [128, 4, 128], F32)
    rhsA = p.sb("rhsA", [128, 16, 128], F32)
    dec = p.sb("dec", [128, 16, 128], F32)
    G = p.sb("G", [128, 16, 128], BF16)
    ybuf = p.sb("ybuf", [128, 32, 64], F32)
    ytmp = p.sb("ytmp", [128, 16, 64], F32)
    Ht = p.sb("Ht", [128, 32, 64], F32)
    for c in range(NT):
        cs = slice(c * 128, (c + 1) * 128)
        x = sm["x"]
        p.tt(x[:], dtraw[:, c, :], rows["dtb"][:], ALU.add)
        p.stt(sm["ab"][:], x[:], -1.0, x[:], ALU.mult, ALU.max)
        p.act(sm["e"][:], sm["ab"][:], AF.Exp, scale=-1.0)
        p.act(sm["l"][:], sm["e"][:], AF.Ln, bias=1.0)
        p.stt(sm["dt"][:], x[:], 0.0, sm["l"][:], ALU.max, ALU.add)
        p.tt(sm["a"][:], sm["dt"][:], A[:], ALU.mult)
        p.mm(Psm[:, 0:32], U[:], sm["a"][:])
        p.mm(Psm[:, 32:64], ones[:], sm["a"][:])
        p.copy(sm["acs"][:], Psm[:, 0:32])
        p.copy(sm["tot"][:], Psm[:, 32:64])
        p.act(sm["eloc"][:], sm["acs"][:], AF.Exp)
        p.tt(sm["acc"][:], sm["acs"][:], sm["carry"][:], ALU.add)
        p.act(Eall[:, c, :], sm["acc"][:], AF.Exp)
        p.tt(sm["carry"][:], sm["carry"][:], sm["tot"][:], ALU.add)
        p.tt(sm["dte"][:], sm["tot"][:], sm["acs"][:], ALU.subtract)
        p.act(sm["dte"][:], sm["dte"][:], AF.Exp)
        p.act(sm["cdec"][:], sm["tot"][:], AF.Exp)
        p.tt(xdt[:], xs[:, c, :].rearrange("p (h d) -> p h d", h=32), sm["dt"][:, :, None].to_broadcast([128, 32, 64]), ALU.mult)
        p.tt(xd2[:], xdt[:], sm["dte"][:, :, None].to_broadcast([128, 32, 64]), ALU.mult)
        for g in range(4):
            p.mm(PX[:, g * 128:(g + 1) * 128], BT[:, g, cs], CT[:, g, cs])
        p.tt(cbm[:], PX[:].rearrange("p (g l) -> p g l", g=4), U[:, None, :].to_broadcast([128, 4, 128]), ALU.mult)
        for half in range(2):
            hs = slice(half * 16, (half + 1) * 16)
            p.tt(rhsA[:], U[:, None, :].to_broadcast([128, 16, 128]), sm["a"][:, hs, None].to_broadcast([128, 16, 128]), ALU.mult)
            rf = rhsA[:].rearrange("p h l -> p (h l)")
            for j in range(4):
                p.mm(PA[:, j * 512:(j + 1) * 512], Lm[:], rf[:, j * 512:(j + 1) * 512])
            p.act(dec[:].rearrange("p h l -> p (h l)"), PA[:], AF.Exp)
            p.tt(G[:].rearrange("p (g r) l -> p g r l", g=2), dec[:].rearrange("p (g r) l -> p g r l", g=2),
                 cbm[:, half * 2:half * 2 + 2, None, :].to_broadcast([128, 2, 8, 128]), ALU.mult)
            for hh in range(16):
                p.mm(PA[:, hh * 64:(hh + 1) * 64], G[:, hh, :], xdt[:, half * 16 + hh, :])
            for g in range(2):
                gg = half * 2 + g
                p.mm(PA[:, 1024 + g * 512:1024 + (g + 1) * 512], CT[:, gg, cs], Hbf[:, gg * 512:(gg + 1) * 512])
            yh = ybuf[:, hs, :]
            p.tt(yh, PA[:, 1024:2048].rearrange("p (h d) -> p h d", h=16), sm["eloc"][:, hs, None].to_broadcast([128, 16, 64]), ALU.mult)
            p.tt(yh, yh, PA[:, 0:1024].rearrange("p (h d) -> p h d", h=16), ALU.add)
            p.tt(ytmp[:], xs[:, c, half * 1024:(half + 1) * 1024].rearrange("p (h d) -> p h d", h=16),
                 rows["dsk"][:, hs, None].to_broadcast([128, 16, 64]), ALU.mult)
            p.tt(yh, yh, ytmp[:], ALU.add)
        p.dma(y_d[cs, :], ybuf[:].rearrange("p h d -> p (h d)"))
        x2f = xd2[:].rearrange("p h d -> p (h d)")
        for g in range(4):
            p.mm(PA[:, g * 512:(g + 1) * 512], Btm[:, c, g * 128:(g + 1) * 128], x2f[:, g * 512:(g + 1) * 512])
        p.tt(Ht[:], H[:], sm["cdec"][:, :, None].to_broadcast([128, 32, 64]), ALU.mult)
        p.tt(H[:], Ht[:], PA[:].rearrange("p (h d) -> p h d", h=32), ALU.add)
        p.copy(Hbf[:], H[:].rearrange("p h d -> p (h d)"), eng="scalar")
    p.dma(hloc_d, H[:].rearrange("p h d -> p (h d)"))
    p.act(sm["cdec"][:], sm["carry"][:], AF.Exp)
    p.dma(dtot_d, sm["cdec"][:])
    p.dma(e_d.rearrange("(a p) h -> p a h", p=128), Eall[:])
    p.end()


def stage_ea3(p, cst_d, sinks_d, mask_d, S, yatt_d):
    p.begin()
    C = load_cst(p, cst_d, ["ident"], bf=["ident"])
    identb = C["ident_bf"]
    qT = p.sb("qT", [128, 8, T], BF16)
    kT = p.sb("kT", [128, 2, TT_], BF16)
    v = p.sb("v", [128, NTT, 128], BF16)
    p.dma(qT[:], S["qT"].rearrange("a p t -> p a t"))
    p.dma(kT[:], S["kT"].rearrange("a p t -> p a t"), q="gpsimd")
    p.dma(v[:], S["v"].rearrange("(a p) d -> p a d", p=128), q="gpsimd")
    msk = p.sb("msk", [128, 2, 256], F32)
    p.dma(msk[:], mask_d.rearrange("a p s -> p a s"))
    snk = p.sb("snk", [128, 16], F32)
    p.dma(snk[:], bcast_rows(sinks_d, 16))
    PL = [p.ps("PL%d" % i, [128, 2, 512]) for i in range(2)]
    PT = [p.ps("PTa%d" % i, [128, 1024], BF16) for i in range(2)]
    PO = p.ps("PO", [128, 1024])
    lg = [p.sb("lg%d" % i, [128, 2, 256], F32) for i in range(2)]
    P_ = [p.sb("Pp%d" % i, [128, 2, 256], BF16) for i in range(2)]
    PTs = [p.sb("PTs%d" % i, [128, 4, 128], BF16) for i in range(2)]
    sm = [{k: p.sb("a_%s%d" % (k, i), [128, 2], F32) for k in ["mx", "m", "nm", "rs", "sk"]} for i in range(2)]
    rden = p.sb("rden", [128, 16], F32)
    yo = [p.sb("yo%d" % i, [128, 16, 64], F32) for i in range(2)]
    scale = 64 ** -0.5
    it = 0
    for blk in range(NT):
        mk = msk[:, 0 if blk == 0 else 1, :]
        for pr in range(8):
            kh = pr // 4
            pl, l_, pp, pt, pts, s_ = PL[it % 2], lg[it % 2], P_[it % 2], PT[it % 2], PTs[it % 2], sm[it % 2]
            it += 1
            for hh in range(2):
                rs_ = slice(hh * 64, (hh + 1) * 64)
                p.mm(pl[:, hh, 0:256], qT[rs_, pr, blk * 128:(blk + 1) * 128], kT[rs_, kh, blk * 128:blk * 128 + 256])
            p.stt(l_[:], pl[:, :, 0:256], scale, mk[:, None, :].to_broadcast([128, 2, 256]), ALU.mult, ALU.add)
            p.reduce(s_["mx"][:], l_[:], ALU.max)
            p.tt(s_["m"][:], s_["mx"][:], snk[:, 2 * pr:2 * pr + 2], ALU.max)
            p.ts(s_["nm"][:], s_["m"][:], -1.0, None, op0=ALU.mult)
            for hh in range(2):
                p.act(pp[:, hh, :], l_[:, hh, :], AF.Exp, bias=s_["nm"][:, hh:hh + 1], accum_out=s_["rs"][:, hh:hh + 1])
            p.tt(s_["sk"][:], snk[:, 2 * pr:2 * pr + 2], s_["m"][:], ALU.subtract)
            p.act(s_["sk"][:], s_["sk"][:], AF.Exp)
            p.tt(s_["sk"][:], s_["sk"][:], s_["rs"][:], ALU.add)
            p.recip(rden[:, 2 * pr:2 * pr + 2], s_["sk"][:])
            for hh in range(2):
                for kt in range(2):
                    p.tr(pt[:, (hh * 2 + kt) * 128:(hh * 2 + kt + 1) * 128], pp[:, hh, kt * 128:(kt + 1) * 128], identb[:])
            p.copy(pts[:], pt[:, 0:512].rearrange("p (a b) -> p a b", a=4), eng="scalar")
            for hh in range(2):
                for kt in range(2):
                    p.mm(PO[:, (2 * pr + hh) * 64:(2 * pr + hh + 1) * 64], pts[:, hh * 2 + kt, :],
                         v[:, blk + kt, kh * 64:(kh + 1) * 64], start=(kt == 0), stop=(kt == 1))
        y_ = yo[blk % 2]
        p.tt(y_[:], PO[:].rearrange("p (h d) -> p h d", h=16), rden[:, :, None].to_broadcast([128, 16, 64]), ALU.mult)
        p.dma(yatt_d[blk * 128:(blk + 1) * 128, :], y_[:].rearrange("p h d -> p (h d)"))
    p.end()


def stage_eb1(p, cst_d, y_d, z_d, yatt_d, ct_d, e_d, hprev_d, dprev_d, ssm_norm_d, mixT_d):
    p.begin()
    C = load_cst(p, cst_d, ["ident"])
    ident = C["ident"]
    PA = p.ps("PA", [128, 2048])
    PB = [p.ps("PB%d" % i, [128, 1024]) for i in range(2)]
    H = p.sb("H", [128, 32, 64], F32)
    Ht = p.sb("Ht", [128, 32, 64], F32)
    Hbf = p.sb("Hbf", [128, 2048], BF16)
    Sj = [p.sb("Sj%d" % i, [128, 32, 64], F32) for i in range(2)]
    Dj = [p.sb("Dj%d" % i, [128, 32], F32) for i in range(2)]
    p.memset(H[:], 0.0)
    for j in range(7):
        s_, d_ = Sj[j % 2], Dj[j % 2]
        p.dma(s_[:].rearrange("p h d -> p (h d)"), hprev_d[j])
        p.dma(d_[:], dprev_d[j], q="gpsimd")
        p.tt(Ht[:], H[:], d_[:, :, None].to_broadcast([128, 32, 64]), ALU.mult)
        p.tt(H[:], Ht[:], s_[:], ALU.add)
    p.copy(Hbf[:], H[:].rearrange("p h d -> p (h d)"))
    CT = p.sb("CT", [128, 4, T], BF16)
    p.dma(CT[:], ct_d.rearrange("g n t -> n g t"), q="gpsimd")
    E = p.sb("E", [128, NT, 32], F32)
    p.dma(E[:], e_d.rearrange("(a p) h -> p a h", p=128))
    nrm = p.sb("nrm", [128, 2048], F32)
    p.dma(nrm[:], bcast_rows(ssm_norm_d, 2048))
    yl = [p.sb("yl%d" % i, [128, 2048], F32) for i in range(2)]
    zt = [p.sb("zt%d" % i, [128, 2048], F32) for i in range(2)]
    mix = [p.sb("mix%d" % i, [128, 3072], F32) for i in range(2)]
    junk = p.sb("junk", [128, 512], F32)
    ss = [p.sb("ss%d" % i, [128, 4], F32) for i in range(2)]
    mT = [p.sb("mT%d" % i, [128, 24, 128], BF16) for i in range(2)]
    for c in range(NT):
        cs = slice(c * 128, (c + 1) * 128)
        y_, z_, m_, s_, t_ = yl[c % 2], zt[c % 2], mix[c % 2], ss[c % 2], mT[c % 2]
        p.dma(y_[:], y_d[cs, :])
        p.dma(z_[:], z_d[cs, :], q="gpsimd")
        p.dma(m_[:, 2048:3072], yatt_d[cs, :])
        for g in range(4):
            p.mm(PA[:, g * 512:(g + 1) * 512], CT[:, g, cs], Hbf[:, g * 512:(g + 1) * 512])
        yv = m_[:, 0:2048]
        p.tt(yv.rearrange("p (h d) -> p h d", h=32), PA[:].rearrange("p (h d) -> p h d", h=32),
             E[:, c, :, None].to_broadcast([128, 32, 64]), ALU.mult)
        p.tt(yv, yv, y_[:], ALU.add)
        p.act(z_[:], z_[:], AF.Silu)
        p.tt(yv, yv, z_[:], ALU.mult)
        for g in range(4):
            p.act(junk[:], m_[:, g * 512:(g + 1) * 512], AF.Square, accum_out=s_[:, g:g + 1])
        p.ts(s_[:], s_[:], 1.0 / 512, EPS, op0=ALU.mult, op1=ALU.add)
        p.act(s_[:], s_[:], AF.Sqrt)
        p.recip(s_[:], s_[:])
        p.tt(yv.rearrange("p (g d) -> p g d", g=4), yv.rearrange("p (g d) -> p g d", g=4),
             s_[:, :, None].to_broadcast([128, 4, 512]), ALU.mult)
        p.tt(yv, yv, nrm[:], ALU.mult, eng="gpsimd")
        for g3 in range(3):
            pb = PB[g3 % 2]
            for j in range(8):
                k = g3 * 8 + j
                p.tr(pb[:, j * 128:(j + 1) * 128], m_[:, k * 128:(k + 1) * 128], ident[:])
            p.copy(t_[:, g3 * 8:(g3 + 1) * 8, :], pb[:].rearrange("p (a b) -> p a b", a=8), eng="scalar" if g3 % 2 else "vector")
        p.dma(mixT_d[:, :, cs].rearrange("a p t -> p a t"), t_[:], q="scalar")
    p.end()


def stage_proj_ln(p, mixT_d, nk, w_d, x_d, ln_g, ln_b, out_d):
    p.begin()
    mT = p.sb("mT", [128, nk, T], BF16)
    p.dma(mT[:], mixT_d.rearrange("a p t -> p a t"))
    g_rows = p.sb("g_rows", [128, D], F32)
    b_rows = p.sb("b_rows", [128, D], F32)
    p.dma(g_rows[:], bcast_rows(ln_g, D), q="gpsimd")
    p.dma(b_rows[:], bcast_rows(ln_b, D), q="gpsimd")
    vb = p.sb("vb", [128, NT, D], F32)
    p.dma(vb[:], x_d.rearrange("(a p) d -> p a d", p=128))
    wst = [p.sb("wst%d" % i, [128, 4, 512], F32) for i in range(2)]
    wbf = [p.sb("wbf%d" % i, [128, nk, 512], BF16) for i in range(2)]
    PS = [p.ps("P%d" % i, [128, 512]) for i in range(4)]
    cnt = 0
    for n in range(4):
        wb = wbf[n % 2]
        load_w_bf16(p, wb, w_d[:, n * 512:(n + 1) * 512].rearrange("(kc k) n -> k kc n", k=128), wst, 512)
        for tt in range(NT):
            ps = PS[cnt % 4]
            cnt += 1
            for kc in range(nk):
                p.mm(ps[:], mT[:, kc, tt * 128:(tt + 1) * 128], wb[:, kc, :], start=(kc == 0), stop=(kc == nk - 1))
            dst = vb[:, tt, n * 512:(n + 1) * 512]
            p.stt(dst, dst, ALPHA, ps[:], ALU.mult, ALU.add)
    lns = ln_scratch(p)
    for tt in range(NT):
        layer_norm_tile(p, vb[:, tt, :], vb[:, tt, :], g_rows[:], b_rows[:], lns[tt % 2])
        p.dma(out_d[tt * 128:(tt + 1) * 128, :], vb[:, tt, :], q="scalar")
    p.end()


O_IN = 4752
NKEY = 8192
NST = NKEY // 128
NIT = 20
TOPK = 256


def stage_op(p, cst_d, inv_d, pos_d, x_d, w_in_d, kv_norm_d, w_uk_d, kiT_d, kcatT_d, ckv_d, qcatT_d, qiT_d, wi_d, mode, parts="all"):
    p.begin()
    C = load_cst(p, cst_d, ["ident"])
    ident = C["ident"]
    cos, sin = rope_tables(p, pos_d, inv_d, NT)
    BFM = mode == "bf"
    DOK = parts in ("all", "k")
    DOQ = parts in ("all", "q")
    xT = p.sb("xT", [128, KC, T], BF16) if BFM else None
    xTf = None if BFM else p.sb("xTf", [128, KC, T], F32)
    PS = [p.ps("P%d" % i, [128, 512]) for i in range(6)]
    xt = [p.sb("xt0", [128, D], F32), p.sb("xt1", [128, D], F32)]
    for tt in range(NT):
        x = xt[tt % 2]
        p.dma(x[:], x_d[tt * 128:(tt + 1) * 128, :], q="sync" if tt % 2 else "gpsimd")
        for g in range(0, KC, 4):
            ps = PS[(g // 4) % 2]
            for j in range(4):
                p.tr(ps[:, j * 128:(j + 1) * 128], x[:, (g + j) * 128:(g + j + 1) * 128], ident[:])
            pv = ps[:].rearrange("p (a b) -> p a b", a=4)
            if BFM:
                p.copy(xT[:, g:g + 4, tt * 128:(tt + 1) * 128], pv, eng="scalar" if (g // 4) % 2 else "vector")
            else:
                p.copy(xTf[:, g:g + 4, tt * 128:(tt + 1) * 128], pv, eng="scalar" if (g // 4) % 2 else "vector")
    wf = None if BFM else p.sb("wf", [128, KC, 512], F32)
    wst = [p.sb("wst%d" % i, [128, 4, 512] if BFM else [128, 1], F32) for i in range(2)]
    wbf = [p.sb("wbf%d" % i, [128, KC, 512] if BFM else [128, 1], BF16) for i in range(2)]
    cnt = 0
    wc = [p.sb("wc%d" % i, [128, KC, 128] if BFM else [128, 1], BF16) for i in range(2)]
    wuk = [p.sb("wuk%d" % i, [128, 1, 512] if BFM else [128, 1], BF16) for i in range(2)]
    qn = [p.sb("qn%d" % i, [128, 512] if BFM else [128, 1], BF16) for i in range(2)]
    ql = [p.sb("ql%d" % i, [128, 4, 512] if BFM else [128, 1], BF16) for i in range(2)]
    for h in range(16 if (BFM and DOQ) else 0):
        w_, uk_ = wc[h % 2], wuk[h % 2]
        load_w_bf16(p, w_, w_in_d[:, h * 192:h * 192 + 128].rearrange("(kc k) n -> k kc n", k=128), wst, 128)
        load_w_bf16(p, uk_, w_uk_d[h].rearrange("(a k) n -> k a n", a=1), wst, 512)
        for tg in range(2):
            ps, q_, l_ = PS[cnt % 6], qn[cnt % 2], ql[cnt % 2]
            cnt += 1
            for kc in range(KC):
                p.mm(ps[:], w_[:, kc, :], xT[:, kc, tg * 512:(tg + 1) * 512], start=(kc == 0), stop=(kc == KC - 1))
            p.copy(q_[:], ps[:], eng="scalar")
            for rc in range(4):
                ps2 = PS[cnt % 6]
                cnt += 1
                p.mm(ps2[:], uk_[:, 0, rc * 128:(rc + 1) * 128], q_[:])
                p.copy(l_[:, rc, :], ps2[:], eng="scalar" if rc % 2 else "vector")
            p.dma(qcatT_d[0:512, h, tg * 512:(tg + 1) * 512].rearrange("(rc r) t -> r rc t", r=128), l_[:], q="scalar")
    qr = [p.sb("qr%d" % i, [128, 8, 64], F32) for i in range(2)]
    ta = p.sb("ta", [128, 8, 32], F32)
    tb = p.sb("tb", [128, 8, 32], F32)
    qTb = [p.sb("qTb%d" % i, [128, 4, 128] if BFM else [128, 1], BF16) for i in range(2)]
    qTf = [p.sb("qTf%d" % i, [128, 4, 128] if not BFM else [128, 1], F32) for i in range(2)]
    for which in (([0] if BFM else [1]) if DOQ else []):
        for hc in range(2):
            wb = wbf[(which * 2 + hc) % 2]
            if which == 0:
                for hh in range(8):
                    hd = hc * 8 + hh
                    load_w_bf16(p, wb[:, :, hh * 64:(hh + 1) * 64],
                                w_in_d[:, hd * 192 + 128:hd * 192 + 192].rearrange("(kc k) n -> k kc n", k=128), wst, 64, g=4)
            else:
                p.dma(wf[:], w_in_d[:, 3648 + hc * 512:3648 + (hc + 1) * 512].rearrange("(kc k) n -> k kc n", k=128))
            for tt in range(NT):
                ps, q_, qt = PS[cnt % 6], qr[cnt % 2], (qTb if which == 0 else qTf)[cnt % 2]
                cnt += 1
                for kc in range(KC):
                    if which == 0:
                        p.mm(ps[:], xT[:, kc, tt * 128:(tt + 1) * 128], wb[:, kc, :], start=(kc == 0), stop=(kc == KC - 1))
                    else:
                        p.mm(ps[:], xTf[:, kc, tt * 128:(tt + 1) * 128], wf[:, kc, :], start=(kc == 0), stop=(kc == KC - 1))
                rope_tm(p, q_[:], ps[:].rearrange("p (h d) -> p h d", h=8), cos[:, tt, :], sin[:, tt, :], 8, ta[:], tb[:])
                pt = PS[cnt % 6]
                cnt += 1
                qf = q_[:].rearrange("p h d -> p (h d)")
                for j in range(4):
                    p.tr(pt[:, j * 128:(j + 1) * 128], qf[:, j * 128:(j + 1) * 128], ident[:])
                p.copy(qt[:], pt[:].rearrange("p (a b) -> p a b", a=4), eng="scalar")
                ts_ = slice(tt * 128, (tt + 1) * 128)
                for hh2 in range(2):
                    src = qt[hh2 * 64:(hh2 + 1) * 64, :, :]
                    if which == 0:
                        dst = qcatT_d[512:576, hc * 8 + hh2:hc * 8 + 8:2, ts_]
                    else:
                        dst = qiT_d[:, hc * 8 + hh2:hc * 8 + 8:2, ts_]
                    p.dma(dst, src, q="scalar")
    wb = wbf[0]
    if BFM:
        load_w_bf16(p, wb, w_in_d[:, 3072:3584].rearrange("(kc k) n -> k kc n", k=128), wst, 512)
    nrm = p.sb("nrm", [128, 512], F32)
    if BFM:
        p.dma(nrm[:], bcast_rows(kv_norm_d, 512))
    cf = [p.sb("cf%d" % i, [128, 512] if BFM else [128, 1], F32) for i in range(2)]
    cbf = [p.sb("cbf%d" % i, [128, 512] if BFM else [128, 1], BF16) for i in range(2)]
    cT = [p.sb("cT%d" % i, [128, 4, 128] if BFM else [128, 1], BF16) for i in range(2)]
    ss = [p.sb("ss%d" % i, [128, 1], F32) for i in range(2)]
    junk = p.sb("junk", [128, 512], F32)
    for tt in range(NT if (BFM and DOK) else 0):
        ps, c_, cb_, ct_, s_ = PS[cnt % 6], cf[tt % 2], cbf[tt % 2], cT[tt % 2], ss[tt % 2]
        cnt += 1
        ts_ = slice(tt * 128, (tt + 1) * 128)
        for kc in range(KC):
            p.mm(ps[:], xT[:, kc, ts_], wb[:, kc, :], start=(kc == 0), stop=(kc == KC - 1))
        p.act(junk[:], ps[:], AF.Square, accum_out=s_[:])
        p.ts(s_[:], s_[:], 1.0 / 512, EPS, op0=ALU.mult, op1=ALU.add)
        p.act(s_[:], s_[:], AF.Sqrt)
        p.recip(s_[:], s_[:])
        p.stt(c_[:], ps[:], s_[:, 0:1], nrm[:], ALU.mult, ALU.mult)
        p.copy(cb_[:], c_[:], eng="scalar")
        p.dma(ckv_d[ts_, :], cb_[:], q="scalar")
        pt = PS[cnt % 6]
        cnt += 1
        for j in range(4):
            p.tr(pt[:, j * 128:(j + 1) * 128], c_[:, j * 128:(j + 1) * 128], ident[:])
        p.copy(ct_[:], pt[:].rearrange("p (a b) -> p a b", a=4))
        p.dma(kcatT_d[0:512, ts_].rearrange("(rc r) t -> r rc t", r=128), ct_[:], q="scalar")
    wb = wbf[1]
    if BFM:
        load_w_bf16(p, wb[:, :, 0:64], w_in_d[:, 3584:3648].rearrange("(kc k) n -> k kc n", k=128), wst, 64)
    else:
        p.dma(wf[:, :, 0:80], w_in_d[:, 4672:4752].rearrange("(kc k) n -> k kc n", k=128))
    k2 = [p.sb("k2_%d" % i, [128, 2, 64], F32) for i in range(2)]
    k2T = [p.sb("k2T_%d" % i, [128, 128] if BFM else [128, 1], BF16) for i in range(2)]
    k2Tf = [p.sb("k2Tf_%d" % i, [128, 128] if not BFM else [128, 1], F32) for i in range(2)]
    wis = [p.sb("wis%d" % i, [128, 16], F32) for i in range(2)]
    for tt in range(NT if (DOK or not BFM) else 0):
        ps, k_, kt_, w_, ktf_ = PS[cnt % 6], k2[tt % 2], k2T[tt % 2], wis[tt % 2], k2Tf[tt % 2]
        cnt += 1
        ts_ = slice(tt * 128, (tt + 1) * 128)
        if BFM:
            for kc in range(KC):
                p.mm(ps[:, 0:64], xT[:, kc, ts_], wb[:, kc, 0:64], start=(kc == 0), stop=(kc == KC - 1))
            p.copy(k_[:, 1, :], ps[:, 0:64], eng="scalar")
            rope_tm(p, k_[:, 0:1, :], k_[:, 1:2, :], cos[:, tt, :], sin[:, tt, :], 1, ta[:, 0:1, :], tb[:, 0:1, :])
            pt = PS[cnt % 6]
            cnt += 1
            p.tr(pt[0:64, 0:128], k_[:, 0, :], ident[:])
            p.copy(kt_[0:64, :], pt[0:64, 0:128], eng="scalar")
            p.dma(kcatT_d[512:576, ts_], kt_[0:64, :], q="scalar")
        else:
            for kc in range(KC):
                p.mm(ps[:, 0:80], xTf[:, kc, ts_], wf[:, kc, 0:80], start=(kc == 0), stop=(kc == KC - 1))
            p.copy(k_[:, 1, :], ps[:, 0:64], eng="scalar")
            rope_tm(p, k_[:, 0:1, :], k_[:, 1:2, :], cos[:, tt, :], sin[:, tt, :], 1, ta[:, 0:1, :], tb[:, 0:1, :])
            if DOQ:
                p.ts(w_[:], ps[:, 64:80], 1.0 / 32, None, op0=ALU.mult)
                p.dma(wi_d[ts_, :], w_[:], q="scalar")
            if DOK:
                pt = PS[cnt % 6]
                cnt += 1
                p.tr(pt[0:64, 0:128], k_[:, 0, :], ident[:])
                p.copy(ktf_[0:64, :], pt[0:64, 0:128])
                p.dma(kiT_d[:, ts_], ktf_[0:64, :], q="scalar")
    p.end()


def stage_oq(p, cst_d, tqm_d, kiT_d, kcatT_d, ckv_d, qcatT_d, qiT_d, wi_d, w_uv_d, oT_d, nqt=NT, nst_of=None):
    p.begin()
    C = load_cst(p, cst_d, ["ident", "iota", "ones"], bf=["ident", "ones"])
    identb, onesb, iota = C["ident_bf"], C["ones_bf"], C["iota"]
    I4 = p.sb("I4", [128, 4, 128], BF16)
    for j in range(4):
        p.copy(I4[:, j, :], identb[:])
    iota512 = p.sb("iota512", [128, 4, 128], F32)
    for j in range(4):
        p.ts(iota512[:, j, :], iota[:], float(j * 128), None, op0=ALU.add)
    io5 = iota512[:].rearrange("p a b -> p (a b)")
    tqm = p.sb("tqm", [128, NT, 16], F32)
    p.dma(tqm[:], tqm_d)
    PLT = [p.ps("PLT%d" % i, [128, 512]) for i in range(2)]
    POT = p.ps("POT", [128, 4, 512])
    PD = p.ps("PD", [128, 512])
    PM = p.ps("PM", [128, 512])
    kich = [p.sb("kich%d" % i, [64, 512], F32) for i in range(2)]
    wuv = p.sb("wuv", [128, 16, 4, 128], BF16)
    wst = [p.sb("wst%d" % i, [128, 4, 128], F32) for i in range(2)]
    for h in range(16):
        s_ = wst[h % 2]
        p.dma(s_[:], w_uv_d[h].rearrange("(rc r) v -> r rc v", r=128))
        p.copy(wuv[:, h, :, :], s_[:], eng="scalar" if h % 2 else "gpsimd")
    SG = 8
    kc_ = [p.sb("kcs%d" % i, [128, 5, SG * 128], BF16) for i in range(2)]
    cv_ = [p.sb("cvs%d" % i, [128, SG, 512], BF16) for i in range(2)]
    qc = [p.sb("qc%d" % i, [128, 5, 16, 128], BF16) for i in range(2)]
    qi = [p.sb("qi%d" % i, [64, 16, 128], F32) for i in range(2)]
    wi = [p.sb("wi%d" % i, [128, 16], F32) for i in range(2)]
    sc = p.sb("sc", [128, NKEY], F32)
    mb = p.sb("mb", [128, NKEY], BF16)
    junk = mb
    rl = [p.sb("rl%d" % i, [128, 512], F32) for i in range(2)]
    acc = p.sb("acc", [128, 512], F32)
    cm = p.sb("cm", [128, 512], F32)
    mn16 = p.sb("mn16", [128, 16], F32)
    b = {k: p.sb("b_" + k, [128, 1], F32) for k in ["lo", "hi", "mid", "hs", "cnt", "ge", "dl", "dh"]}
    PTt = [p.sb("PTt%d" % i, [128, 512], BF16) for i in range(2)]
    OTn = p.sb("OTn", [128, 4, 512], BF16)
    rrow = p.sb("rrow", [1, 512], F32)
    rrowb = p.sb("rrowb", [1, 512], BF16)
    rdb = p.sb("rdb", [128, 512], F32)
    oTt = [p.sb("oTt%d" % i, [128, 128], BF16) for i in range(2)]
    scale = 192 ** -0.5
    it = 0
    ld = 0
    for qt in range(nqt):
        nst_q = NST if nst_of is None else nst_of[qt]
        nck = nst_q // 4
        nky = nst_q * 128
        q_, qi_, wi_ = qc[qt % 2], qi[qt % 2], wi[qt % 2]
        ts_ = slice(qt * 128, (qt + 1) * 128)
        for ch in range(4):
            p.dma(q_[:, ch, :, :], qcatT_d[ch * 128:(ch + 1) * 128, :, ts_], q="gpsimd")
        p.dma(q_[0:64, 4, :, :], qcatT_d[512:576, :, ts_], q="gpsimd")
        p.dma(qi_[:], qiT_d[:, :, ts_], q="gpsimd")
        p.dma(wi_[:], wi_d[ts_, :], q="gpsimd")
        for ck in range(nck):
            kk = kich[ck % 2]
            p.dma(kk[:], kiT_d[:, ck * 512:(ck + 1) * 512], q="gpsimd")
            for h in range(16):
                ps, r_ = PLT[it % 2], rl[it % 2]
                it += 1
                p.mm(ps[:], qi_[:, h, :], kk[:])
                p.act(r_[:], ps[:], AF.Relu)
                if h == 0:
                    p.ts(acc[:], r_[:], wi_[:, 0:1], None, op0=ALU.mult)
                else:
                    p.stt(acc[:], r_[:], wi_[:, h:h + 1], acc[:], ALU.mult, ALU.add)
            p.reduce(mn16[:, ck:ck + 1], acc[:], ALU.min)
            p.ts(cm[:], io5, tqm[:, qt, ck:ck + 1], -1e30, op0=ALU.is_gt, op1=ALU.mult)
            p.tt(sc[:, ck * 512:(ck + 1) * 512], acc[:], cm[:], ALU.add)
        p.reduce(b["lo"][:], mn16[:, 0:nck], ALU.min)
        p.reduce(b["hi"][:], sc[:, 0:nky], ALU.max)
        for _ in range(NIT):
            p.ts(b["hs"][:], b["hi"][:], 0.5, None, op0=ALU.mult)
            p.stt(b["mid"][:], b["lo"][:], 0.5, b["hs"][:], ALU.mult, ALU.add)
            p.ts(junk[:, 0:nky], sc[:, 0:nky], b["mid"][:, 0:1], 0.0, op0=ALU.is_ge, op1=ALU.add, accum_out=b["cnt"][:])
            p.ts(b["ge"][:], b["cnt"][:], float(TOPK), None, op0=ALU.is_ge)
            p.tt(b["dl"][:], b["mid"][:], b["lo"][:], ALU.subtract)
            p.tt(b["dh"][:], b["hi"][:], b["mid"][:], ALU.subtract)
            p.stt(b["lo"][:], b["dl"][:], b["ge"][:, 0:1], b["lo"][:], ALU.mult, ALU.add)
            p.stt(b["hi"][:], b["dh"][:], b["ge"][:, 0:1], b["mid"][:], ALU.mult, ALU.add)
        p.ts(mb[:, 0:nky], sc[:, 0:nky], b["lo"][:, 0:1], -30000.0, op0=ALU.is_lt, op1=ALU.mult)
        for hg in range(4):
            hs = slice(hg * 4, (hg + 1) * 4)
            for st in range(nst_q):
                if st % SG == 0:
                    k_, c_ = kc_[ld % 2], cv_[ld % 2]
                    ld += 1
                    ks = slice(st * 128, (st + SG) * 128)
                    p.dma(k_[:, 0:4, :], kcatT_d[0:512, ks].rearrange("(c r) s -> r c s", r=128), q="sync")
                    p.dma(k_[0:64, 4, :], kcatT_d[512:576, ks], q="sync")
                    p.dma(c_[:], ckv_d[ks, :].rearrange("(a s) r -> s a r", s=128), q="sync")
                so = (st % SG) * 128
                ps, pt_ = PLT[it % 2], PTt[it % 2]
                it += 1
                for ch in range(4):
                    p.mm(ps[:], k_[:, ch, so:so + 128], q_[:, ch, hs, :], start=(ch == 0), stop=False)
                p.mm(ps[:], k_[0:64, 4, so:so + 128], q_[0:64, 4, hs, :], start=False, stop=False)
                p.mm(ps[:], mb[:, st * 128:(st + 1) * 128], I4[:], start=False, stop=True)
                p.act(pt_[:], ps[:], AF.Exp, scale=scale)
                for rc in range(4):
                    p.mm(POT[:, rc, :], c_[:, st % SG, rc * 128:(rc + 1) * 128], pt_[:], start=(st == 0), stop=(st == nst_q - 1))
                p.mm(PD[0:1, :], onesb[:, 0:1], pt_[:], start=(st == 0), stop=(st == nst_q - 1))
            p.copy(rrow[:], PD[0:1, :])
            p.recip(rrow[:], rrow[:])
            p.mm(PM[:], C["ones"][0:1, :], rrow[:])
            p.copy(rdb[:], PM[:], eng="scalar")
            for rc in range(4):
                p.tt(OTn[:, rc, :], POT[:, rc, :], rdb[:], ALU.mult)
            for hh in range(4):
                h = hg * 4 + hh
                o_ = oTt[h % 2]
                for rc in range(4):
                    p.mm(PM[:, 0:128], wuv[:, h, rc, :], OTn[:, rc, hh * 128:(hh + 1) * 128], start=(rc == 0), stop=(rc == 3))
                p.copy(o_[:], PM[:, 0:128], eng="scalar")
                p.dma(oT_d[h, :, ts_], o_[:], q="scalar")
    p.end()


NPDT = {F32: np.float32, I32: np.int32}


def _mk(nc):
    def din(name, shape, dt=F32):
        return nc.dram_tensor(name, list(shape), dt, kind="ExternalInput").ap()

    def dout(name, shape, dt=F32):
        return nc.dram_tensor(name, list(shape), dt, kind="ExternalOutput").ap()
    return din, dout


NCST = 128 * len(CST_NAMES)


def build_ea(stages=(1, 2, 3)):
    nc = bass.Bass("TRN2", target_bir_lowering=False)
    din, dout = _mk(nc)
    cst = din("cst", [128, NCST]); inv = din("inv", [128, 32]); pos = din("pos", [128, NTT], I32)
    xh = din("xh", [TT_, D]); w_in = din("w_in", [D, E_IN]); conv_w = din("conv_w", [4, 3072]); conv_b = din("conv_b", [3072])
    dt_bias = din("dt_bias", [32]); a_log = din("a_log", [32]); d_skip = din("d_skip", [32]); sinks = din("sinks", [16])
    mask = din("mask", [2, 128, 256])
    y = dout("y", [T, 2048]); z = dout("z", [T, 2048]); yatt = dout("yatt", [T, 1024]); ct = dout("ct", [4, 128, T], BF16)
    e = dout("e", [T, 32]); hloc = dout("hloc", [128, 2048]); dtot = dout("dtot", [128, 32])
    p = Prog(nc)
    SC = ea_scratch(p, "a")
    if 1 in stages:
        stage_ea1(p, cst, inv, pos, xh, w_in, conv_w, conv_b, z, ct, SC)
    if 2 in stages:
        stage_ea2(p, cst, dt_bias, a_log, d_skip, ct, SC, y, e, hloc, dtot)
    if 3 in stages:
        stage_ea3(p, cst, sinks, mask, SC, yatt)
    p.finish()
    return nc


def _moe_tail(p, nc, din, cst, x1, out):
    rg = din("rg", [D, 4]); rgb = din("rgb", [4]); re_ = din("re", [D, 32]); reb = din("reb", [32])
    wg = din("wg", [32, D, 512]); wu = din("wu", [32, D, 512]); wd = din("wd", [32, 512, D])
    pl = din("pl", [T, 256]); g2 = din("g2", [D]); b2 = din("b2", [D]); pwg = din("pwg", [D, D]); pbg = din("pbg", [D]); pwp = din("pwp", [256, D])
    ffn = p.dram("ffn_s", [T, D]).ap()
    stage_moe(p, cst, x1, rg, rgb, re_, reb, wg, wu, wd, ffn, "m")
    stage_tail(p, {"ident": cst[:, 0:128]}, x1, ffn, pl, g2, b2, pwg, pbg, pwp, out)


def _op(p, din, cst, inv, x, ext_k, parts):
    pos8 = din("pos8", [128, NT], I32)
    ow_in = din("ow_in", [D, O_IN]); kvn = din("kvn", [512]); wuk = din("wuk", [16, 128, 512])
    if ext_k:
        _, dout = _mk(p.nc)
        kiT = dout("kiT", [64, T]); kcatT = dout("kcatT", [576, T], BF16); ckv = dout("ckv", [T, 512], BF16)
    else:
        kiT = p.dram("kiT_s", [64, T]).ap(); kcatT = p.dram("kcatT_s", [576, T], BF16).ap(); ckv = p.dram("ckv_s", [T, 512], BF16).ap()
    qcatT = p.dram("qcatT_s", [576, 16, T], BF16).ap(); qiT = p.dram("qiT_s", [64, 16, T]).ap(); wi = p.dram("wi_s", [T, 16]).ap()
    for mode in ("bf", "f32"):
        stage_op(p, cst, inv, pos8, x, ow_in, kvn, wuk, kiT, kcatT, ckv, qcatT, qiT, wi, mode, parts)
    return qcatT, qiT, wi


def build_eb():
    nc = bass.Bass("TRN2", target_bir_lowering=False)
    din, dout = _mk(nc)
    cst = din("cst", [128, NCST]); inv = din("inv", [128, 32])
    y = din("y", [T, 2048]); z = din("z", [T, 2048]); yatt = din("yatt", [T, 1024]); ct = din("ct", [4, 128, T], BF16)
    e = din("e", [T, 32]); hprev = din("hprev", [7, 128, 2048]); dprev = din("dprev", [7, 128, 32])
    x = din("x", [T, D]); ssm_norm = din("ssm_norm", [2048]); w_out = din("w_out", [3072, D]); g1 = din("g1", [D]); b1 = din("b1", [D])
    xo = dout("xo", [T, D])
    p = Prog(nc)
    mixT = p.dram("mixT_s", [24, 128, T], BF16).ap()
    x1 = p.dram("x1_s", [T, D]).ap()
    stage_eb1(p, cst, y, z, yatt, ct, e, hprev, dprev, ssm_norm, mixT)
    stage_proj_ln(p, mixT, 24, w_out, x, g1, b1, x1)
    _moe_tail(p, nc, din, cst, x1, xo)
    _op(p, din, cst, inv, xo, True, "k")
    p.finish()
    return nc


def build_odd():
    nc = bass.Bass("TRN2", target_bir_lowering=False)
    din, dout = _mk(nc)
    cst = din("cst", [128, NCST]); inv = din("inv", [128, 32])
    x = din("x", [T, D]); tqm = din("tqm", [128, NT, 16])
    kiT_f = din("kiT_f", [64, NKEY]); kcatT_f = din("kcatT_f", [576, NKEY], BF16); ckv_f = din("ckv_f", [NKEY, 512], BF16)
    wuv = din("wuv", [16, 512, 128]); ow_out = din("ow_out", [D, D]); g1 = din("g1", [D]); b1 = din("b1", [D])
    xo = dout("xo", [T, D])
    p = Prog(nc)
    qcatT, qiT, wi = _op(p, din, cst, inv, x, False, "q")
    oT = p.dram("oT_s", [16, 128, T], BF16).ap()
    x1 = p.dram("x1_s", [T, D]).ap()
    stage_oq(p, cst, tqm, kiT_f, kcatT_f, ckv_f, qcatT, qiT, wi, wuv, oT, nst_of=[8 * (j + 1) for j in range(NT)])
    stage_proj_ln(p, oT, 16, ow_out, x, g1, b1, x1)
    _moe_tail(p, nc, din, cst, x1, xo)
    p.finish()
    return nc


def _run(nc, in_maps):
    res = run_bass_kernel_spmd(nc, in_maps, core_ids=list(range(8)))
    return res.results


def kernel(_dbg=None, **I):
    NCORE = 8
    C = lambda a: np.ascontiguousarray(a)
    cst = make_cst(); inv = make_inv()
    xcur = C(I["x"][0])
    P = np.asarray(I["positions"][0], np.int32)
    qi_ = np.arange(128)[:, None]; kj = np.arange(256)[None, :]
    band = (kj > qi_) & (kj <= qi_ + 128)
    m_rest = np.where(band, 0.0, -30000.0).astype(np.float32)
    m_first0 = np.where(band & (kj >= 128), 0.0, -30000.0).astype(np.float32)

    def moe_tail_inputs(L, c):
        sl = slice(c * T, (c + 1) * T)
        return {"rg": C(I["moe_router_group"][L]), "rgb": C(I["moe_router_group_b"][L]), "re": C(I["moe_router_expert"][L]),
                "reb": C(I["moe_router_expert_b"][L]), "wg": WG[L], "wu": WU[L], "wd": WD[L],
                "pl": C(I["p"][L, 0, sl]), "g2": C(I["ln2_g"][L]), "b2": C(I["ln2_b"][L]), "pwg": PWG[L],
                "pbg": C(I["ple_b_gate"][L]), "pwp": C(I["ple_w_proj"][L])}
    WG = [C(I["moe_w_gate"][L]) for L in range(4)]; WU = [C(I["moe_w_up"][L]) for L in range(4)]; WD = [C(I["moe_w_down"][L]) for L in range(4)]
    PWG = [C(I["ple_w_gate"][L]) for L in range(4)]
    nc_ea = nc_eb = nc_odd = None
    for L in range(4):
        j = L // 2
        if L % 2 == 0:
            nc_ea = nc_ea or build_ea()
            w_in = C(I["ev_w_in"][j])
            ims = []
            for c in range(NCORE):
                xh = np.zeros((TT_, D), np.float32); pe = np.zeros(TT_, np.int32)
                lo = c * T - 128
                if c > 0:
                    xh[:] = xcur[lo:lo + TT_]; pe[:] = P[lo:lo + TT_]
                else:
                    xh[128:] = xcur[0:T]; pe[128:] = P[0:T]
                ims.append({"cst": cst, "inv": inv, "pos": C(pe.reshape(NTT, 128).T), "xh": xh, "w_in": w_in,
                            "conv_w": C(I["ev_conv_w"][j]), "conv_b": C(I["ev_conv_b"][j]), "dt_bias": C(I["ev_dt_bias"][j]),
                            "a_log": C(I["ev_a_log"][j]), "d_skip": C(I["ev_d_skip"][j]), "sinks": C(I["ev_sinks"][j]),
                            "mask": np.stack([m_first0 if c == 0 else m_rest, m_rest])})
            ra = _run(nc_ea, ims)
            nc_eb = nc_eb or build_eb()
            ims = []
            for c in range(NCORE):
                sl = slice(c * T, (c + 1) * T)
                hp = np.zeros((7, 128, 2048), np.float32); dp = np.ones((7, 128, 32), np.float32)
                for cc in range(c):
                    hp[7 - c + cc] = ra[cc]["hloc"]; dp[7 - c + cc] = ra[cc]["dtot"]
                m = {"cst": cst, "inv": inv, "y": ra[c]["y"], "z": ra[c]["z"], "yatt": ra[c]["yatt"], "ct": ra[c]["ct"], "e": ra[c]["e"],
                     "hprev": hp, "dprev": dp, "x": C(xcur[sl]), "ssm_norm": C(I["ev_ssm_norm"][j]), "w_out": C(I["ev_w_out"][j]),
                     "g1": C(I["ln1_g"][L]), "b1": C(I["ln1_b"][L]), "pos8": C(P[sl].reshape(NT, 128).T),
                     "ow_in": C(I["od_w_in"][j]), "kvn": C(I["od_kv_norm"][j]), "wuk": C(I["od_w_uk"][j])}
                m.update(moe_tail_inputs(L, c))
                ims.append(m)
            rb = _run(nc_eb, ims)
            xcur = np.concatenate([rb[c]["xo"] for c in range(NCORE)], 0)
            kiT_f = np.concatenate([rb[c]["kiT"] for c in range(NCORE)], 1)
            kcatT_f = np.concatenate([rb[c]["kcatT"] for c in range(NCORE)], 1)
            ckv_f = np.concatenate([rb[c]["ckv"] for c in range(NCORE)], 0)
        else:
            nc_odd = nc_odd or build_odd()
            ims = []
            for c in range(NCORE):
                tok = ((np.arange(NT)[:, None] * NCORE + c) * 128 + np.arange(128)[None, :]).reshape(-1)
                tq = tok.reshape(NT, 128).T.astype(np.float32)
                tqm = C(tq[:, :, None] - (np.arange(16) * 512)[None, None, :].astype(np.float32))
                m = {"cst": cst, "inv": inv, "x": C(xcur[tok]), "tqm": tqm, "kiT_f": C(kiT_f), "kcatT_f": C(kcatT_f), "ckv_f": C(ckv_f),
                     "wuv": C(I["od_w_uv"][j]), "ow_out": C(I["od_w_out"][j]), "g1": C(I["ln1_g"][L]), "b1": C(I["ln1_b"][L]),
                     "pos8": C(P[tok].reshape(NT, 128).T), "ow_in": C(I["od_w_in"][j]), "kvn": C(I["od_kv_norm"][j]), "wuk": C(I["od_w_uk"][j])}
                m.update(moe_tail_inputs(L, c))
                m["pl"] = C(I["p"][L, 0][tok])
                ims.append(m)
            ro = _run(nc_odd, ims)
            xnew = np.empty_like(xcur)
            for c in range(NCORE):
                tok = ((np.arange(NT)[:, None] * NCORE + c) * 128 + np.arange(128)[None, :]).reshape(-1)
                xnew[tok] = ro[c]["xo"]
            xcur = xnew
        if _dbg is not None:
            _dbg(L, xcur)
    return xcur[None].astype(np.float32)
```

```python
import numpy as np
from contextlib import ExitStack
import concourse.bass as bass
import concourse.mybir as mybir
from concourse.bass_utils import run_bass_kernel_spmd

F32 = mybir.dt.float32
BF16 = mybir.dt.bfloat16
I32 = mybir.dt.int32
AF = mybir.ActivationFunctionType
ALU = mybir.AluOpType
AX = mybir.AxisListType

COMPUTE = ("tensor", "vector", "scalar", "gpsimd")
QUEUES = ("sync", "scalar", "gpsimd")
NSLOT = 8


class Prog:
    def __init__(self, nc):
        self.nc = nc
        self.ops = []
        self.es = None

    def sb(self, name, shape, dt=F32):
        self.uid = getattr(self, "uid", 0) + 1
        return self.es.enter_context(self.nc.sbuf_tensor("s%d_%s" % (self.uid, name), list(shape), dt))

    def ps(self, name, shape, dt=F32):
        self.uid = getattr(self, "uid", 0) + 1
        return self.es.enter_context(self.nc.psum_tensor("p%d_%s" % (self.uid, name), list(shape), dt))

    def dram(self, name, shape, dt=F32, kind=None):
        if kind is None:
            return self.nc.dram_tensor(name, list(shape), dt)
        return self.nc.dram_tensor(name, list(shape), dt, kind=kind)

    def allgather(self, out_t, in_t, n=8):
        self.add("gpsimd", lambda e: e.collective_compute("AllGather", ALU.bypass, replica_groups=[list(range(n))],
                                                          ins=[in_t.ap().opt()], outs=[out_t.ap().opt()]),
                 [in_t], [out_t], dma="cc")

    @staticmethod
    def _keys(aps):
        ks = []
        for a in aps:
            if a is None or isinstance(a, (int, float)):
                continue
            ks.append(a.tensor.name if hasattr(a, "tensor") else a.name)
        return ks

    def add(self, eng, fn, r, w, dma=False):
        self.ops.append(dict(eng=eng, fn=fn, r=self._keys(r), w=self._keys(w), dma=dma))

    def mm(self, out, lhsT, rhs, start=True, stop=True):
        self.add("tensor", lambda e: e.matmul(out, lhsT, rhs, start=start, stop=stop), [lhsT, rhs], [out])

    def tr(self, out, in_, ident):
        self.add("tensor", lambda e: e.transpose(out, in_, ident), [in_, ident], [out])

    def act(self, out, in_, func, bias=0.0, scale=1.0, accum_out=None, eng="scalar"):
        r = [in_] + [b for b in (bias, scale) if not isinstance(b, (int, float))]
        w = [out] + ([accum_out] if accum_out is not None else [])
        if accum_out is None:
            self.add(eng, lambda e: e.activation(out, in_, func, bias=bias, scale=scale), r, w)
        else:
            self.add(eng, lambda e: e.activation(out, in_, func, bias=bias, scale=scale, accum_out=accum_out), r, w)

    def tt(self, out, in0, in1, op, eng="vector"):
        self.add(eng, lambda e: e.tensor_tensor(out, in0, in1, op), [in0, in1], [out])

    def ts(self, out, in0, s1, s2=None, op0=ALU.mult, op1=None, accum_out=None, eng="vector"):
        r = [in0] + [s for s in (s1, s2) if s is not None and not isinstance(s, (int, float))]
        w = [out] + ([accum_out] if accum_out is not None else [])
        kw = {}
        if op1 is not None:
            kw["op1"] = op1
        if accum_out is not None:
            kw["accum_out"] = accum_out
        self.add(eng, lambda e: e.tensor_scalar(out, in0, s1, s2, op0, **kw), r, w)

    def stt(self, out, in0, scalar, in1, op0, op1, eng="vector"):
        r = [in0, in1] + ([scalar] if not isinstance(scalar, (int, float)) else [])
        self.add(eng, lambda e: e.scalar_tensor_tensor(out, in0, scalar, in1, op0, op1), r, [out])

    def copy(self, out, in_, eng="vector"):
        if eng == "scalar":
            self.add(eng, lambda e: e.copy(out, in_), [in_], [out])
        else:
            self.add(eng, lambda e: e.tensor_copy(out, in_), [in_], [out])

    def reduce(self, out, in_, op, axis=AX.X, eng="vector"):
        self.add(eng, lambda e: e.tensor_reduce(out, in_, axis, op), [in_], [out])

    def memset(self, ap, val, eng="vector"):
        self.add(eng, lambda e: e.memset(ap, val), [], [ap])

    def recip(self, out, in_):
        self.add("vector", lambda e: e.reciprocal(out, in_), [in_], [out])

    def dma(self, out, in_, q="sync", **kw):
        self.add(q, lambda e: e.dma_start(out, in_, **kw), [in_], [out], dma=True)

    def raw(self, eng, fn, r, w):
        self.add(eng, fn, r, w)

    def _init_sems(self):
        nc = self.nc
        self.ges = ExitStack()
        self.eng_sem = {e: self.ges.enter_context(nc.semaphore("sem_" + e)) for e in COMPUTE}
        self.slot_sem = {q: [self.ges.enter_context(nc.semaphore("dq_%s_%d" % (q, s))) for s in range(NSLOT)]
                         for q in QUEUES}
        self.cc_sem = self.ges.enter_context(nc.semaphore("cc_sem"))
        self.cc_cnt = 0
        self.eng_cnt = {e: 0 for e in COMPUTE}
        self.q_cnt = {q: 0 for q in QUEUES}
        self.slot_val = {q: [0] * NSLOT for q in QUEUES}
        self.total_ops = 0

    def begin(self):
        if not hasattr(self, "eng_sem"):
            self._init_sems()
        self.es = ExitStack()
        self.ops = []

    def end(self):
        nc = self.nc
        ops = self.ops
        last_w = {}
        readers = {}
        for i, op in enumerate(ops):
            deps = set()
            for k in op["r"]:
                if k in last_w:
                    deps.add(last_w[k])
            for k in op["w"]:
                if k in last_w:
                    deps.add(last_w[k])
                deps.update(readers.get(k, ()))
            deps.discard(i)
            op["deps"] = deps
            for k in op["r"]:
                readers.setdefault(k, []).append(i)
            for k in op["w"]:
                last_w[k] = i
                readers[k] = []
        eng_sem, slot_sem = self.eng_sem, self.slot_sem
        eng_cnt, q_cnt, slot_val = self.eng_cnt, self.q_cnt, self.slot_val
        for op in ops:
            if op["dma"] == "cc":
                self.cc_cnt += 1
                op["tok"] = (self.cc_sem, self.cc_cnt)
                op["slot_prev"] = (self.cc_sem, self.cc_cnt - 1)
            elif op["dma"]:
                q = op["eng"]
                s = q_cnt[q] % NSLOT
                q_cnt[q] += 1
                op["slot_prev"] = (slot_sem[q][s], slot_val[q][s])
                slot_val[q][s] += 16
                op["tok"] = (slot_sem[q][s], slot_val[q][s])
            else:
                e = op["eng"]
                eng_cnt[e] += 1
                op["tok"] = (eng_sem[e], eng_cnt[e])
        per_eng = {e: [] for e in set(COMPUTE) | set(QUEUES)}
        for i, op in enumerate(ops):
            per_eng[op["eng"]].append(i)
        self.total_ops += len(ops)
        final = []
        for q in QUEUES:
            for s in range(NSLOT):
                if slot_val[q][s] > 0:
                    final.append((slot_sem[q][s], slot_val[q][s]))
        for en in COMPUTE:
            if eng_cnt[en] > 0:
                final.append((eng_sem[en], eng_cnt[en]))
        if self.cc_cnt > 0:
            final.append((self.cc_sem, self.cc_cnt))

        def run_engine(ename, e):
            seen = {}
            for i in per_eng[ename]:
                op = ops[i]
                waits = {}
                for j in op["deps"]:
                    dj = ops[j]
                    if ename == "tensor" and dj["eng"] == "tensor" and not dj["dma"]:
                        continue
                    sem, val = dj["tok"]
                    waits[sem] = max(waits.get(sem, 0), val)
                if op["dma"]:
                    sem, val = op["slot_prev"]
                    if val > 0:
                        waits[sem] = max(waits.get(sem, 0), val)
                for sem, val in waits.items():
                    if seen.get(sem, 0) >= val:
                        continue
                    e.wait_ge(sem, val)
                    seen[sem] = val
                ins = op["fn"](e)
                sem, val = op["tok"]
                if op["dma"] == "cc":
                    ins.then_inc(sem)
                else:
                    ins.then_inc(sem, 16 if op["dma"] else 1)
            for sem, val in final:
                e.wait_ge(sem, val)

        with nc.Block() as block:
            @block.tensor
            def _(e):
                run_engine("tensor", e)

            @block.vector
            def _(e):
                run_engine("vector", e)

            @block.scalar
            def _(e):
                run_engine("scalar", e)

            @block.gpsimd
            def _(e):
                run_engine("gpsimd", e)

            @block.sync
            def _(e):
                run_engine("sync", e)
        self.es.close()
        self.ops = []

    def finish(self):
        self.ges.close()


T = 1024
NT = T // 128
D = 2048
KC = D // 128
ALPHA = 8 ** 0.25
EPS = 1e-5


def bcast_rows(ap1d, n, parts=128):
    return ap1d.rearrange("(o n) -> o n", o=1).broadcast_to([parts, n])


def load_consts(p, c):
    ident = p.sb("ident", [128, 128], F32)
    p.dma(ident[:], c["ident"][:, :])
    identb = p.sb("identb", [128, 128], BF16)
    p.copy(identb[:], ident[:])
    return ident, identb


def ln_scratch(p, n=2):
    return [(p.sb("lnst_%d" % i, [128, 4, 6], F32), p.sb("lnmv_%d" % i, [128, 2], F32),
             p.sb("lnrs_%d" % i, [128, 1], F32)) for i in range(n)]


def layer_norm_tile(p, out, v, g_rows, b_rows, scr):
    stats, mv, rstd = scr
    for c in range(4):
        p.raw("vector", lambda e, c=c: e.bn_stats(stats[:, c, :], v[:, c * 512:(c + 1) * 512]), [v], [stats])
    p.raw("vector", lambda e: e.bn_aggr(mv[:], stats[:].rearrange("p a b -> p (a b)")), [stats], [mv])
    p.ts(rstd[:], mv[:, 1:2], EPS, None, op0=ALU.add)
    p.act(rstd[:], rstd[:], AF.Sqrt)
    p.recip(rstd[:], rstd[:])
    p.ts(out, v, mv[:, 0:1], rstd[:, 0:1], op0=ALU.subtract, op1=ALU.mult)
    p.tt(out, out, g_rows, ALU.mult, eng="gpsimd")
    p.tt(out, out, b_rows, ALU.add, eng="gpsimd")


def transpose_to(p, dstT, src, ident, nchunks, tcol, pst, copy_engs=("vector", "scalar")):
    for g in range(0, nchunks, 4):
        ps = pst[(g // 4) % len(pst)]
        n = min(4, nchunks - g)
        for j in range(n):
            p.tr(ps[:, j * 128:(j + 1) * 128], src[:, (g + j) * 128:(g + j + 1) * 128], ident[:])
        eng = copy_engs[(g // 4) % len(copy_engs)]
        p.copy(dstT[:, g:g + n, tcol:tcol + 128], ps[:, 0:n * 128].rearrange("p (a b) -> p a b", a=n), eng=eng)


def load_w_bf16(p, dst, src, stg, n, engs=("scalar", "gpsimd"), qs=("sync", "sync"), g=4):
    kc = dst.shape[1]
    st = getattr(p, "_wctr", 0)
    for i, k0 in enumerate(range(0, kc, g)):
        k1 = min(kc, k0 + g)
        s_ = stg[(st + i) % len(stg)]
        p.dma(s_[:, 0:k1 - k0, 0:n], src[:, k0:k1, :], q=qs[(st + i) % len(qs)])
        p.copy(dst[:, k0:k1, :], s_[:, 0:k1 - k0, 0:n], eng=engs[(st + i) % len(engs)])
    p._wctr = st + (kc + g - 1) // g


def stage_tail(p, c, x1_d, ffn_d, pl_d, ln_g, ln_b, wg_d, bg_d, wp_d, out_d):
    p.begin()
    ident, identb = load_consts(p, c)
    g_rows = p.sb("g_rows", [128, D], F32)
    b_rows = p.sb("b_rows", [128, D], F32)
    bg_rows = p.sb("bg_rows", [128, D], F32)
    p.dma(g_rows[:], bcast_rows(ln_g, D))
    p.dma(b_rows[:], bcast_rows(ln_b, D))
    p.dma(bg_rows[:], bcast_rows(bg_d, D))
    x2 = p.sb("x2", [128, NT, D], F32)
    x2T = p.sb("x2T", [128, KC, T], BF16)
    plT = p.sb("plT", [128, 2, T], BF16)
    pst = [p.ps("pst0", [128, 512]), p.ps("pst1", [128, 512])]
    fa = [p.sb("fa0", [128, D], F32)] * 2
    pa = [p.sb("pa0", [128, 256], F32), p.sb("pa1", [128, 256], F32)]
    lns = ln_scratch(p)
    for tt in range(NT):
        a, f, pp = x2[:, tt, :], fa[tt % 2], pa[tt % 2]
        p.dma(a, x1_d[tt * 128:(tt + 1) * 128, :])
        p.dma(f[:], ffn_d[tt * 128:(tt + 1) * 128, :], q="gpsimd")
        p.dma(pp[:], pl_d[tt * 128:(tt + 1) * 128, :])
        p.stt(a, a, ALPHA, f[:], ALU.mult, ALU.add)
        layer_norm_tile(p, a, a, g_rows[:], b_rows[:], lns[tt % 2])
        transpose_to(p, x2T, x2[:, tt, :], ident, KC, tt * 128, pst)
        transpose_to(p, plT, pp, ident, 2, tt * 128, pst)
    wst = [p.sb("wst0", [128, 4, 512], F32), p.sb("wst1", [128, 4, 512], F32)]
    wbf = [p.sb("wbf0", [128, KC, 512], BF16), p.sb("wbf1", [128, KC, 512], BF16)]
    wpbf = [p.sb("wpbf0", [128, 2, 512], BF16), p.sb("wpbf1", [128, 2, 512], BF16)]
    psg = [p.ps("psg0", [128, 512]), p.ps("psg1", [128, 512])]
    psp = [p.ps("psp0", [128, 512]), p.ps("psp1", [128, 512])]
    gate = [p.sb("gate0", [128, 512], F32), p.sb("gate1", [128, 512], F32)]
    ob = [p.sb("ob0", [128, 512], F32), p.sb("ob1", [128, 512], F32)]
    for n in range(4):
        cs = slice(n * 512, (n + 1) * 512)
        wb, wpb = wbf[n % 2], wpbf[n % 2]
        load_w_bf16(p, wb, wg_d[:, cs].rearrange("(kc k) n -> k kc n", k=128), wst, 512)
        load_w_bf16(p, wpb, wp_d[:, cs].rearrange("(kc k) n -> k kc n", k=128), wst, 512)
        for tt in range(NT):
            i = n * NT + tt
            pg, pp_, gt, o = psg[i % 2], psp[i % 2], gate[i % 2], ob[i % 2]
            for kc in range(KC):
                p.mm(pg[:], x2T[:, kc, tt * 128:(tt + 1) * 128], wb[:, kc, :], start=(kc == 0), stop=(kc == KC - 1))
            for kc in range(2):
                p.mm(pp_[:], plT[:, kc, tt * 128:(tt + 1) * 128], wpb[:, kc, :], start=(kc == 0), stop=(kc == 1))
            p.tt(gt[:], pg[:], bg_rows[:, cs], ALU.add)
            p.act(gt[:], gt[:], AF.Sigmoid)
            p.tt(o[:], pp_[:], gt[:], ALU.mult)
            p.tt(o[:], o[:], x2[:, tt, cs], ALU.add, eng="gpsimd")
            p.dma(out_d[tt * 128:(tt + 1) * 128, cs], o[:], q="scalar")
    p.end()


CST_NAMES = ["ident", "triu", "trius", "ones", "iota", "tril_s"]


def make_cst():
    r = np.arange(128)
    ident = np.eye(128, dtype=np.float32)
    triu = (r[:, None] <= r[None, :]).astype(np.float32)
    trius = (r[:, None] < r[None, :]).astype(np.float32)
    ones = np.ones((128, 128), np.float32)
    iota = np.broadcast_to(r[None, :].astype(np.float32), (128, 128)).copy()
    tril_s = (r[:, None] > r[None, :]).astype(np.float32)
    return np.concatenate([ident, triu, trius, ones, iota, tril_s], axis=1)


def load_cst(p, cst_d, names, bf=()):
    out = {}
    for nm in names:
        i = CST_NAMES.index(nm)
        t = p.sb("c_" + nm, [128, 128], F32)
        p.dma(t[:], cst_d[:, i * 128:(i + 1) * 128])
        out[nm] = t
    for nm in bf:
        tb = p.sb("cb_" + nm, [128, 128], BF16)
        p.copy(tb[:], out[nm][:])
        out[nm + "_bf"] = tb
    return out


CAP = 128
NE = 32
FF = 512


def stage_moe(p, cst_d, x1_d, rg_d, rgb_d, re_d, reb_d, wg_d, wu_d, wd_d, ffn_d, tag):
    xbf_d = p.dram("moe_xbf_" + tag, [T, D], BF16).ap()
    rt_d = p.dram("moe_rt_" + tag, [3, 128, NT * NE], F32).ap()
    p.begin()
    C = load_cst(p, cst_d, ["ident", "trius", "ones", "iota"], bf=["ident", "trius", "ones"])
    ident, iota = C["ident"], C["iota"]
    PA = p.ps("PA", [128, 2048])
    PG = p.ps("PG", [128, 512])
    PU = p.ps("PU", [128, 512])
    Xbf = p.sb("Xbf", [128, NT, D], BF16)
    asg = p.sb("asg", [128, NT, NE], F32)
    gts = p.sb("gts", [128, NT, NE], F32)
    pos = p.sb("pos", [128, NT, NE], F32)
    asgb = p.sb("asgb", [128, NT, NE], BF16)
    wr = p.sb("wr", [128, KC, 36], F32)
    p.dma(wr[:, :, 0:4], rg_d.rearrange("(kc k) n -> k kc n", k=128))
    p.dma(wr[:, :, 4:36], re_d.rearrange("(kc k) n -> k kc n", k=128))
    rb = p.sb("rb", [128, 36], F32)
    p.dma(rb[:, 0:4], bcast_rows(rgb_d, 4))
    p.dma(rb[:, 4:36], bcast_rows(reb_d, 32))
    xt = [p.sb("xt0", [128, D], F32), p.sb("xt1", [128, D], F32)]
    xTf = [p.sb("xTf0", [128, KC, 128], F32), p.sb("xTf1", [128, KC, 128], F32)]
    sm = {k: p.sb("sm_" + k, [128, n], F32) for k, n in
          [("lg", 36), ("gmax", 1), ("oh", 4), ("ge", 4), ("gsum", 1), ("tmp", 32), ("es", 8), ("v1", 1), ("m1", 8),
           ("e2", 8), ("v2", 1), ("m2", 8), ("d", 1), ("p1", 1), ("p2", 1), ("mm", 8), ("gm", 8)]}
    for tt in range(NT):
        x, xf = xt[tt % 2], xTf[tt % 2]
        p.dma(x[:], x1_d[tt * 128:(tt + 1) * 128, :])
        p.copy(Xbf[:, tt, :], x[:], eng="gpsimd")
        for g in range(0, KC, 4):
            for j in range(4):
                p.tr(PA[:, j * 128:(j + 1) * 128], x[:, (g + j) * 128:(g + j + 1) * 128], ident[:])
            p.copy(xf[:, g:g + 4, :], PA[:, 0:512].rearrange("p (a b) -> p a b", a=4), eng="scalar" if (g // 4) % 2 else "vector")
        for kc in range(KC):
            p.mm(PG[:, 0:36], xf[:, kc, :], wr[:, kc, :], start=(kc == 0), stop=(kc == KC - 1))
        lg = sm["lg"]
        p.tt(lg[:], PG[:, 0:36], rb[:], ALU.add)
        p.reduce(sm["gmax"][:], lg[:, 0:4], ALU.max)
        p.ts(sm["oh"][:], lg[:, 0:4], sm["gmax"][:, 0:1], None, op0=ALU.is_equal)
        p.ts(sm["ge"][:], lg[:, 0:4], sm["gmax"][:, 0:1], None, op0=ALU.subtract)
        p.act(sm["ge"][:], sm["ge"][:], AF.Exp, accum_out=sm["gsum"][:])
        p.recip(sm["gsum"][:], sm["gsum"][:])
        p.tt(sm["tmp"][:].rearrange("p (g i) -> p g i", g=4), lg[:, 4:36].rearrange("p (g i) -> p g i", g=4),
             sm["oh"][:, :, None].to_broadcast([128, 4, 8]), ALU.mult)
        p.reduce(sm["es"][:], sm["tmp"][:].rearrange("p (g i) -> p i g", g=4), ALU.add)
        p.reduce(sm["v1"][:], sm["es"][:], ALU.max)
        p.ts(sm["m1"][:], sm["es"][:], sm["v1"][:, 0:1], None, op0=ALU.is_equal)
        p.stt(sm["e2"][:], sm["m1"][:], -1e30, sm["es"][:], ALU.mult, ALU.add)
        p.reduce(sm["v2"][:], sm["e2"][:], ALU.max)
        p.ts(sm["m2"][:], sm["e2"][:], sm["v2"][:, 0:1], None, op0=ALU.is_equal)
        p.tt(sm["d"][:], sm["v2"][:], sm["v1"][:], ALU.subtract)
        p.act(sm["d"][:], sm["d"][:], AF.Exp)
        p.ts(sm["p1"][:], sm["d"][:], 1.0, None, op0=ALU.add)
        p.recip(sm["p1"][:], sm["p1"][:])
        p.tt(sm["p2"][:], sm["d"][:], sm["p1"][:], ALU.mult)
        p.tt(sm["p1"][:], sm["p1"][:], sm["gsum"][:], ALU.mult)
        p.tt(sm["p2"][:], sm["p2"][:], sm["gsum"][:], ALU.mult)
        p.tt(sm["mm"][:], sm["m1"][:], sm["m2"][:], ALU.add)
        p.ts(sm["gm"][:], sm["m1"][:], sm["p1"][:, 0:1], None, op0=ALU.mult)
        p.stt(sm["gm"][:], sm["m2"][:], sm["p2"][:, 0:1], sm["gm"][:], ALU.mult, ALU.add)
        p.tt(asg[:, tt, :].rearrange("p (g i) -> p g i", g=4), sm["oh"][:, :, None].to_broadcast([128, 4, 8]),
             sm["mm"][:, None, :].to_broadcast([128, 4, 8]), ALU.mult)
        p.tt(gts[:, tt, :].rearrange("p (g i) -> p g i", g=4), sm["oh"][:, :, None].to_broadcast([128, 4, 8]),
             sm["gm"][:, None, :].to_broadcast([128, 4, 8]), ALU.mult)
        p.copy(asgb[:, tt, :], asg[:, tt, :])
        for t2 in range(tt + 1):
            lhs = C["trius_bf"] if t2 == tt else C["ones_bf"]
            p.mm(PU[:, 0:32], lhs[:], asgb[:, t2, :], start=(t2 == 0), stop=(t2 == tt))
        p.copy(pos[:, tt, :], PU[:, 0:32])
        p.dma(xbf_d[tt * 128:(tt + 1) * 128, :], Xbf[:, tt, :], q="gpsimd")
    p.dma(rt_d[0], asg[:].rearrange("p a b -> p (a b)"))
    p.dma(rt_d[1], gts[:].rearrange("p a b -> p (a b)"))
    p.dma(rt_d[2], pos[:].rearrange("p a b -> p (a b)"))
    p.end()
    p.begin()
    C = load_cst(p, cst_d, ["ident", "iota"], bf=["ident"])
    iota = C["iota"]
    PA = p.ps("PA", [128, 2048])
    PG = p.ps("PG", [128, 512])
    PU = p.ps("PU", [128, 512])
    PT = p.ps("PT", [128, 1024], BF16)
    PC = p.ps("PC", [128, 512])
    Xbf = p.sb("Xbf", [128, NT, D], BF16)
    yacc = p.sb("yacc", [128, NT, D], F32)
    asg = p.sb("asg", [128, NT, NE], F32)
    gts = p.sb("gts", [128, NT, NE], F32)
    pos = p.sb("pos", [128, NT, NE], F32)
    p.dma(Xbf[:], xbf_d.rearrange("(a p) d -> p a d", p=128))
    p.dma(asg[:].rearrange("p a b -> p (a b)"), rt_d[0])
    p.dma(gts[:].rearrange("p a b -> p (a b)"), rt_d[1])
    p.dma(pos[:].rearrange("p a b -> p (a b)"), rt_d[2])
    wst = [p.sb("wst%d" % i, [128, 4, 512], F32) for i in range(2)]
    Wg = p.sb("Wg", [128, KC, FF], BF16)
    Wu = p.sb("Wu", [128, KC, FF], BF16)
    Wd = p.sb("Wd", [128, 4, D], BF16)
    Se = [p.sb("Se%d" % i, [128, NT, CAP], BF16) for i in range(2)]
    GeT = p.sb("GeT", [128, 4, NT, 128], BF16)
    Ge = [p.sb("Ge%d" % i, [128, NT, CAP], BF16) for i in range(2)]
    XeT = p.sb("XeT", [128, KC, CAP], BF16)
    sg = p.sb("sg", [128, FF], F32)
    hidT = p.sb("hidT", [128, 4, CAP], BF16)
    Y = p.sb("Y", [128, 4, D], BF16)
    for e in range(NE):
        slot = e % 4
        S, G = Se[e % 2], Ge[e % 2]
        posb = pos[:, :, e:e + 1].to_broadcast([128, NT, CAP])
        p.tt(S[:], iota[:, None, :].to_broadcast([128, NT, CAP]), posb, ALU.is_equal)
        p.tt(G[:], S[:], gts[:, :, e:e + 1].to_broadcast([128, NT, CAP]), ALU.mult)
        p.tt(S[:], S[:], asg[:, :, e:e + 1].to_broadcast([128, NT, CAP]), ALU.mult)
        load_w_bf16(p, Wg, wg_d[e].rearrange("(kc k) n -> k kc n", k=128), wst, 512, engs=("scalar", "gpsimd", "vector"), qs=("sync",))
        load_w_bf16(p, Wu, wu_d[e].rearrange("(kc k) n -> k kc n", k=128), wst, 512, engs=("scalar", "gpsimd", "vector"), qs=("sync",))
        wdv = wd_d[e].rearrange("(kc k) n -> k kc n", k=128)
        for dc in range(4):
            load_w_bf16(p, Wd[:, :, dc * 512:(dc + 1) * 512], wdv[:, :, dc * 512:(dc + 1) * 512], wst, 512,
                        engs=("scalar", "gpsimd", "vector"), qs=("sync",))
        for fc in range(KC):
            for tt in range(NT):
                p.mm(PA[:, fc * 128:(fc + 1) * 128], Xbf[:, tt, fc * 128:(fc + 1) * 128], S[:, tt, :],
                     start=(tt == 0), stop=(tt == NT - 1))
        p.copy(XeT[:], PA[:].rearrange("p (a b) -> p a b", a=KC), eng="scalar")
        for fch in range(4):
            for kc in range(KC):
                p.mm(PG[:, fch * 128:(fch + 1) * 128], Wg[:, kc, fch * 128:(fch + 1) * 128], XeT[:, kc, :],
                     start=(kc == 0), stop=(kc == KC - 1))
        for fch in range(4):
            for kc in range(KC):
                p.mm(PU[:, fch * 128:(fch + 1) * 128], Wu[:, kc, fch * 128:(fch + 1) * 128], XeT[:, kc, :],
                     start=(kc == 0), stop=(kc == KC - 1))
        p.act(sg[:], PG[:], AF.Silu)
        p.tt(hidT[:].rearrange("p a b -> p (a b)"), sg[:], PU[:], ALU.mult)
        for dc in range(4):
            for fch in range(4):
                p.mm(PA[:, dc * 512:(dc + 1) * 512], hidT[:, fch, :], Wd[:, fch, dc * 512:(dc + 1) * 512],
                     start=(fch == 0), stop=(fch == 3))
        p.copy(Y[:, slot, :], PA[:], eng="scalar")
        for tt in range(NT):
            p.tr(PT[:, tt * 128:(tt + 1) * 128], G[:, tt, :], C["ident_bf"][:])
        p.copy(GeT[:, slot, :, :], PT[:].rearrange("p (a b) -> p a b", a=NT))
        if slot == 3:
            grp = e // 4
            for tt in range(NT):
                for dc in range(4):
                    for s4 in range(4):
                        p.mm(PC[:], GeT[:, s4, tt, :], Y[:, s4, dc * 512:(dc + 1) * 512], start=(s4 == 0), stop=(s4 == 3))
                    dst = yacc[:, tt, dc * 512:(dc + 1) * 512]
                    if grp == 0:
                        p.copy(dst, PC[:])
                    else:
                        p.tt(dst, PC[:], dst, ALU.add)
    for tt in range(NT):
        p.dma(ffn_d[tt * 128:(tt + 1) * 128, :], yacc[:, tt, :])
    p.end()


TWO_PI = 2.0 * np.pi
CW1 = 6.28125
CW2 = TWO_PI - CW1


def make_inv():
    inv = (10000.0 ** (-np.arange(0, 64, 2, dtype=np.float32) / 64)).astype(np.float32)
    return np.broadcast_to(inv[None, :], (128, 32)).copy()


def rope_tables(p, pos_d, inv_d, ntiles):
    n = ntiles * 32
    posi = p.sb("posi", [128, ntiles], I32)
    posf = p.sb("posf", [128, ntiles], F32)
    inv = p.sb("inv", [128, 32], F32)
    ang = p.sb("ang", [128, ntiles, 32], F32)
    ki = p.sb("ki", [128, ntiles, 32], I32)
    kf = p.sb("kf", [128, ntiles, 32], F32)
    r = p.sb("r", [128, ntiles, 32], F32)
    m = p.sb("rm", [128, ntiles, 32], F32)
    cos = p.sb("cos", [128, ntiles, 32], F32)
    sin = p.sb("sin", [128, ntiles, 32], F32)
    p.dma(posi[:], pos_d)
    p.dma(inv[:], inv_d)
    p.copy(posf[:], posi[:], eng="scalar")
    p.tt(ang[:], inv[:, None, :].to_broadcast([128, ntiles, 32]), posf[:, :, None].to_broadcast([128, ntiles, 32]), ALU.mult)
    p.ts(kf[:], ang[:], 1.0 / TWO_PI, None, op0=ALU.mult)
    p.copy(ki[:], kf[:])
    p.copy(kf[:], ki[:])
    p.stt(r[:], kf[:], -CW1, ang[:], ALU.mult, ALU.add)
    p.stt(r[:], kf[:], -CW2, r[:], ALU.mult, ALU.add)

    def wrap(t):
        p.ts(m[:], t[:], np.pi, None, op0=ALU.is_gt)
        p.stt(t[:], m[:], -TWO_PI, t[:], ALU.mult, ALU.add)
        p.ts(m[:], t[:], -np.pi, None, op0=ALU.is_lt)
        p.stt(t[:], m[:], TWO_PI, t[:], ALU.mult, ALU.add)
    wrap(r)
    p.act(sin[:], r[:], AF.Sin)
    p.ts(r[:], r[:], np.pi / 2, None, op0=ALU.add)
    wrap(r)
    p.act(cos[:], r[:], AF.Sin)
    return cos, sin


def rope_tm(p, dst, src, cos, sin, H, ta, tb):
    cb = cos[:, None, :].to_broadcast([128, H, 32])
    sb_ = sin[:, None, :].to_broadcast([128, H, 32])
    t1, t2 = src[:, :, 0:32], src[:, :, 32:64]
    p.tt(ta, t1, cb, ALU.mult)
    p.tt(tb, t2, sb_, ALU.mult)
    p.tt(dst[:, :, 0:32], ta, tb, ALU.subtract)
    p.tt(ta, t2, cb, ALU.mult)
    p.tt(tb, t1, sb_, ALU.mult)
    p.tt(dst[:, :, 32:64], ta, tb, ALU.add)


TT_ = T + 128
NTT = NT + 1
E_IN = 6432


def ea_scratch(p, tag):
    S = {}
    S["dt"] = p.dram("ea_dt_" + tag, [128, NT * 32], F32).ap()
    S["qT"] = p.dram("ea_qT_" + tag, [8, 128, T], BF16).ap()
    S["kT"] = p.dram("ea_kT_" + tag, [2, 128, TT_], BF16).ap()
    S["v"] = p.dram("ea_v_" + tag, [TT_, 128], BF16).ap()
    S["xs"] = p.dram("ea_xs_" + tag, [T, 2048], BF16).ap()
    S["BT"] = p.dram("ea_BT_" + tag, [4, 128, T], BF16).ap()
    S["Btm"] = p.dram("ea_Btm_" + tag, [T, 512], BF16).ap()
    return S


def stage_ea1(p, cst_d, inv_d, pos_d, xh_d, w_in_d, conv_w_d, conv_b_d, z_d, ct_d, S):
    import os
    UPTO = int(os.environ.get("EA1_UPTO", "9"))
    p.begin()
    C = load_cst(p, cst_d, ["ident"])
    ident = C["ident"]
    cos, sin = rope_tables(p, pos_d, inv_d, NTT)
    xT = p.sb("xT", [128, KC, TT_], BF16)
    PS = [p.ps("P%d" % i, [128, 512]) for i in range(4)]
    PB = [p.ps("PB%d" % i, [128, 1024]) for i in range(2)]
    xt = [p.sb("xt0", [128, D], F32), p.sb("xt1", [128, D], F32)]
    for tt in range(NTT):
        x = xt[tt % 2]
        p.dma(x[:], xh_d[tt * 128:(tt + 1) * 128, :], q="sync" if tt % 2 else "gpsimd")
        transpose_to(p, xT, x, ident, KC, tt * 128, PS[0:2])
    if UPTO <= 1:
        p.end()
        return
    wst = [p.sb("wst%d" % i, [128, 4, 512], F32) for i in range(2)]
    wbf = [p.sb("wbf%d" % i, [128, KC, 512], BF16) for i in range(2)]
    ob = [p.sb("ob%d" % i, [128, 512], F32) for i in range(2)]

    def wview(c0, n):
        return w_in_d[:, c0:c0 + n].rearrange("(kc k) n -> k kc n", k=128)
    cnt = 0
    for n in range(4):
        wb = wbf[n % 2]
        load_w_bf16(p, wb, wview(n * 512, 512), wst, 512)
        for tt in range(1, NTT):
            ps, o = PS[cnt % 4], ob[cnt % 2]
            for kc in range(KC):
                p.mm(ps[:], xT[:, kc, tt * 128:(tt + 1) * 128], wb[:, kc, :], start=(kc == 0), stop=(kc == KC - 1))
            p.copy(o[:], ps[:], eng="scalar" if cnt % 2 else "vector")
            p.dma(z_d[(tt - 1) * 128:tt * 128, n * 512:(n + 1) * 512], o[:], q="scalar")
            cnt += 1
    if UPTO <= 2:
        p.end()
        return
    wb = wbf[0]
    load_w_bf16(p, wb[:, :, 0:32], wview(5120, 32), wst, 32)
    dtraw = p.sb("dtraw", [128, NT, 32], F32)
    for tt in range(1, NTT):
        ps = PS[cnt % 4]
        for kc in range(KC):
            p.mm(ps[:, 0:32], xT[:, kc, tt * 128:(tt + 1) * 128], wb[:, kc, 0:32], start=(kc == 0), stop=(kc == KC - 1))
        p.copy(dtraw[:, tt - 1, :], ps[:, 0:32])
        cnt += 1
    p.dma(S["dt"], dtraw[:].rearrange("p a b -> p (a b)"))
    if UPTO <= 3:
        p.end()
        return
    qr = [p.sb("qr%d" % i, [128, 8, 64], F32) for i in range(2)]
    ta = p.sb("ta", [128, 8, 32], F32)
    tb = p.sb("tb", [128, 8, 32], F32)
    qTb = [p.sb("qTb%d" % i, [128, 4, 128], BF16) for i in range(2)]
    for hc in range(2):
        wb = wbf[(hc + 1) % 2]
        load_w_bf16(p, wb, wview(5152 + hc * 512, 512), wst, 512)
        for tt in range(1, NTT):
            ps, q_, qt = PS[cnt % 4], qr[cnt % 2], qTb[cnt % 2]
            for kc in range(KC):
                p.mm(ps[:], xT[:, kc, tt * 128:(tt + 1) * 128], wb[:, kc, :], start=(kc == 0), stop=(kc == KC - 1))
            rope_tm(p, q_[:], ps[:].rearrange("p (h d) -> p h d", h=8), cos[:, tt, :], sin[:, tt, :], 8, ta[:], tb[:])
            pt = PS[(cnt + 2) % 4]
            qf = q_[:].rearrange("p h d -> p (h d)")
            for j in range(4):
                p.tr(pt[:, j * 128:(j + 1) * 128], qf[:, j * 128:(j + 1) * 128], ident[:])
            p.copy(qt[:], pt[:].rearrange("p (a b) -> p a b", a=4), eng="scalar")
            p.dma(S["qT"][hc * 4:(hc + 1) * 4, :, (tt - 1) * 128:tt * 128].rearrange("a p t -> p a t"), qt[:], q="scalar")
            cnt += 1
    if UPTO <= 4:
        p.end()
        return
    wb = wbf[1]
    load_w_bf16(p, wb[:, :, 0:256], wview(6176, 256), wst, 256)
    kr = p.sb("kr", [128, 2, 64], F32)
    ksb = p.sb("ksb", [128, 128], F32)
    kd = [p.sb("kd%d" % i, [128, 2, 2, 64], F32) for i in range(2)]
    kTb = [p.sb("kTb%d" % i, [128, 2, 128], BF16) for i in range(2)]
    vb = [p.sb("vb%d" % i, [128, 128], BF16) for i in range(2)]
    KVL = int(os.environ.get("EA1_KV", "9"))
    for tt in range(NTT):
        ps, kd_, kt_, v_ = PS[cnt % 4], kd[cnt % 2], kTb[cnt % 2], vb[cnt % 2]
        for kc in range(KC):
            p.mm(ps[:, 0:256], xT[:, kc, tt * 128:(tt + 1) * 128], wb[:, kc, 0:256], start=(kc == 0), stop=(kc == KC - 1))
        if KVL >= 2:
            p.copy(ksb[:], ps[:, 0:128], eng="scalar")
            rope_tm(p, kr[:], ksb[:].rearrange("p (h d) -> p h d", h=2), cos[:, tt, :], sin[:, tt, :], 2, ta[:, 0:2, :], tb[:, 0:2, :])
        if KVL >= 3:
            p.copy(kd_[:, :, 0, :], kr[:])
            p.copy(kd_[:, :, 1, :], kr[:])
        p.copy(v_[:], ps[:, 128:256], eng="scalar")
        if KVL >= 4:
            pt = PS[(cnt + 2) % 4]
            kf = kd_[:].rearrange("p a b d -> p (a b d)")
            for j in range(2):
                p.tr(pt[:, j * 128:(j + 1) * 128], kf[:, j * 128:(j + 1) * 128], ident[:])
            p.copy(kt_[:], pt[:, 0:256].rearrange("p (a b) -> p a b", a=2), eng="scalar")
        if KVL >= 5:
            p.dma(S["kT"][:, :, tt * 128:(tt + 1) * 128].rearrange("a p t -> p a t"), kt_[:], q="scalar")
        if KVL >= 1:
            p.dma(S["v"][tt * 128:(tt + 1) * 128, :], v_[:], q="scalar")
        cnt += 1
    if UPTO <= 5:
        p.end()
        return
    cw5 = p.sb("cw5", [5, 3072], F32)
    p.dma(cw5[0:4, :], conv_w_d)
    p.dma(cw5[4:5, :], conv_b_d.rearrange("(o c) -> o c", o=1))
    cwt = p.sb("cwt", [128, 24, 5], F32)
    for ch in range(24):
        p.tr(PS[0][:, ch * 5:(ch + 1) * 5], cw5[:, ch * 128:(ch + 1) * 128], ident[0:5, 0:5])
    p.copy(cwt[:], PS[0][:, 0:120].rearrange("p (a b) -> p a b", a=24))
    cw = cwt
    cb = cwt[:, :, 4]
    if UPTO <= 6:
        p.end()
        return
    wc = [p.sb("wc%d" % i, [128, KC, 128], BF16) for i in range(2)]
    u = [p.sb("u%d" % i, [128, TT_], F32) for i in range(2)]
    acc = [p.sb("acc%d" % i, [128, T], F32) for i in range(2)]
    xcb = [p.sb("xcb%d" % i, [128, T], BF16) for i in range(2)]
    xsb = [p.sb("xsb%d" % i, [128, NT, 128], BF16) for i in range(2)]
    for ch in range(24):
        w_, u_, a_, xb_, xs_ = wc[ch % 2], u[ch % 2], acc[ch % 2], xcb[ch % 2], xsb[ch % 2]
        load_w_bf16(p, w_, wview(2048 + ch * 128, 128), wst, 128)
        for gi, (t0, tn) in enumerate([(0, 512), (512, 512), (1024, 128)]):
            ps = PS[cnt % 4]
            for kc in range(KC):
                p.mm(ps[:, 0:tn], w_[:, kc, :], xT[:, kc, t0:t0 + tn], start=(kc == 0), stop=(kc == KC - 1))
            p.copy(u_[:, t0:t0 + tn], ps[:, 0:tn], eng="scalar" if cnt % 2 else "vector")
            cnt += 1
        p.ts(a_[:], u_[:, 128:TT_], cw[:, ch, 3:4], cw[:, ch, 4:5], op0=ALU.mult, op1=ALU.add)
        for k in range(3):
            sh = 3 - k
            p.stt(a_[:], u_[:, 128 - sh:TT_ - sh], cw[:, ch, k:k + 1], a_[:], ALU.mult, ALU.add)
        if ch < 16 or 16 <= ch < 20:
            p.act(a_[:], a_[:], AF.Silu)
            pb = PB[ch % 2]
            for j in range(NT):
                p.tr(pb[:, j * 128:(j + 1) * 128], a_[:, j * 128:(j + 1) * 128], ident[:])
            p.copy(xs_[:], pb[:].rearrange("p (a b) -> p a b", a=NT), eng="vector")
            if ch < 16:
                p.dma(S["xs"][:, ch * 128:(ch + 1) * 128].rearrange("(a p) c -> p a c", p=128), xs_[:], q="scalar")
            else:
                g = ch - 16
                p.dma(S["Btm"][:, g * 128:(g + 1) * 128].rearrange("(a p) c -> p a c", p=128), xs_[:], q="scalar")
                p.copy(xb_[:], a_[:], eng="gpsimd")
                p.dma(S["BT"][g], xb_[:], q="scalar")
        else:
            g = ch - 20
            p.act(xb_[:], a_[:], AF.Silu)
            p.dma(ct_d[g], xb_[:], q="scalar")
    p.end()


def stage_ea2(p, cst_d, dt_bias_d, a_log_d, d_skip_d, ct_d, S, y_d, e_d, hloc_d, dtot_d):
    p.begin()
    C = load_cst(p, cst_d, ["triu", "tril_s", "ones"])
    U, Lm, ones = C["triu"], C["tril_s"], C["ones"]
    Psm = p.ps("Psm", [128, 512])
    PX = p.ps("PX", [128, 512])
    PA = p.ps("PA", [128, 2048])
    xs = p.sb("xs", [128, NT, 2048], BF16)
    BT = p.sb("BT", [128, 4, T], BF16)
    CT = p.sb("CT", [128, 4, T], BF16)
    Btm = p.sb("Btm", [128, NT, 512], BF16)
    dtraw = p.sb("dtraw", [128, NT, 32], F32)
    p.dma(xs[:], S["xs"].rearrange("(a p) c -> p a c", p=128))
    p.dma(BT[:], S["BT"].rearrange("g n t -> n g t"), q="gpsimd")
    p.dma(CT[:], ct_d.rearrange("g n t -> n g t"), q="gpsimd")
    p.dma(Btm[:], S["Btm"].rearrange("(a p) c -> p a c", p=128))
    p.dma(dtraw[:].rearrange("p a b -> p (a b)"), S["dt"])
    rows = {}
    for nm, d_ in (("dtb", dt_bias_d), ("alog", a_log_d), ("dsk", d_skip_d)):
        rows[nm] = p.sb("row_" + nm, [128, 32], F32)
        p.dma(rows[nm][:], bcast_rows(d_, 32))
    A = p.sb("A", [128, 32], F32)
    p.act(A[:], rows["alog"][:], AF.Exp)
    p.ts(A[:], A[:], -1.0, None, op0=ALU.mult)
    sm = {k: p.sb("s_" + k, [128, 32], F32) for k in ["x", "ab", "e", "l", "dt", "a", "acs", "tot", "eloc", "acc", "dte", "cdec", "carry"]}
    Eall = p.sb("Eall", [128, NT, 32], F32)
    p.memset(sm["carry"][:], 0.0)
    H = p.sb("H", [128, 32, 64], F32)
    Hbf = p.sb("Hbf", [128, 2048], BF16)
    p.memset(H[:], 0.0)
    p.memset(Hbf[:], 0.0)
    xdt = p.sb("xdt", [128, 32, 64], BF16)
    xd2 = p.sb("xd2", [128, 32, 64], BF16)
    cbm = p.sb("cbm", [128, 4, 128], F32)
    rhsA = p.sb("rhsA", [128, 16, 128], F32)
    dec = p.sb("dec", [128, 16, 128], F32)
    G = p.sb("G", [128, 16, 128], BF16)
    ybuf = p.sb("ybuf", [128, 32, 64], F32)
    ytmp = p.sb("ytmp", [128, 16, 64], F32)
    Ht = p.sb("Ht", [128, 32, 64], F32)
    for c in range(NT):
        cs = slice(c * 128, (c + 1) * 128)
        x = sm["x"]
        p.tt(x[:], dtraw[:, c, :], rows["dtb"][:], ALU.add)
        p.stt(sm["ab"][:], x[:], -1.0, x[:], ALU.mult, ALU.max)
        p.act(sm["e"][:], sm["ab"][:], AF.Exp, scale=-1.0)
        p.act(sm["l"][:], sm["e"][:], AF.Ln, bias=1.0)
        p.stt(sm["dt"][:], x[:], 0.0, sm["l"][:], ALU.max, ALU.add)
        p.tt(sm["a"][:], sm["dt"][:], A[:], ALU.mult)
        p.mm(Psm[:, 0:32], U[:], sm["a"][:])
        p.mm(Psm[:, 32:64], ones[:], sm["a"][:])
        p.copy(sm["acs"][:], Psm[:, 0:32])
        p.copy(sm["tot"][:], Psm[:, 32:64])
        p.act(sm["eloc"][:], sm["acs"][:], AF.Exp)
        p.tt(sm["acc"][:], sm["acs"][:], sm["carry"][:], ALU.add)
        p.act(Eall[:, c, :], sm["acc"][:], AF.Exp)
        p.tt(sm["carry"][:], sm["carry"][:], sm["tot"][:], ALU.add)
        p.tt(sm["dte"][:], sm["tot"][:], sm["acs"][:], ALU.subtract)
        p.act(sm["dte"][:], sm["dte"][:], AF.Exp)
        p.act(sm["cdec"][:], sm["tot"][:], AF.Exp)
        p.tt(xdt[:], xs[:, c, :].rearrange("p (h d) -> p h d", h=32), sm["dt"][:, :, None].to_broadcast([128, 32, 64]), ALU.mult)
        p.tt(xd2[:], xdt[:], sm["dte"][:, :, None].to_broadcast([128, 32, 64]), ALU.mult)
        for g in range(4):
            p.mm(PX[:, g * 128:(g + 1) * 128], BT[:, g, cs], CT[:, g, cs])
        p.tt(cbm[:], PX[:].rearrange("p (g l) -> p g l", g=4), U[:, None, :].to_broadcast([128, 4, 128]), ALU.mult)
        for half in range(2):
            hs = slice(half * 16, (half + 1) * 16)
            p.tt(rhsA[:], U[:, None, :].to_broadcast([128, 16, 128]), sm["a"][:, hs, None].to_broadcast([128, 16, 128]), ALU.mult)
            rf = rhsA[:].rearrange("p h l -> p (h l)")
            for j in range(4):
                p.mm(PA[:, j * 512:(j + 1) * 512], Lm[:], rf[:, j * 512:(j + 1) * 512])
            p.act(dec[:].rearrange("p h l -> p (h l)"), PA[:], AF.Exp)
            p.tt(G[:].rearrange("p (g r) l -> p g r l", g=2), dec[:].rearrange("p (g r) l -> p g r l", g=2),
                 cbm[:, half * 2:half * 2 + 2, None, :].to_broadcast([128, 2, 8, 128]), ALU.mult)
            for hh in range(16):
                p.mm(PA[:, hh * 64:(hh + 1) * 64], G[:, hh, :], xdt[:, half * 16 + hh, :])
            for g in range(2):
                gg = half * 2 + g
                p.mm(PA[:, 1024 + g * 512:1024 + (g + 1) * 512], CT[:, gg, cs], Hbf[:, gg * 512:(gg + 1) * 512])
            yh = ybuf[:, hs, :]
            p.tt(yh, PA[:, 1024:2048].rearrange("p (h d) -> p h d", h=16), sm["eloc"][:, hs, None].to_broadcast([128, 16, 64]), ALU.mult)
            p.tt(yh, yh, PA[:, 0:1024].rearrange("p (h d) -> p h d", h=16), ALU.add)
            p.tt(ytmp[:], xs[:, c, half * 1024:(half + 1) * 1024].rearrange("p (h d) -> p h d", h=16),
                 rows["dsk"][:, hs, None].to_broadcast([128, 16, 64]), ALU.mult)
            p.tt(yh, yh, ytmp[:], ALU.add)
        p.dma(y_d[cs, :], ybuf[:].rearrange("p h d -> p (h d)"))
        x2f = xd2[:].rearrange("p h d -> p (h d)")
        for g in range(4):
            p.mm(PA[:, g * 512:(g + 1) * 512], Btm[:, c, g * 128:(g + 1) * 128], x2f[:, g * 512:(g + 1) * 512])
        p.tt(Ht[:], H[:], sm["cdec"][:, :, None].to_broadcast([128, 32, 64]), ALU.mult)
        p.tt(H[:], Ht[:], PA[:].rearrange("p (h d) -> p h d", h=32), ALU.add)
        p.copy(Hbf[:], H[:].rearrange("p h d -> p (h d)"), eng="scalar")
    p.dma(hloc_d, H[:].rearrange("p h d -> p (h d)"))
    p.act(sm["cdec"][:], sm["carry"][:], AF.Exp)
    p.dma(dtot_d, sm["cdec"][:])
    p.dma(e_d.rearrange("(a p) h -> p a h", p=128), Eall[:])
    p.end()


def stage_ea3(p, cst_d, sinks_d, mask_d, S, yatt_d):
    p.begin()
    C = load_cst(p, cst_d, ["ident"], bf=["ident"])
    identb = C["ident_bf"]
    qT = p.sb("qT", [128, 8, T], BF16)
    kT = p.sb("kT", [128, 2, TT_], BF16)
    v = p.sb("v", [128, NTT, 128], BF16)
    p.dma(qT[:], S["qT"].rearrange("a p t -> p a t"))
    p.dma(kT[:], S["kT"].rearrange("a p t -> p a t"), q="gpsimd")
    p.dma(v[:], S["v"].rearrange("(a p) d -> p a d", p=128), q="gpsimd")
    msk = p.sb("msk", [128, 2, 256], F32)
    p.dma(msk[:], mask_d.rearrange("a p s -> p a s"))
    snk = p.sb("snk", [128, 16], F32)
    p.dma(snk[:], bcast_rows(sinks_d, 16))
    PL = [p.ps("PL%d" % i, [128, 2, 512]) for i in range(2)]
    PT = [p.ps("PTa%d" % i, [128, 1024], BF16) for i in range(2)]
    PO = p.ps("PO", [128, 1024])
    lg = [p.sb("lg%d" % i, [128, 2, 256], F32) for i in range(2)]
    P_ = [p.sb("Pp%d" % i, [128, 2, 256], BF16) for i in range(2)]
    PTs = [p.sb("PTs%d" % i, [128, 4, 128], BF16) for i in range(2)]
    sm = [{k: p.sb("a_%s%d" % (k, i), [128, 2], F32) for k in ["mx", "m", "nm", "rs", "sk"]} for i in range(2)]
    rden = p.sb("rden", [128, 16], F32)
    yo = [p.sb("yo%d" % i, [128, 16, 64], F32) for i in range(2)]
    scale = 64 ** -0.5
    it = 0
    for blk in range(NT):
        mk = msk[:, 0 if blk == 0 else 1, :]
        for pr in range(8):
            kh = pr // 4
            pl, l_, pp, pt, pts, s_ = PL[it % 2], lg[it % 2], P_[it % 2], PT[it % 2], PTs[it % 2], sm[it % 2]
            it += 1
            for hh in range(2):
                rs_ = slice(hh * 64, (hh + 1) * 64)
                p.mm(pl[:, hh, 0:256], qT[rs_, pr, blk * 128:(blk + 1) * 128], kT[rs_, kh, blk * 128:blk * 128 + 256])
            p.stt(l_[:], pl[:, :, 0:256], scale, mk[:, None, :].to_broadcast([128, 2, 256]), ALU.mult, ALU.add)
            p.reduce(s_["mx"][:], l_[:], ALU.max)
            p.tt(s_["m"][:], s_["mx"][:], snk[:, 2 * pr:2 * pr + 2], ALU.max)
            p.ts(s_["nm"][:], s_["m"][:], -1.0, None, op0=ALU.mult)
            for hh in range(2):
                p.act(pp[:, hh, :], l_[:, hh, :], AF.Exp, bias=s_["nm"][:, hh:hh + 1], accum_out=s_["rs"][:, hh:hh + 1])
            p.tt(s_["sk"][:], snk[:, 2 * pr:2 * pr + 2], s_["m"][:], ALU.subtract)
            p.act(s_["sk"][:], s_["sk"][:], AF.Exp)
            p.tt(s_["sk"][:], s_["sk"][:], s_["rs"][:], ALU.add)
            p.recip(rden[:, 2 * pr:2 * pr + 2], s_["sk"][:])
            for hh in range(2):
                for kt in range(2):
                    p.tr(pt[:, (hh * 2 + kt) * 128:(hh * 2 + kt + 1) * 128], pp[:, hh, kt * 128:(kt + 1) * 128], identb[:])
            p.copy(pts[:], pt[:, 0:512].rearrange("p (a b) -> p a b", a=4), eng="scalar")
            for hh in range(2):
                for kt in range(2):
                    p.mm(PO[:, (2 * pr + hh) * 64:(2 * pr + hh + 1) * 64], pts[:, hh * 2 + kt, :],
                         v[:, blk + kt, kh * 64:(kh + 1) * 64], start=(kt == 0), stop=(kt == 1))
        y_ = yo[blk % 2]
        p.tt(y_[:], PO[:].rearrange("p (h d) -> p h d", h=16), rden[:, :, None].to_broadcast([128, 16, 64]), ALU.mult)
        p.dma(yatt_d[blk * 128:(blk + 1) * 128, :], y_[:].rearrange("p h d -> p (h d)"))
    p.end()


def stage_eb1(p, cst_d, y_d, z_d, yatt_d, ct_d, e_d, hprev_d, dprev_d, ssm_norm_d, mixT_d):
    p.begin()
    C = load_cst(p, cst_d, ["ident"])
    ident = C["ident"]
    PA = p.ps("PA", [128, 2048])
    PB = [p.ps("PB%d" % i, [128, 1024]) for i in range(2)]
    H = p.sb("H", [128, 32, 64], F32)
    Ht = p.sb("Ht", [128, 32, 64], F32)
    Hbf = p.sb("Hbf", [128, 2048], BF16)
    Sj = [p.sb("Sj%d" % i, [128, 32, 64], F32) for i in range(2)]
    Dj = [p.sb("Dj%d" % i, [128, 32], F32) for i in range(2)]
    p.memset(H[:], 0.0)
    for j in range(7):
        s_, d_ = Sj[j % 2], Dj[j % 2]
        p.dma(s_[:].rearrange("p h d -> p (h d)"), hprev_d[j])
        p.dma(d_[:], dprev_d[j], q="gpsimd")
        p.tt(Ht[:], H[:], d_[:, :, None].to_broadcast([128, 32, 64]), ALU.mult)
        p.tt(H[:], Ht[:], s_[:], ALU.add)
    p.copy(Hbf[:], H[:].rearrange("p h d -> p (h d)"))
    CT = p.sb("CT", [128, 4, T], BF16)
    p.dma(CT[:], ct_d.rearrange("g n t -> n g t"), q="gpsimd")
    E = p.sb("E", [128, NT, 32], F32)
    p.dma(E[:], e_d.rearrange("(a p) h -> p a h", p=128))
    nrm = p.sb("nrm", [128, 2048], F32)
    p.dma(nrm[:], bcast_rows(ssm_norm_d, 2048))
    yl = [p.sb("yl%d" % i, [128, 2048], F32) for i in range(2)]
    zt = [p.sb("zt%d" % i, [128, 2048], F32) for i in range(2)]
    mix = [p.sb("mix%d" % i, [128, 3072], F32) for i in range(2)]
    junk = p.sb("junk", [128, 512], F32)
    ss = [p.sb("ss%d" % i, [128, 4], F32) for i in range(2)]
    mT = [p.sb("mT%d" % i, [128, 24, 128], BF16) for i in range(2)]
    for c in range(NT):
        cs = slice(c * 128, (c + 1) * 128)
        y_, z_, m_, s_, t_ = yl[c % 2], zt[c % 2], mix[c % 2], ss[c % 2], mT[c % 2]
        p.dma(y_[:], y_d[cs, :])
        p.dma(z_[:], z_d[cs, :], q="gpsimd")
        p.dma(m_[:, 2048:3072], yatt_d[cs, :])
        for g in range(4):
            p.mm(PA[:, g * 512:(g + 1) * 512], CT[:, g, cs], Hbf[:, g * 512:(g + 1) * 512])
        yv = m_[:, 0:2048]
        p.tt(yv.rearrange("p (h d) -> p h d", h=32), PA[:].rearrange("p (h d) -> p h d", h=32),
             E[:, c, :, None].to_broadcast([128, 32, 64]), ALU.mult)
        p.tt(yv, yv, y_[:], ALU.add)
        p.act(z_[:], z_[:], AF.Silu)
        p.tt(yv, yv, z_[:], ALU.mult)
        for g in range(4):
            p.act(junk[:], m_[:, g * 512:(g + 1) * 512], AF.Square, accum_out=s_[:, g:g + 1])
        p.ts(s_[:], s_[:], 1.0 / 512, EPS, op0=ALU.mult, op1=ALU.add)
        p.act(s_[:], s_[:], AF.Sqrt)
        p.recip(s_[:], s_[:])
        p.tt(yv.rearrange("p (g d) -> p g d", g=4), yv.rearrange("p (g d) -> p g d", g=4),
             s_[:, :, None].to_broadcast([128, 4, 512]), ALU.mult)
        p.tt(yv, yv, nrm[:], ALU.mult, eng="gpsimd")
        for g3 in range(3):
            pb = PB[g3 % 2]
            for j in range(8):
                k = g3 * 8 + j
                p.tr(pb[:, j * 128:(j + 1) * 128], m_[:, k * 128:(k + 1) * 128], ident[:])
            p.copy(t_[:, g3 * 8:(g3 + 1) * 8, :], pb[:].rearrange("p (a b) -> p a b", a=8), eng="scalar" if g3 % 2 else "vector")
        p.dma(mixT_d[:, :, cs].rearrange("a p t -> p a t"), t_[:], q="scalar")
    p.end()


def stage_proj_ln(p, mixT_d, nk, w_d, x_d, ln_g, ln_b, out_d):
    p.begin()
    mT = p.sb("mT", [128, nk, T], BF16)
    p.dma(mT[:], mixT_d.rearrange("a p t -> p a t"))
    g_rows = p.sb("g_rows", [128, D], F32)
    b_rows = p.sb("b_rows", [128, D], F32)
    p.dma(g_rows[:], bcast_rows(ln_g, D), q="gpsimd")
    p.dma(b_rows[:], bcast_rows(ln_b, D), q="gpsimd")
    vb = p.sb("vb", [128, NT, D], F32)
    p.dma(vb[:], x_d.rearrange("(a p) d -> p a d", p=128))
    wst = [p.sb("wst%d" % i, [128, 4, 512], F32) for i in range(2)]
    wbf = [p.sb("wbf%d" % i, [128, nk, 512], BF16) for i in range(2)]
    PS = [p.ps("P%d" % i, [128, 512]) for i in range(4)]
    cnt = 0
    for n in range(4):
        wb = wbf[n % 2]
        load_w_bf16(p, wb, w_d[:, n * 512:(n + 1) * 512].rearrange("(kc k) n -> k kc n", k=128), wst, 512)
        for tt in range(NT):
            ps = PS[cnt % 4]
            cnt += 1
            for kc in range(nk):
                p.mm(ps[:], mT[:, kc, tt * 128:(tt + 1) * 128], wb[:, kc, :], start=(kc == 0), stop=(kc == nk - 1))
            dst = vb[:, tt, n * 512:(n + 1) * 512]
            p.stt(dst, dst, ALPHA, ps[:], ALU.mult, ALU.add)
    lns = ln_scratch(p)
    for tt in range(NT):
        layer_norm_tile(p, vb[:, tt, :], vb[:, tt, :], g_rows[:], b_rows[:], lns[tt % 2])
        p.dma(out_d[tt * 128:(tt + 1) * 128, :], vb[:, tt, :], q="scalar")
    p.end()


O_IN = 4752
NKEY = 8192
NST = NKEY // 128
NIT = 20
TOPK = 256


def stage_op(p, cst_d, inv_d, pos_d, x_d, w_in_d, kv_norm_d, w_uk_d, kiT_d, kcatT_d, ckv_d, qcatT_d, qiT_d, wi_d, mode, parts="all"):
    p.begin()
    C = load_cst(p, cst_d, ["ident"])
    ident = C["ident"]
    cos, sin = rope_tables(p, pos_d, inv_d, NT)
    BFM = mode == "bf"
    DOK = parts in ("all", "k")
    DOQ = parts in ("all", "q")
    xT = p.sb("xT", [128, KC, T], BF16) if BFM else None
    xTf = None if BFM else p.sb("xTf", [128, KC, T], F32)
    PS = [p.ps("P%d" % i, [128, 512]) for i in range(6)]
    xt = [p.sb("xt0", [128, D], F32), p.sb("xt1", [128, D], F32)]
    for tt in range(NT):
        x = xt[tt % 2]
        p.dma(x[:], x_d[tt * 128:(tt + 1) * 128, :], q="sync" if tt % 2 else "gpsimd")
        for g in range(0, KC, 4):
            ps = PS[(g // 4) % 2]
            for j in range(4):
                p.tr(ps[:, j * 128:(j + 1) * 128], x[:, (g + j) * 128:(g + j + 1) * 128], ident[:])
            pv = ps[:].rearrange("p (a b) -> p a b", a=4)
            if BFM:
                p.copy(xT[:, g:g + 4, tt * 128:(tt + 1) * 128], pv, eng="scalar" if (g // 4) % 2 else "vector")
            else:
                p.copy(xTf[:, g:g + 4, tt * 128:(tt + 1) * 128], pv, eng="scalar" if (g // 4) % 2 else "vector")
    wf = None if BFM else p.sb("wf", [128, KC, 512], F32)
    wst = [p.sb("wst%d" % i, [128, 4, 512] if BFM else [128, 1], F32) for i in range(2)]
    wbf = [p.sb("wbf%d" % i, [128, KC, 512] if BFM else [128, 1], BF16) for i in range(2)]
    cnt = 0
    wc = [p.sb("wc%d" % i, [128, KC, 128] if BFM else [128, 1], BF16) for i in range(2)]
    wuk = [p.sb("wuk%d" % i, [128, 1, 512] if BFM else [128, 1], BF16) for i in range(2)]
    qn = [p.sb("qn%d" % i, [128, 512] if BFM else [128, 1], BF16) for i in range(2)]
    ql = [p.sb("ql%d" % i, [128, 4, 512] if BFM else [128, 1], BF16) for i in range(2)]
    for h in range(16 if (BFM and DOQ) else 0):
        w_, uk_ = wc[h % 2], wuk[h % 2]
        load_w_bf16(p, w_, w_in_d[:, h * 192:h * 192 + 128].rearrange("(kc k) n -> k kc n", k=128), wst, 128)
        load_w_bf16(p, uk_, w_uk_d[h].rearrange("(a k) n -> k a n", a=1), wst, 512)
        for tg in range(2):
            ps, q_, l_ = PS[cnt % 6], qn[cnt % 2], ql[cnt % 2]
            cnt += 1
            for kc in range(KC):
                p.mm(ps[:], w_[:, kc, :], xT[:, kc, tg * 512:(tg + 1) * 512], start=(kc == 0), stop=(kc == KC - 1))
            p.copy(q_[:], ps[:], eng="scalar")
            for rc in range(4):
                ps2 = PS[cnt % 6]
                cnt += 1
                p.mm(ps2[:], uk_[:, 0, rc * 128:(rc + 1) * 128], q_[:])
                p.copy(l_[:, rc, :], ps2[:], eng="scalar" if rc % 2 else "vector")
            p.dma(qcatT_d[0:512, h, tg * 512:(tg + 1) * 512].rearrange("(rc r) t -> r rc t", r=128), l_[:], q="scalar")
    qr = [p.sb("qr%d" % i, [128, 8, 64], F32) for i in range(2)]
    ta = p.sb("ta", [128, 8, 32], F32)
    tb = p.sb("tb", [128, 8, 32], F32)
    qTb = [p.sb("qTb%d" % i, [128, 4, 128] if BFM else [128, 1], BF16) for i in range(2)]
    qTf = [p.sb("qTf%d" % i, [128, 4, 128] if not BFM else [128, 1], F32) for i in range(2)]
    for which in (([0] if BFM else [1]) if DOQ else []):
        for hc in range(2):
            wb = wbf[(which * 2 + hc) % 2]
            if which == 0:
                for hh in range(8):
                    hd = hc * 8 + hh
                    load_w_bf16(p, wb[:, :, hh * 64:(hh + 1) * 64],
                                w_in_d[:, hd * 192 + 128:hd * 192 + 192].rearrange("(kc k) n -> k kc n", k=128), wst, 64, g=4)
            else:
                p.dma(wf[:], w_in_d[:, 3648 + hc * 512:3648 + (hc + 1) * 512].rearrange("(kc k) n -> k kc n", k=128))
            for tt in range(NT):
                ps, q_, qt = PS[cnt % 6], qr[cnt % 2], (qTb if which == 0 else qTf)[cnt % 2]
                cnt += 1
                for kc in range(KC):
                    if which == 0:
                        p.mm(ps[:], xT[:, kc, tt * 128:(tt + 1) * 128], wb[:, kc, :], start=(kc == 0), stop=(kc == KC - 1))
                    else:
                        p.mm(ps[:], xTf[:, kc, tt * 128:(tt + 1) * 128], wf[:, kc, :], start=(kc == 0), stop=(kc == KC - 1))
                rope_tm(p, q_[:], ps[:].rearrange("p (h d) -> p h d", h=8), cos[:, tt, :], sin[:, tt, :], 8, ta[:], tb[:])
                pt = PS[cnt % 6]
                cnt += 1
                qf = q_[:].rearrange("p h d -> p (h d)")
                for j in range(4):
                    p.tr(pt[:, j * 128:(j + 1) * 128], qf[:, j * 128:(j + 1) * 128], ident[:])
                p.copy(qt[:], pt[:].rearrange("p (a b) -> p a b", a=4), eng="scalar")
                ts_ = slice(tt * 128, (tt + 1) * 128)
                for hh2 in range(2):
                    src = qt[hh2 * 64:(hh2 + 1) * 64, :, :]
                    if which == 0:
                        dst = qcatT_d[512:576, hc * 8 + hh2:hc * 8 + 8:2, ts_]
                    else:
                        dst = qiT_d[:, hc * 8 + hh2:hc * 8 + 8:2, ts_]
                    p.dma(dst, src, q="scalar")
    wb = wbf[0]
    if BFM:
        load_w_bf16(p, wb, w_in_d[:, 3072:3584].rearrange("(kc k) n -> k kc n", k=128), wst, 512)
    nrm = p.sb("nrm", [128, 512], F32)
    if BFM:
        p.dma(nrm[:], bcast_rows(kv_norm_d, 512))
    cf = [p.sb("cf%d" % i, [128, 512] if BFM else [128, 1], F32) for i in range(2)]
    cbf = [p.sb("cbf%d" % i, [128, 512] if BFM else [128, 1], BF16) for i in range(2)]
    cT = [p.sb("cT%d" % i, [128, 4, 128] if BFM else [128, 1], BF16) for i in range(2)]
    ss = [p.sb("ss%d" % i, [128, 1], F32) for i in range(2)]
    junk = p.sb("junk", [128, 512], F32)
    for tt in range(NT if (BFM and DOK) else 0):
        ps, c_, cb_, ct_, s_ = PS[cnt % 6], cf[tt % 2], cbf[tt % 2], cT[tt % 2], ss[tt % 2]
        cnt += 1
        ts_ = slice(tt * 128, (tt + 1) * 128)
        for kc in range(KC):
            p.mm(ps[:], xT[:, kc, ts_], wb[:, kc, :], start=(kc == 0), stop=(kc == KC - 1))
        p.act(junk[:], ps[:], AF.Square, accum_out=s_[:])
        p.ts(s_[:], s_[:], 1.0 / 512, EPS, op0=ALU.mult, op1=ALU.add)
        p.act(s_[:], s_[:], AF.Sqrt)
        p.recip(s_[:], s_[:])
        p.stt(c_[:], ps[:], s_[:, 0:1], nrm[:], ALU.mult, ALU.mult)
        p.copy(cb_[:], c_[:], eng="scalar")
        p.dma(ckv_d[ts_, :], cb_[:], q="scalar")
        pt = PS[cnt % 6]
        cnt += 1
        for j in range(4):
            p.tr(pt[:, j * 128:(j + 1) * 128], c_[:, j * 128:(j + 1) * 128], ident[:])
        p.copy(ct_[:], pt[:].rearrange("p (a b) -> p a b", a=4))
        p.dma(kcatT_d[0:512, ts_].rearrange("(rc r) t -> r rc t", r=128), ct_[:], q="scalar")
    wb = wbf[1]
    if BFM:
        load_w_bf16(p, wb[:, :, 0:64], w_in_d[:, 3584:3648].rearrange("(kc k) n -> k kc n", k=128), wst, 64)
    else:
        p.dma(wf[:, :, 0:80], w_in_d[:, 4672:4752].rearrange("(kc k) n -> k kc n", k=128))
    k2 = [p.sb("k2_%d" % i, [128, 2, 64], F32) for i in range(2)]
    k2T = [p.sb("k2T_%d" % i, [128, 128] if BFM else [128, 1], BF16) for i in range(2)]
    k2Tf = [p.sb("k2Tf_%d" % i, [128, 128] if not BFM else [128, 1], F32) for i in range(2)]
    wis = [p.sb("wis%d" % i, [128, 16], F32) for i in range(2)]
    for tt in range(NT if (DOK or not BFM) else 0):
        ps, k_, kt_, w_, ktf_ = PS[cnt % 6], k2[tt % 2], k2T[tt % 2], wis[tt % 2], k2Tf[tt % 2]
        cnt += 1
        ts_ = slice(tt * 128, (tt + 1) * 128)
        if BFM:
            for kc in range(KC):
                p.mm(ps[:, 0:64], xT[:, kc, ts_], wb[:, kc, 0:64], start=(kc == 0), stop=(kc == KC - 1))
            p.copy(k_[:, 1, :], ps[:, 0:64], eng="scalar")
            rope_tm(p, k_[:, 0:1, :], k_[:, 1:2, :], cos[:, tt, :], sin[:, tt, :], 1, ta[:, 0:1, :], tb[:, 0:1, :])
            pt = PS[cnt % 6]
            cnt += 1
            p.tr(pt[0:64, 0:128], k_[:, 0, :], ident[:])
            p.copy(kt_[0:64, :], pt[0:64, 0:128], eng="scalar")
            p.dma(kcatT_d[512:576, ts_], kt_[0:64, :], q="scalar")
        else:
            for kc in range(KC):
                p.mm(ps[:, 0:80], xTf[:, kc, ts_], wf[:, kc, 0:80], start=(kc == 0), stop=(kc == KC - 1))
            p.copy(k_[:, 1, :], ps[:, 0:64], eng="scalar")
            rope_tm(p, k_[:, 0:1, :], k_[:, 1:2, :], cos[:, tt, :], sin[:, tt, :], 1, ta[:, 0:1, :], tb[:, 0:1, :])
            if DOQ:
                p.ts(w_[:], ps[:, 64:80], 1.0 / 32, None, op0=ALU.mult)
                p.dma(wi_d[ts_, :], w_[:], q="scalar")
            if DOK:
                pt = PS[cnt % 6]
                cnt += 1
                p.tr(pt[0:64, 0:128], k_[:, 0, :], ident[:])
                p.copy(ktf_[0:64, :], pt[0:64, 0:128])
                p.dma(kiT_d[:, ts_], ktf_[0:64, :], q="scalar")
    p.end()


def stage_oq(p, cst_d, tqm_d, kiT_d, kcatT_d, ckv_d, qcatT_d, qiT_d, wi_d, w_uv_d, oT_d, nqt=NT, nst_of=None):
    p.begin()
    C = load_cst(p, cst_d, ["ident", "iota", "ones"], bf=["ident", "ones"])
    identb, onesb, iota = C["ident_bf"], C["ones_bf"], C["iota"]
    I4 = p.sb("I4", [128, 4, 128], BF16)
    for j in range(4):
        p.copy(I4[:, j, :], identb[:])
    iota512 = p.sb("iota512", [128, 4, 128], F32)
    for j in range(4):
        p.ts(iota512[:, j, :], iota[:], float(j * 128), None, op0=ALU.add)
    io5 = iota512[:].rearrange("p a b -> p (a b)")
    tqm = p.sb("tqm", [128, NT, 16], F32)
    p.dma(tqm[:], tqm_d)
    PLT = [p.ps("PLT%d" % i, [128, 512]) for i in range(2)]
    POT = p.ps("POT", [128, 4, 512])
    PD = p.ps("PD", [128, 512])
    PM = p.ps("PM", [128, 512])
    kich = [p.sb("kich%d" % i, [64, 512], F32) for i in range(2)]
    wuv = p.sb("wuv", [128, 16, 4, 128], BF16)
    wst = [p.sb("wst%d" % i, [128, 4, 128], F32) for i in range(2)]
    for h in range(16):
        s_ = wst[h % 2]
        p.dma(s_[:], w_uv_d[h].rearrange("(rc r) v -> r rc v", r=128))
        p.copy(wuv[:, h, :, :], s_[:], eng="scalar" if h % 2 else "gpsimd")
    SG = 8
    kc_ = [p.sb("kcs%d" % i, [128, 5, SG * 128], BF16) for i in range(2)]
    cv_ = [p.sb("cvs%d" % i, [128, SG, 512], BF16) for i in range(2)]
    qc = [p.sb("qc%d" % i, [128, 5, 16, 128], BF16) for i in range(2)]
    qi = [p.sb("qi%d" % i, [64, 16, 128], F32) for i in range(2)]
    wi = [p.sb("wi%d" % i, [128, 16], F32) for i in range(2)]
    sc = p.sb("sc", [128, NKEY], F32)
    mb = p.sb("mb", [128, NKEY], BF16)
    junk = mb
    rl = [p.sb("rl%d" % i, [128, 512], F32) for i in range(2)]
    acc = p.sb("acc", [128, 512], F32)
    cm = p.sb("cm", [128, 512], F32)
    mn16 = p.sb("mn16", [128, 16], F32)
    b = {k: p.sb("b_" + k, [128, 1], F32) for k in ["lo", "hi", "mid", "hs", "cnt", "ge", "dl", "dh"]}
    PTt = [p.sb("PTt%d" % i, [128, 512], BF16) for i in range(2)]
    OTn = p.sb("OTn", [128, 4, 512], BF16)
    rrow = p.sb("rrow", [1, 512], F32)
    rrowb = p.sb("rrowb", [1, 512], BF16)
    rdb = p.sb("rdb", [128, 512], F32)
    oTt = [p.sb("oTt%d" % i, [128, 128], BF16) for i in range(2)]
    scale = 192 ** -0.5
    it = 0
    ld = 0
    for qt in range(nqt):
        nst_q = NST if nst_of is None else nst_of[qt]
        nck = nst_q // 4
        nky = nst_q * 128
        q_, qi_, wi_ = qc[qt % 2], qi[qt % 2], wi[qt % 2]
        ts_ = slice(qt * 128, (qt + 1) * 128)
        for ch in range(4):
            p.dma(q_[:, ch, :, :], qcatT_d[ch * 128:(ch + 1) * 128, :, ts_], q="gpsimd")
        p.dma(q_[0:64, 4, :, :], qcatT_d[512:576, :, ts_], q="gpsimd")
        p.dma(qi_[:], qiT_d[:, :, ts_], q="gpsimd")
        p.dma(wi_[:], wi_d[ts_, :], q="gpsimd")
        for ck in range(nck):
            kk = kich[ck % 2]
            p.dma(kk[:], kiT_d[:, ck * 512:(ck + 1) * 512], q="gpsimd")
            for h in range(16):
                ps, r_ = PLT[it % 2], rl[it % 2]
                it += 1
                p.mm(ps[:], qi_[:, h, :], kk[:])
                p.act(r_[:], ps[:], AF.Relu)
                if h == 0:
                    p.ts(acc[:], r_[:], wi_[:, 0:1], None, op0=ALU.mult)
                else:
                    p.stt(acc[:], r_[:], wi_[:, h:h + 1], acc[:], ALU.mult, ALU.add)
            p.reduce(mn16[:, ck:ck + 1], acc[:], ALU.min)
            p.ts(cm[:], io5, tqm[:, qt, ck:ck + 1], -1e30, op0=ALU.is_gt, op1=ALU.mult)
            p.tt(sc[:, ck * 512:(ck + 1) * 512], acc[:], cm[:], ALU.add)
        p.reduce(b["lo"][:], mn16[:, 0:nck], ALU.min)
        p.reduce(b["hi"][:], sc[:, 0:nky], ALU.max)
        for _ in range(NIT):
            p.ts(b["hs"][:], b["hi"][:], 0.5, None, op0=ALU.mult)
            p.stt(b["mid"][:], b["lo"][:], 0.5, b["hs"][:], ALU.mult, ALU.add)
            p.ts(junk[:, 0:nky], sc[:, 0:nky], b["mid"][:, 0:1], 0.0, op0=ALU.is_ge, op1=ALU.add, accum_out=b["cnt"][:])
            p.ts(b["ge"][:], b["cnt"][:], float(TOPK), None, op0=ALU.is_ge)
            p.tt(b["dl"][:], b["mid"][:], b["lo"][:], ALU.subtract)
            p.tt(b["dh"][:], b["hi"][:], b["mid"][:], ALU.subtract)
            p.stt(b["lo"][:], b["dl"][:], b["ge"][:, 0:1], b["lo"][:], ALU.mult, ALU.add)
            p.stt(b["hi"][:], b["dh"][:], b["ge"][:, 0:1], b["mid"][:], ALU.mult, ALU.add)
        p.ts(mb[:, 0:nky], sc[:, 0:nky], b["lo"][:, 0:1], -30000.0, op0=ALU.is_lt, op1=ALU.mult)
        for hg in range(4):
            hs = slice(hg * 4, (hg + 1) * 4)
            grp = {}

            def emit_qk(st):
                nonlocal ld
                g = st // SG
                if st % SG == 0:
                    k_, c_ = kc_[ld % 2], cv_[ld % 2]
                    ld += 1
                    grp[g] = (k_, c_)
                    ks = slice(st * 128, (st + SG) * 128)
                    p.dma(k_[:, 0:4, :], kcatT_d[0:512, ks].rearrange("(c r) s -> r c s", r=128), q="sync")
                    p.dma(k_[0:64, 4, :], kcatT_d[512:576, ks], q="sync")
                    p.dma(c_[:], ckv_d[ks, :].rearrange("(a s) r -> s a r", s=128), q="sync")
                k_, c_ = grp[g]
                so = (st % SG) * 128
                ps = PLT[st % 2]
                for ch in range(4):
                    p.mm(ps[:], k_[:, ch, so:so + 128], q_[:, ch, hs, :], start=(ch == 0), stop=False)
                p.mm(ps[:], k_[0:64, 4, so:so + 128], q_[0:64, 4, hs, :], start=False, stop=False)
                p.mm(ps[:], mb[:, st * 128:(st + 1) * 128], I4[:], start=False, stop=True)

            def emit_pv(st):
                k_, c_ = grp[st // SG]
                ps, pt_ = PLT[st % 2], PTt[st % 2]
                p.act(pt_[:], ps[:], AF.Exp, scale=scale)
                for rc in range(4):
                    p.mm(POT[:, rc, :], c_[:, st % SG, rc * 128:(rc + 1) * 128], pt_[:], start=(st == 0), stop=(st == nst_q - 1))
                p.mm(PD[0:1, :], onesb[:, 0:1], pt_[:], start=(st == 0), stop=(st == nst_q - 1))

            emit_qk(0)
            for st in range(nst_q):
                if st + 1 < nst_q:
                    emit_qk(st + 1)
                emit_pv(st)
            p.copy(rrow[:], PD[0:1, :])
            p.recip(rrow[:], rrow[:])
            p.mm(PM[:], C["ones"][0:1, :], rrow[:])
            p.copy(rdb[:], PM[:], eng="scalar")
            for rc in range(4):
                p.tt(OTn[:, rc, :], POT[:, rc, :], rdb[:], ALU.mult)
            for hh in range(4):
                h = hg * 4 + hh
                o_ = oTt[h % 2]
                for rc in range(4):
                    p.mm(PM[:, 0:128], wuv[:, h, rc, :], OTn[:, rc, hh * 128:(hh + 1) * 128], start=(rc == 0), stop=(rc == 3))
                p.copy(o_[:], PM[:, 0:128], eng="scalar")
                p.dma(oT_d[h, :, ts_], o_[:], q="scalar")
    p.end()


NPDT = {F32: np.float32, I32: np.int32}


def _mk(nc):
    def din(name, shape, dt=F32):
        return nc.dram_tensor(name, list(shape), dt, kind="ExternalInput").ap()

    def dout(name, shape, dt=F32):
        return nc.dram_tensor(name, list(shape), dt, kind="ExternalOutput").ap()
    return din, dout


NCST = 128 * len(CST_NAMES)


def build_ea(stages=(1, 2, 3)):
    nc = bass.Bass("TRN2", target_bir_lowering=False)
    din, dout = _mk(nc)
    cst = din("cst", [128, NCST]); inv = din("inv", [128, 32]); pos = din("pos", [128, NTT], I32)
    xh = din("xh", [TT_, D]); w_in = din("w_in", [D, E_IN]); conv_w = din("conv_w", [4, 3072]); conv_b = din("conv_b", [3072])
    dt_bias = din("dt_bias", [32]); a_log = din("a_log", [32]); d_skip = din("d_skip", [32]); sinks = din("sinks", [16])
    mask = din("mask", [2, 128, 256])
    y = dout("y", [T, 2048]); z = dout("z", [T, 2048]); yatt = dout("yatt", [T, 1024]); ct = dout("ct", [4, 128, T], BF16)
    e = dout("e", [T, 32]); hloc = dout("hloc", [128, 2048]); dtot = dout("dtot", [128, 32])
    p = Prog(nc)
    SC = ea_scratch(p, "a")
    if 1 in stages:
        stage_ea1(p, cst, inv, pos, xh, w_in, conv_w, conv_b, z, ct, SC)
    if 2 in stages:
        stage_ea2(p, cst, dt_bias, a_log, d_skip, ct, SC, y, e, hloc, dtot)
    if 3 in stages:
        stage_ea3(p, cst, sinks, mask, SC, yatt)
    p.finish()
    return nc


def _moe_tail(p, nc, din, cst, x1, out):
    rg = din("rg", [D, 4]); rgb = din("rgb", [4]); re_ = din("re", [D, 32]); reb = din("reb", [32])
    wg = din("wg", [32, D, 512]); wu = din("wu", [32, D, 512]); wd = din("wd", [32, 512, D])
    pl = din("pl", [T, 256]); g2 = din("g2", [D]); b2 = din("b2", [D]); pwg = din("pwg", [D, D]); pbg = din("pbg", [D]); pwp = din("pwp", [256, D])
    ffn = p.dram("ffn_s", [T, D]).ap()
    stage_moe(p, cst, x1, rg, rgb, re_, reb, wg, wu, wd, ffn, "m")
    stage_tail(p, {"ident": cst[:, 0:128]}, x1, ffn, pl, g2, b2, pwg, pbg, pwp, out)


def _op(p, din, cst, inv, x, ext_k, parts):
    pos8 = din("pos8", [128, NT], I32)
    ow_in = din("ow_in", [D, O_IN]); kvn = din("kvn", [512]); wuk = din("wuk", [16, 128, 512])
    if ext_k:
        _, dout = _mk(p.nc)
        kiT = dout("kiT", [64, T]); kcatT = dout("kcatT", [576, T], BF16); ckv = dout("ckv", [T, 512], BF16)
    else:
        kiT = p.dram("kiT_s", [64, T]).ap(); kcatT = p.dram("kcatT_s", [576, T], BF16).ap(); ckv = p.dram("ckv_s", [T, 512], BF16).ap()
    qcatT = p.dram("qcatT_s", [576, 16, T], BF16).ap(); qiT = p.dram("qiT_s", [64, 16, T]).ap(); wi = p.dram("wi_s", [T, 16]).ap()
    for mode in ("bf", "f32"):
        stage_op(p, cst, inv, pos8, x, ow_in, kvn, wuk, kiT, kcatT, ckv, qcatT, qiT, wi, mode, parts)
    return qcatT, qiT, wi


def build_eb():
    nc = bass.Bass("TRN2", target_bir_lowering=False)
    din, dout = _mk(nc)
    cst = din("cst", [128, NCST]); inv = din("inv", [128, 32])
    y = din("y", [T, 2048]); z = din("z", [T, 2048]); yatt = din("yatt", [T, 1024]); ct = din("ct", [4, 128, T], BF16)
    e = din("e", [T, 32]); hprev = din("hprev", [7, 128, 2048]); dprev = din("dprev", [7, 128, 32])
    x = din("x", [T, D]); ssm_norm = din("ssm_norm", [2048]); w_out = din("w_out", [3072, D]); g1 = din("g1", [D]); b1 = din("b1", [D])
    xo = dout("xo", [T, D])
    p = Prog(nc)
    mixT = p.dram("mixT_s", [24, 128, T], BF16).ap()
    x1 = p.dram("x1_s", [T, D]).ap()
    stage_eb1(p, cst, y, z, yatt, ct, e, hprev, dprev, ssm_norm, mixT)
    stage_proj_ln(p, mixT, 24, w_out, x, g1, b1, x1)
    _moe_tail(p, nc, din, cst, x1, xo)
    _op(p, din, cst, inv, xo, True, "k")
    p.finish()
    return nc


def build_odd():
    nc = bass.Bass("TRN2", target_bir_lowering=False)
    din, dout = _mk(nc)
    cst = din("cst", [128, NCST]); inv = din("inv", [128, 32])
    x = din("x", [T, D]); tqm = din("tqm", [128, NT, 16])
    kiT_f = din("kiT_f", [64, NKEY]); kcatT_f = din("kcatT_f", [576, NKEY], BF16); ckv_f = din("ckv_f", [NKEY, 512], BF16)
    wuv = din("wuv", [16, 512, 128]); ow_out = din("ow_out", [D, D]); g1 = din("g1", [D]); b1 = din("b1", [D])
    xo = dout("xo", [T, D])
    p = Prog(nc)
    qcatT, qiT, wi = _op(p, din, cst, inv, x, False, "q")
    oT = p.dram("oT_s", [16, 128, T], BF16).ap()
    x1 = p.dram("x1_s", [T, D]).ap()
    stage_oq(p, cst, tqm, kiT_f, kcatT_f, ckv_f, qcatT, qiT, wi, wuv, oT, nst_of=[8 * (j + 1) for j in range(NT)])
    stage_proj_ln(p, oT, 16, ow_out, x, g1, b1, x1)
    _moe_tail(p, nc, din, cst, x1, xo)
    p.finish()
    return nc


def _run(nc, in_maps):
    res = run_bass_kernel_spmd(nc, in_maps, core_ids=list(range(8)))
    return res.results


def kernel(_dbg=None, **I):
    NCORE = 8
    C = lambda a: np.ascontiguousarray(a)
    cst = make_cst(); inv = make_inv()
    xcur = C(I["x"][0])
    P = np.asarray(I["positions"][0], np.int32)
    qi_ = np.arange(128)[:, None]; kj = np.arange(256)[None, :]
    band = (kj > qi_) & (kj <= qi_ + 128)
    m_rest = np.where(band, 0.0, -30000.0).astype(np.float32)
    m_first0 = np.where(band & (kj >= 128), 0.0, -30000.0).astype(np.float32)

    def moe_tail_inputs(L, c):
        sl = slice(c * T, (c + 1) * T)
        return {"rg": C(I["moe_router_group"][L]), "rgb": C(I["moe_router_group_b"][L]), "re": C(I["moe_router_expert"][L]),
                "reb": C(I["moe_router_expert_b"][L]), "wg": WG[L], "wu": WU[L], "wd": WD[L],
                "pl": C(I["p"][L, 0, sl]), "g2": C(I["ln2_g"][L]), "b2": C(I["ln2_b"][L]), "pwg": PWG[L],
                "pbg": C(I["ple_b_gate"][L]), "pwp": C(I["ple_w_proj"][L])}
    WG = [C(I["moe_w_gate"][L]) for L in range(4)]; WU = [C(I["moe_w_up"][L]) for L in range(4)]; WD = [C(I["moe_w_down"][L]) for L in range(4)]
    PWG = [C(I["ple_w_gate"][L]) for L in range(4)]
    nc_ea = nc_eb = nc_odd = None
    for L in range(4):
        j = L // 2
        if L % 2 == 0:
            nc_ea = nc_ea or build_ea()
            w_in = C(I["ev_w_in"][j])
            ims = []
            for c in range(NCORE):
                xh = np.zeros((TT_, D), np.float32); pe = np.zeros(TT_, np.int32)
                lo = c * T - 128
                if c > 0:
                    xh[:] = xcur[lo:lo + TT_]; pe[:] = P[lo:lo + TT_]
                else:
                    xh[128:] = xcur[0:T]; pe[128:] = P[0:T]
                ims.append({"cst": cst, "inv": inv, "pos": C(pe.reshape(NTT, 128).T), "xh": xh, "w_in": w_in,
                            "conv_w": C(I["ev_conv_w"][j]), "conv_b": C(I["ev_conv_b"][j]), "dt_bias": C(I["ev_dt_bias"][j]),
                            "a_log": C(I["ev_a_log"][j]), "d_skip": C(I["ev_d_skip"][j]), "sinks": C(I["ev_sinks"][j]),
                            "mask": np.stack([m_first0 if c == 0 else m_rest, m_rest])})
            ra = _run(nc_ea, ims)
            nc_eb = nc_eb or build_eb()
            ims = []
            for c in range(NCORE):
                sl = slice(c * T, (c + 1) * T)
                hp = np.zeros((7, 128, 2048), np.float32); dp = np.ones((7, 128, 32), np.float32)
                for cc in range(c):
                    hp[7 - c + cc] = ra[cc]["hloc"]; dp[7 - c + cc] = ra[cc]["dtot"]
                m = {"cst": cst, "inv": inv, "y": ra[c]["y"], "z": ra[c]["z"], "yatt": ra[c]["yatt"], "ct": ra[c]["ct"], "e": ra[c]["e"],
                     "hprev": hp, "dprev": dp, "x": C(xcur[sl]), "ssm_norm": C(I["ev_ssm_norm"][j]), "w_out": C(I["ev_w_out"][j]),
                     "g1": C(I["ln1_g"][L]), "b1": C(I["ln1_b"][L]), "pos8": C(P[sl].reshape(NT, 128).T),
                     "ow_in": C(I["od_w_in"][j]), "kvn": C(I["od_kv_norm"][j]), "wuk": C(I["od_w_uk"][j])}
                m.update(moe_tail_inputs(L, c))
                ims.append(m)
            rb = _run(nc_eb, ims)
            xcur = np.concatenate([rb[c]["xo"] for c in range(NCORE)], 0)
            kiT_f = np.concatenate([rb[c]["kiT"] for c in range(NCORE)], 1)
            kcatT_f = np.concatenate([rb[c]["kcatT"] for c in range(NCORE)], 1)
            ckv_f = np.concatenate([rb[c]["ckv"] for c in range(NCORE)], 0)
        else:
            nc_odd = nc_odd or build_odd()
            ims = []
            for c in range(NCORE):
                tok = ((np.arange(NT)[:, None] * NCORE + c) * 128 + np.arange(128)[None, :]).reshape(-1)
                tq = tok.reshape(NT, 128).T.astype(np.float32)
                tqm = C(tq[:, :, None] - (np.arange(16) * 512)[None, None, :].astype(np.float32))
                m = {"cst": cst, "inv": inv, "x": C(xcur[tok]), "tqm": tqm, "kiT_f": C(kiT_f), "kcatT_f": C(kcatT_f), "ckv_f": C(ckv_f),
                     "wuv": C(I["od_w_uv"][j]), "ow_out": C(I["od_w_out"][j]), "g1": C(I["ln1_g"][L]), "b1": C(I["ln1_b"][L]),
                     "pos8": C(P[tok].reshape(NT, 128).T), "ow_in": C(I["od_w_in"][j]), "kvn": C(I["od_kv_norm"][j]), "wuk": C(I["od_w_uk"][j])}
                m.update(moe_tail_inputs(L, c))
                m["pl"] = C(I["p"][L, 0][tok])
                ims.append(m)
            ro = _run(nc_odd, ims)
            xnew = np.empty_like(xcur)
            for c in range(NCORE):
                tok = ((np.arange(NT)[:, None] * NCORE + c) * 128 + np.arange(128)[None, :]).reshape(-1)
                xnew[tok] = ro[c]["xo"]
            xcur = xnew
        if _dbg is not None:
            _dbg(L, xcur)
    return xcur[None].astype(np.float32)
```
